# Optimizing a Trainium2 kernel written in Bass

```python
import math
import jax, jax.numpy as jnp
from jax import lax
import numpy as np

D_MODEL = 2048
BATCH = 1
SEQ = 16384
DEPTH = 1

D_MIX = D_MODEL
ATTN_WIDTH = D_MIX // 2
REC_WIDTH = D_MIX - ATTN_WIDTH
DA_HEAD_DIM = 128
DA_HEADS = ATTN_WIDTH // (2 * DA_HEAD_DIM)
REC_BLOCKS = 8
REC_BLOCK_DIM = REC_WIDTH // REC_BLOCKS
CONV_W = 4
RG_C = 8.0
IN_PROJ_DIM = 3 * ATTN_WIDTH + 2 * REC_WIDTH
Q_BLOCK = 128
N_MEM = 256
CROSS_HEADS = 4
CROSS_HEAD_DIM = D_MODEL // CROSS_HEADS
N_EXPERTS = 32
TOP_K = 4
D_FF = D_MODEL
SWIGLU_LIMIT = 7.0
SWIGLU_ALPHA = 1.702
MOE_BLOCK = 128
EPS = 1e-6
DA_EPS = 1e-5

kernel_name = "hymba_diffattn_rglru_moe_layer"


def rmsnorm(x, g, eps=EPS):
    x32 = x.astype(jnp.float32)
    y = x32 * lax.rsqrt(jnp.mean(x32 * x32, axis=-1, keepdims=True) + eps)
    return (y * g.astype(jnp.float32)).astype(x.dtype)


def diff_attention(q, k, v, lq1, lk1, lq2, lk2, subln_g, lambda_init):
    B, S, _ = q.shape
    nb = S // Q_BLOCK
    out_dtype = q.dtype
    qb = q.reshape(B, nb, Q_BLOCK, DA_HEADS, 2, DA_HEAD_DIM).transpose(1, 0, 3, 4, 2, 5).astype(jnp.float32)
    kh = k.reshape(B, S, DA_HEADS, 2, DA_HEAD_DIM).transpose(0, 2, 3, 1, 4).astype(jnp.float32)
    vh = v.reshape(B, S, DA_HEADS, 2 * DA_HEAD_DIM).transpose(0, 2, 1, 3).astype(jnp.float32)
    f32 = jnp.float32
    lam = (jnp.exp(jnp.sum(lq1.astype(f32) * lk1.astype(f32)))
           - jnp.exp(jnp.sum(lq2.astype(f32) * lk2.astype(f32))) + lambda_init)
    scale = DA_HEAD_DIM ** -0.5
    k_pos = jnp.arange(S)

    def one_block(args):
        q_blk, blk = args
        q_pos = blk * Q_BLOCK + jnp.arange(Q_BLOCK)
        s = jnp.einsum('bhiqd,bhikd->bhiqk', q_blk, kh) * scale
        s = jnp.where(k_pos[None, :] <= q_pos[:, None], s, -jnp.inf)
        p = jax.nn.softmax(s, axis=-1)
        a = p[:, :, 0] - lam * p[:, :, 1]
        return jnp.einsum('bhqk,bhkd->bhqd', a, vh)

    o = lax.map(one_block, (qb, jnp.arange(nb)))
    o = o.transpose(1, 0, 3, 2, 4).reshape(B, S, DA_HEADS, 2 * DA_HEAD_DIM)
    o = rmsnorm(o, subln_g, DA_EPS) * (1.0 - lambda_init)
    return o.reshape(B, S, ATTN_WIDTH).astype(out_dtype)


def rg_lru_group(xr, xg, conv_w, conv_b, w_a, b_a, w_x, b_x, rg_lambda, rec_norm_g):
    B, S, C = xr.shape
    xc = lax.conv_general_dilated(
        xr, conv_w[:, None, :].astype(xr.dtype), window_strides=(1,),
        padding=[(CONV_W - 1, 0)], dimension_numbers=('NWC', 'WIO', 'NWC'),
        feature_group_count=C) + conv_b
    xb = xc.reshape(B, S, REC_BLOCKS, REC_BLOCK_DIM)
    r = jax.nn.sigmoid(jnp.einsum('bsnc,ncd->bsnd', xb, w_a).reshape(B, S, C) + b_a)
    i = jax.nn.sigmoid(jnp.einsum('bsnc,ncd->bsnd', xb, w_x).reshape(B, S, C) + b_x)
    log_a = -RG_C * jax.nn.softplus(-rg_lambda.astype(jnp.float32)) * r.astype(jnp.float32)
    a = jnp.exp(log_a)
    b = jnp.sqrt(-jnp.expm1(2.0 * log_a)) * (i * xc).astype(jnp.float32)

    def combine(left, right):
        a1, b1 = left
        a2, b2 = right
        return a1 * a2, a2 * b1 + b2

    _, h = lax.associative_scan(combine, (a, b), axis=1)
    y = h.astype(xr.dtype) * jax.nn.gelu(xg)
    return rmsnorm(y, rec_norm_g)


def cross_attention(h, mem_n, w_cq, w_ckv, w_co):
    B, S, D = h.shape
    M = mem_n.shape[1]
    q = (h @ w_cq).reshape(B, S, CROSS_HEADS, CROSS_HEAD_DIM)
    kv = mem_n @ w_ckv
    k = kv[..., :D].reshape(B, M, CROSS_HEADS, CROSS_HEAD_DIM)
    v = kv[..., D:].reshape(B, M, CROSS_HEADS, CROSS_HEAD_DIM)
    s = jnp.einsum('bshd,bmhd->bhsm', q, k).astype(jnp.float32) * (CROSS_HEAD_DIM ** -0.5)
    p = jax.nn.softmax(s, axis=-1).astype(v.dtype)
    o = jnp.einsum('bhsm,bmhd->bshd', p, v).reshape(B, S, D)
    return o @ w_co


def moe(h, w_router, b_router, w_gu, b_gu, w_down, b_down):
    B, S, D = h.shape
    T = B * S
    M = T * TOP_K
    xf = h.reshape(T, D)
    logits = jnp.dot(xf, w_router).astype(jnp.float32) + b_router.astype(jnp.float32)
    top_vals, top_idx = lax.top_k(logits, TOP_K)
    gates = jax.nn.softmax(top_vals, axis=-1)
    flat_e = top_idx.reshape(M)
    order = jnp.argsort(flat_e)
    e_sorted = flat_e[order]
    tok_sorted = order // TOP_K
    g_sorted = gates.reshape(M)[order]
    counts = jnp.bincount(flat_e, length=N_EXPERTS)
    padded = ((counts + MOE_BLOCK - 1) // MOE_BLOCK) * MOE_BLOCK
    starts = jnp.cumsum(counts) - counts
    pad_ends = jnp.cumsum(padded)
    pad_starts = pad_ends - padded
    dest = pad_starts[e_sorted] + jnp.arange(M) - starts[e_sorted]
    m_pad = M + N_EXPERTS * MOE_BLOCK
    n_blk = m_pad // MOE_BLOCK
    row_tok = jnp.zeros((m_pad,), jnp.int32).at[dest].set(tok_sorted.astype(jnp.int32))
    row_gate = jnp.zeros((m_pad,), jnp.float32).at[dest].set(g_sorted)
    blk_e = jnp.minimum(jnp.searchsorted(pad_ends, jnp.arange(n_blk) * MOE_BLOCK, side='right'),
                        N_EXPERTS - 1)

    def step(acc, args):
        tok, g, e = args
        xb = xf[tok]
        gu = xb @ w_gu[e] + b_gu[e]
        gate = jnp.minimum(gu[:, :D_FF], SWIGLU_LIMIT)
        up = jnp.clip(gu[:, D_FF:], -SWIGLU_LIMIT, SWIGLU_LIMIT)
        act = (up + 1.0) * gate * jax.nn.sigmoid(SWIGLU_ALPHA * gate)
        y = act @ w_down[e] + b_down[e]
        return acc.at[tok].add(y * g[:, None].astype(y.dtype)), None

    acc, _ = lax.scan(step, jnp.zeros_like(xf),
                      (row_tok.reshape(n_blk, MOE_BLOCK), row_gate.reshape(n_blk, MOE_BLOCK), blk_e))
    return acc.reshape(B, S, D)


def setup_inputs(seed: int = 0) -> dict:
    key = jax.random.key(seed)
    ks = iter(jax.random.split(key, 40))
    f32 = jnp.float32

    def nrm(shape, scale):
        return jax.random.normal(next(ks), shape, f32) * scale

    def gain(shape):
        return 1.0 + 0.02 * jax.random.normal(next(ks), shape, f32)

    L = DEPTH
    x = nrm((BATCH, SEQ, D_MODEL), 1.0)
    mem = nrm((BATCH, N_MEM, D_MODEL), 1.0)
    norm_mix_g = gain((L, D_MODEL))
    w_in = nrm((L, D_MODEL, IN_PROJ_DIM), D_MODEL ** -0.5)
    conv_w = nrm((L, CONV_W, REC_WIDTH), CONV_W ** -0.5)
    conv_b = nrm((L, REC_WIDTH), 0.02)
    w_rg_a = nrm((L, REC_BLOCKS, REC_BLOCK_DIM, REC_BLOCK_DIM), REC_BLOCK_DIM ** -0.5)
    b_rg_a = nrm((L, REC_WIDTH), 0.02)
    w_rg_x = nrm((L, REC_BLOCKS, REC_BLOCK_DIM, REC_BLOCK_DIM), REC_BLOCK_DIM ** -0.5)
    b_rg_x = nrm((L, REC_WIDTH), 0.02)
    a_c = jax.random.uniform(next(ks), (L, REC_WIDTH), f32, 0.9, 0.999)
    a0 = a_c ** (1.0 / RG_C)
    rg_lambda = jnp.log(a0) - jnp.log1p(-a0)
    rec_norm_g = gain((L, REC_WIDTH))
    lambda_q1 = nrm((L, DA_HEAD_DIM), 0.1)
    lambda_k1 = nrm((L, DA_HEAD_DIM), 0.1)
    lambda_q2 = nrm((L, DA_HEAD_DIM), 0.1)
    lambda_k2 = nrm((L, DA_HEAD_DIM), 0.1)
    subln_g = gain((L, 2 * DA_HEAD_DIM))
    w_out = nrm((L, D_MIX, D_MODEL), D_MIX ** -0.5)
    norm_cross_g = gain((L, D_MODEL))
    norm_mem_g = gain((L, D_MODEL))
    w_cq = nrm((L, D_MODEL, D_MODEL), D_MODEL ** -0.5)
    w_ckv = nrm((L, D_MODEL, 2 * D_MODEL), D_MODEL ** -0.5)
    w_co = nrm((L, D_MODEL, D_MODEL), D_MODEL ** -0.5)
    norm_ffn_g = gain((L, D_MODEL))
    w_router = nrm((L, D_MODEL, N_EXPERTS), D_MODEL ** -0.5)
    b_router = nrm((L, N_EXPERTS), 0.01)
    w_gate_up = nrm((L, N_EXPERTS, D_MODEL, 2 * D_FF), D_MODEL ** -0.5)
    b_gate_up = nrm((L, N_EXPERTS, 2 * D_FF), 0.02)
    w_down = nrm((L, N_EXPERTS, D_FF, D_MODEL), D_FF ** -0.5)
    b_down = nrm((L, N_EXPERTS, D_MODEL), 0.02)
    norm_final_g = gain((D_MODEL,))
    return {"x": x, "mem": mem, "norm_mix_g": norm_mix_g, "w_in": w_in,
            "conv_w": conv_w, "conv_b": conv_b, "w_rg_a": w_rg_a, "b_rg_a": b_rg_a,
            "w_rg_x": w_rg_x, "b_rg_x": b_rg_x, "rg_lambda": rg_lambda, "rec_norm_g": rec_norm_g,
            "lambda_q1": lambda_q1, "lambda_k1": lambda_k1, "lambda_q2": lambda_q2, "lambda_k2": lambda_k2,
            "subln_g": subln_g, "w_out": w_out, "norm_cross_g": norm_cross_g, "norm_mem_g": norm_mem_g,
            "w_cq": w_cq, "w_ckv": w_ckv, "w_co": w_co, "norm_ffn_g": norm_ffn_g,
            "w_router": w_router, "b_router": b_router, "w_gate_up": w_gate_up, "b_gate_up": b_gate_up,
            "w_down": w_down, "b_down": b_down, "norm_final_g": norm_final_g}


def reference(x, mem, norm_mix_g, w_in, conv_w, conv_b, w_rg_a, b_rg_a, w_rg_x, b_rg_x,
              rg_lambda, rec_norm_g, lambda_q1, lambda_k1, lambda_q2, lambda_k2, subln_g, w_out,
              norm_cross_g, norm_mem_g, w_cq, w_ckv, w_co, norm_ffn_g, w_router, b_router,
              w_gate_up, b_gate_up, w_down, b_down, norm_final_g):
    h = x
    o_k = ATTN_WIDTH
    o_v = 2 * ATTN_WIDTH
    o_r = 3 * ATTN_WIDTH
    o_g = 3 * ATTN_WIDTH + REC_WIDTH
    for l in range(DEPTH):
        lambda_init = 0.8 - 0.6 * math.exp(-0.3 * l)
        n = rmsnorm(h, norm_mix_g[l])
        proj = n @ w_in[l]
        q = proj[..., :o_k]
        k = proj[..., o_k:o_v]
        v = proj[..., o_v:o_r]
        xr = proj[..., o_r:o_g]
        xg = proj[..., o_g:]
        attn_out = diff_attention(q, k, v, lambda_q1[l], lambda_k1[l], lambda_q2[l], lambda_k2[l],
                                  subln_g[l], lambda_init)
        rec_out = rg_lru_group(xr, xg, conv_w[l], conv_b[l], w_rg_a[l], b_rg_a[l], w_rg_x[l], b_rg_x[l],
                               rg_lambda[l], rec_norm_g[l])
        h = h + jnp.concatenate([attn_out, rec_out], axis=-1) @ w_out[l]
        h = h + cross_attention(rmsnorm(h, norm_cross_g[l]), rmsnorm(mem, norm_mem_g[l]),
                                w_cq[l], w_ckv[l], w_co[l])
        h = h + moe(rmsnorm(h, norm_ffn_g[l]), w_router[l], b_router[l], w_gate_up[l], b_gate_up[l],
                    w_down[l], b_down[l])
    return rmsnorm(h, norm_final_g)
```

```python
import math
from contextlib import ExitStack
import numpy as np
import ml_dtypes
import concourse.bass as bass
import concourse.mybir as mybir
from concourse.bass_utils import run_bass_kernel_spmd

F32 = mybir.dt.float32
BF = mybir.dt.bfloat16
AF = mybir.ActivationFunctionType
ALU = mybir.AluOpType
AX = mybir.AxisListType

D = 2048
KC = 16
ST = 512
NCORES = 8
NEXP = 32
NMEM = 256
EPS = 1e-6
DA_EPS = 1e-5
LAMBDA_INIT = 0.8 - 0.6 * math.exp(0.0)


class Buf:
    __slots__ = ("w", "r")

    def __init__(self):
        self.w = None
        self.r = {}


class EngQ:
    def __init__(self, name, sem, dsems):
        self.name = name
        self.sem = sem
        self.cnt = 0
        self.items = []
        self.dsems = dsems
        self.dn = 0
        self.waited = {}


class K:
    def __init__(self, nc, nds=6):
        self.nc = nc
        self.epoch = 0
        self.q = {}
        self.h = {"pe": nc.tensor, "act": nc.scalar, "dve": nc.vector, "pool": nc.gpsimd, "sp": nc.sync}
        for name in ("pe", "act", "dve", "pool", "sp"):
            sem = nc.alloc_semaphore("prog_" + name)
            dsems = []
            if name in ("sp", "pool", "act"):
                dsems = [nc.alloc_semaphore("dma_%s_%d" % (name, i)) for i in range(nds)]
            self.q[name] = EngQ(name, sem, dsems)

    def _waits(self, e, deps):
        out = []
        for (sem, val, src) in deps:
            if src == "pe" and e.name == "pe":
                continue
            key = id(sem)
            if e.waited.get(key, (None, 0))[1] >= val:
                continue
            e.waited[key] = (sem, val)
            out.append((sem, val))
        return out

    def op(self, eng, fn, reads=(), writes=(), dma=False):
        e = self.q[eng]
        deps = []
        for b in reads:
            if b.w is not None:
                deps.append(b.w)
        for b in writes:
            if b.w is not None:
                deps.append(b.w)
            deps.extend(b.r.values())
        if dma:
            ns = len(e.dsems)
            slot = e.dn % ns
            rnd = e.dn // ns
            e.dn += 1
            sem = e.dsems[slot]
            if rnd > 0:
                deps.append((sem, 16 * rnd, "dma"))
            tok = (sem, 16 * (rnd + 1), "dma")
            inc = (sem, 16)
        else:
            e.cnt += 1
            tok = (e.sem, e.cnt, eng)
            inc = (e.sem, 1)
        waits = self._waits(e, deps)
        h = self.h[eng]
        for (wsem, wval) in waits:
            h.wait_ge(wsem, wval)
        fn(h).then_inc(inc[0], inc[1])
        for b in reads:
            k = id(tok[0])
            if k not in b.r or b.r[k][1] < tok[1]:
                b.r[k] = tok
        for b in writes:
            b.w = tok
            b.r = {}
        return tok

    def all_tokens(self):
        toks = []
        for e in self.q.values():
            if e.cnt > 0:
                toks.append((e.sem, e.cnt, e.name))
            ns = len(e.dsems)
            for slot in range(min(ns, e.dn)):
                n_uses = (e.dn - slot + ns - 1) // ns
                toks.append((e.dsems[slot], 16 * n_uses, "dma"))
        return toks

    def barrier(self, engines=("pe", "act", "dve", "pool", "sp")):
        toks = self.all_tokens()
        for name in engines:
            e = self.q[name]
            waits = []
            for (sem, val, src) in toks:
                key = id(sem)
                if e.waited.get(key, (None, 0))[1] >= val:
                    continue
                e.waited[key] = (sem, val)
                waits.append((sem, val))
            for (wsem, wval) in waits:
                self.h[name].wait_ge(wsem, wval)
        self.epoch += 1
        for name in engines:
            e = self.q[name]
            if e.cnt > 0:
                e.sem = self.nc.alloc_semaphore("prog%d_%s" % (self.epoch, name))
                e.cnt = 0


def build(SEQ, dbg=False, nexp=NEXP):
    NEXP_ = nexp
    OWN = SEQ // NCORES
    NST = SEQ // ST
    NOWN = OWN // ST
    S0 = NST - NOWN
    NKT = SEQ // 128

    nc = bass.Bass("TRN2", target_bir_lowering=False)

    def din(name, shape, dt=F32):
        return nc.dram_tensor(name, list(shape), dt, kind="ExternalInput").ap()

    xseq = din("xseq", [SEQ, D])
    validrow = din("validrow", [128, SEQ])
    kbias_d = din("kbias", [128, NKT])
    mem_d = din("mem", [NMEM, D])
    w_in = din("w_in", [D, 5120])
    w_out = din("w_out", [D, D])
    w_cq = din("w_cq", [D, D])
    w_ckv = din("w_ckv", [D, 2 * D])
    w_co = din("w_co", [D, D])
    w_router = din("w_router", [D, NEXP_])
    w_gu = din("w_gu", [NEXP_, D, 2 * D])
    w_down = din("w_down", [NEXP_, D, D])
    w_rga = din("w_rga", [8, 128, 128])
    w_rgx = din("w_rgx", [8, 128, 128])
    g_mix = din("g_mix", [128, D])
    g_cross = din("g_cross", [128, D])
    g_mem = din("g_mem", [128, D])
    g_ffn = din("g_ffn", [128, D])
    g_final = din("g_final", [128, D])
    convw_d = din("convw", [128, 8, 4])
    recv_d = din("recv", [128, 5, 8])
    lamv_d = din("lamv", [128, 4, 128])
    subg_d = din("subg", [128, 2])
    brt_d = din("brt", [128, NEXP_])
    bgu_d = din("bgu", [128, NEXP_, 32])
    bdown_d = din("bdown", [NEXP_, D])
    ident_d = din("ident", [128, 128], BF)
    identf_d = din("identf", [128, 128])
    dmask_d = din("dmask", [128, 4, ST], BF)

    out_d = nc.dram_tensor("out", [OWN, D], F32, kind="ExternalOutput").ap()

    skind = "ExternalOutput" if dbg else "Internal"

    def dscr(name, shape, dt):
        return nc.dram_tensor(name, list(shape), dt, kind=skind).ap()

    kT_d = dscr("kT_s", [1024, SEQ], BF)
    v_d = dscr("v_s", [SEQ, 1024], BF)
    hown_d = dscr("hown_s", [1024, OWN], F32)
    qT_d = dscr("qT_s", [1024, OWN], BF)
    cat_d = dscr("cat_s", [D, OWN], BF)
    h_d = dscr("h_s", [OWN, D], F32)
    oc_d = dscr("oc_s", [D, OWN], BF)

    k = K(nc)
    B_kT = [[Buf() for _ in range(NST)] for _ in range(8)]
    B_v = [Buf() for _ in range(NKT)]
    B_hown = [[Buf() for _ in range(NOWN)] for _ in range(8)]
    B_qT = [[Buf() for _ in range(NOWN)] for _ in range(8)]
    B_cat = [[Buf() for _ in range(NOWN)] for _ in range(KC)]
    B_h = [Buf() for _ in range(OWN // 128)]
    B_oc = [Buf() for _ in range(NOWN)]
    B_out = Buf()

    stack_outer = ExitStack()
    cur = [stack_outer]

    sbn = [0]

    def sb(name, shape, dt):
        sbn[0] += 1
        return cur[0].enter_context(nc.sbuf_tensor("sb%d_%s" % (sbn[0], name), list(shape), dt))

    ident = sb("ident", [128, 128], BF); b_ident = Buf()
    identf = sb("identf", [128, 128], F32); b_identf = Buf()
    ones_bf = sb("ones_bf", [128, 128], BF); b_ones = Buf()
    k.op("sp", lambda e: e.dma_start(out=ident[:, :], in_=ident_d[:, :]), writes=[b_ident], dma=True)
    k.op("sp", lambda e: e.dma_start(out=identf[:, :], in_=identf_d[:, :]), writes=[b_identf], dma=True)
    k.op("dve", lambda e: e.memset(ones_bf[:, :], 1.0), writes=[b_ones])
    epst = {}
    for ci, cv in enumerate((EPS, DA_EPS, 1.0)):
        tcst = sb("cst%d" % ci, [128, 1], F32)
        k.op("dve", lambda e, tcst=tcst, cv=cv: e.memset(tcst[:, :], cv), writes=[Buf()])
        epst[cv] = tcst[:, :]
    k.barrier()

    PS = [nc.alloc_psum_tensor("ps%d" % i, [128, 512], F32) for i in range(8)]
    PSB = [Buf() for _ in range(8)]

    def psbf(i):
        return PS[i][:, :].bitcast(BF)

    def load_w(dst, dst_buf, src2d, c0, c1, kcn=KC, dcol0=0):
        srcv = src2d.rearrange("(kc p) n -> p kc n", p=128)
        for kc in range(kcn):
            k.op("pool", lambda e, kc=kc: e.dma_start(out=dst[:, kc, dcol0:dcol0 + (c1 - c0)], in_=srcv[:, kc, c0:c1]),
                 writes=[dst_buf], dma=True)

    def rms_tile(xt_ap, b_x, grep, b_g, nb_ap, b_nb, junk, b_junk, ss, b_ss, rstd, b_rstd, eps=EPS):
        k.op("act", lambda e: e.activation(out=junk, in_=xt_ap, func=AF.Square, accum_out=ss),
             reads=[b_x], writes=[b_junk, b_ss])
        k.op("act", lambda e: e.activation(out=rstd, in_=ss, func=AF.Sqrt, scale=1.0 / D, bias=epst[eps]),
             reads=[b_ss], writes=[b_rstd])
        k.op("dve", lambda e: e.reciprocal(out=rstd, in_=rstd),
             reads=[b_rstd], writes=[b_rstd])
        k.op("dve", lambda e: e.scalar_tensor_tensor(out=nb_ap, in0=xt_ap, scalar=rstd, in1=grep, op0=ALU.mult, op1=ALU.mult),
             reads=[b_x, b_rstd, b_g], writes=[b_nb])

    def transpose_tile(nb_t, b_nb, nT, b_nT, col0, banks=(0, 1)):
        for half in range(2):
            bank = banks[half]
            pv = psbf(bank)
            for i in range(8):
                kc = half * 8 + i
                k.op("pe", lambda e, kc=kc, i=i, pv=pv: e.transpose(out=pv[:, i * 128:(i + 1) * 128], in_=nb_t[:, kc * 128:(kc + 1) * 128], identity=ident[:, :]),
                     reads=[b_nb, b_ident], writes=[PSB[bank]])
            k.op("act", lambda e, half=half, pv=pv: e.activation(out=nT[:, half * 8:(half + 1) * 8, col0:col0 + 128],
                                                                 in_=pv.rearrange("p (a b) -> p a b", a=8), func=AF.Copy),
                 reads=[PSB[bank]], writes=[b_nT])

    ph1 = ExitStack()
    cur[0] = ph1
    W1 = sb("W1", [128, KC, 3072], BF); b_W1 = Buf()
    load_w(W1, b_W1, w_in, 1024, 4096)
    gmix = sb("gmix", [128, D], F32); b_gmix = Buf()
    k.op("sp", lambda e: e.dma_start(out=gmix[:, :], in_=g_mix[:, :]), writes=[b_gmix], dma=True)
    wa = sb("wa", [128, 8, 128], BF); b_wa = Buf()
    wx = sb("wx", [128, 8, 128], BF); b_wx = Buf()
    k.op("pool", lambda e: e.dma_start(out=wa[:, :, :], in_=w_rga.rearrange("n c d -> c n d")), writes=[b_wa], dma=True)
    k.op("pool", lambda e: e.dma_start(out=wx[:, :, :], in_=w_rgx.rearrange("n c d -> c n d")), writes=[b_wx], dma=True)
    convw = sb("convw", [128, 8, 4], F32); b_convw = Buf()
    recv = sb("recv", [128, 5, 8], F32); b_recv = Buf()
    k.op("sp", lambda e: e.dma_start(out=convw[:, :, :], in_=convw_d[:, :, :]), writes=[b_convw], dma=True)
    k.op("sp", lambda e: e.dma_start(out=recv[:, :, :], in_=recv_d[:, :, :]), writes=[b_recv], dma=True)

    uu = sb("uu", [128, 8], F32); b_uu = Buf()
    pp = sb("pp", [128, 8], F32); b_pp = Buf()
    cneg = sb("cneg", [128, 8], F32); b_cneg = Buf()
    cneg2 = sb("cneg2", [128, 8], F32); b_cneg2 = Buf()
    k.op("act", lambda e: e.activation(out=uu[:, :], in_=recv[:, 3, :], func=AF.Exp, scale=-1.0), reads=[b_recv], writes=[b_uu])
    k.op("dve", lambda e: e.tensor_scalar(out=pp[:, :], in0=uu[:, :], scalar1=-1.0 / 8, scalar2=1.0 / 7, op0=ALU.mult, op1=ALU.add), reads=[b_uu], writes=[b_pp])
    for c in (6, 5, 4, 3, 2, 1):
        k.op("dve", lambda e: e.tensor_tensor(out=pp[:, :], in0=pp[:, :], in1=uu[:, :], op=ALU.mult), reads=[b_pp, b_uu], writes=[b_pp])
        k.op("dve", lambda e, c=c: e.tensor_scalar(out=pp[:, :], in0=pp[:, :], scalar1=-1.0, scalar2=1.0 / c, op0=ALU.mult, op1=ALU.add), reads=[b_pp], writes=[b_pp])
    k.op("dve", lambda e: e.tensor_tensor(out=pp[:, :], in0=pp[:, :], in1=uu[:, :], op=ALU.mult), reads=[b_pp, b_uu], writes=[b_pp])
    k.op("dve", lambda e: e.tensor_scalar(out=cneg[:, :], in0=pp[:, :], scalar1=-8.0, scalar2=None, op0=ALU.mult), reads=[b_pp], writes=[b_cneg])
    k.op("dve", lambda e: e.tensor_scalar(out=cneg2[:, :], in0=pp[:, :], scalar1=-16.0, scalar2=None, op0=ALU.mult), reads=[b_pp], writes=[b_cneg2])

    xt = [sb("xt%d" % i, [128, D], F32) for i in range(2)]; b_xt = [Buf(), Buf()]
    nb = [sb("nb%d" % i, [128, D], BF) for i in range(2)]; b_nb = [Buf(), Buf()]
    ss = [sb("ss%d" % i, [128, 1], F32) for i in range(2)]; b_ss = [Buf(), Buf()]
    rstd = [sb("rstd%d" % i, [128, 1], F32) for i in range(2)]; b_rstd = [Buf(), Buf()]
    nT0 = sb("nT0", [128, KC, ST], BF); nT = [nT0, nT0]; b_nT0 = Buf(); b_nT = [b_nT0, b_nT0]

    tile_ctr = [0]

    def norm_transpose_supertile(s, grep, b_g, nTbuf, b_nTbuf, src_rows, b_src=None, eps=EPS):
        for tt in range(4):
            i = tile_ctr[0] % 2
            tile_ctr[0] += 1
            rd = [b_src[tt]] if b_src is not None else []
            k.op("sp", lambda e, tt=tt, i=i: e.dma_start(out=xt[i][:, :], in_=src_rows(tt)), reads=rd, writes=[b_xt[i]], dma=True)
            rms_tile(xt[i][:, :], b_xt[i], grep[:, :], b_g, nb[i][:, :], b_nb[i], nb[i][:, :], b_nb[i], ss[i][:, :], b_ss[i], rstd[i][:, :], b_rstd[i], eps)
            transpose_tile(nb[i], b_nb[i], nTbuf, b_nTbuf, tt * 128)

    xrp = sb("xrp", [128, 8, 3 + ST], F32); b_xrp = [Buf() for _ in range(8)]
    k.op("pool", lambda e: e.memset(xrp[:, :, :], 0.0), writes=b_xrp)
    hlast = sb("hlast", [128, 8], F32); b_hlast = [Buf() for _ in range(8)]
    k.op("pool", lambda e: e.memset(hlast[:, :], 0.0), writes=b_hlast)
    NR = 1
    xc = [sb("xc%d" % i, [128, ST], F32) for i in range(NR)]; b_xc = [Buf() for _ in range(NR)]
    xcb = [sb("xcb%d" % i, [128, ST], BF) for i in range(NR)]; b_xcb = [Buf() for _ in range(NR)]
    rr = [sb("rr%d" % i, [128, ST], F32) for i in range(NR)]; b_rr = [Buf() for _ in range(NR)]
    ii = [sb("ii%d" % i, [128, ST], F32) for i in range(NR)]; b_ii = [Buf() for _ in range(NR)]
    aa = [sb("aa%d" % i, [128, ST], F32) for i in range(NR)]; b_aa = [Buf() for _ in range(NR)]
    a2 = [sb("a2%d" % i, [128, ST], F32) for i in range(NR)]; b_a2 = [Buf() for _ in range(NR)]
    bb = [sb("bb%d" % i, [128, ST], F32) for i in range(NR)]; b_bb = [Buf() for _ in range(NR)]
    hh = [sb("hh%d" % i, [128, ST], F32) for i in range(NR)]; b_hh = [Buf() for _ in range(NR)]
    vrow = [sb("vrow%d" % i, [128, ST], F32) for i in range(2)]; b_vrow = [Buf(), Buf()]
    kst = [sb("kst%d" % i, [128, ST], BF) for i in range(2)]; b_kst = [Buf(), Buf()]
    vst = [sb("vst%d" % i, [128, 1024], BF) for i in range(2)]; b_vst = [Buf(), Buf()]

    rec_ctr = [0]
    for s in range(NST):
        nTb, b_nTb = nT[s % 2], b_nT[s % 2]
        norm_transpose_supertile(s, gmix, b_gmix, nTb, b_nTb, lambda tt, s=s: xseq[s * ST + tt * 128: s * ST + (tt + 1) * 128, :])
        own = s >= S0
        if not own:
            vi = s % 2
            k.op("sp", lambda e, s=s, vi=vi: e.dma_start(out=vrow[vi][:, :], in_=validrow[:, s * ST:(s + 1) * ST]), writes=[b_vrow[vi]], dma=True)
        for m in range(8):
            bank = 2 + (m % 2)
            for kc in range(KC):
                k.op("pe", lambda e, kc=kc, m=m, bank=bank: e.matmul(PS[bank][:, :], lhsT=W1[:, kc, m * 128:(m + 1) * 128], rhs=nTb[:, kc, :], start=(kc == 0), stop=(kc == KC - 1)),
                     reads=[b_W1, b_nTb], writes=[PSB[bank]])
            ki = m % 2
            k.op("act", lambda e, ki=ki, bank=bank: e.activation(out=kst[ki][:, :], in_=PS[bank][:, :], func=AF.Copy), reads=[PSB[bank]], writes=[b_kst[ki]])
            k.op("sp", lambda e, ki=ki, m=m, s=s: e.dma_start(out=kT_d[m * 128:(m + 1) * 128, s * ST:(s + 1) * ST], in_=kst[ki][:, :]),
                 reads=[b_kst[ki]], writes=[B_kT[m][s]], dma=True)
        for tt in range(4):
            vi = tt % 2
            for half in range(2):
                bank = 2 + half
                for kc in range(KC):
                    k.op("pe", lambda e, kc=kc, tt=tt, half=half, bank=bank: e.matmul(PS[bank][:, :], lhsT=nTb[:, kc, tt * 128:(tt + 1) * 128], rhs=W1[:, kc, 1024 + half * 512:1024 + (half + 1) * 512], start=(kc == 0), stop=(kc == KC - 1)),
                         reads=[b_W1, b_nTb], writes=[PSB[bank]])
                k.op("act", lambda e, vi=vi, half=half, bank=bank: e.activation(out=vst[vi][:, half * 512:(half + 1) * 512], in_=PS[bank][:, :], func=AF.Copy), reads=[PSB[bank]], writes=[b_vst[vi]])
            kt = s * 4 + tt
            k.op("sp", lambda e, vi=vi, kt=kt: e.dma_start(out=v_d[kt * 128:(kt + 1) * 128, :], in_=vst[vi][:, :]), reads=[b_vst[vi]], writes=[B_v[kt]], dma=True)
        for ct in range(8):
            ri = rec_ctr[0] % NR
            rec_ctr[0] += 1
            bank = 4 + (ct % 2)
            for kc in range(KC):
                k.op("pe", lambda e, kc=kc, ct=ct, bank=bank: e.matmul(PS[bank][:, :], lhsT=W1[:, kc, 2048 + ct * 128:2048 + (ct + 1) * 128], rhs=nTb[:, kc, :], start=(kc == 0), stop=(kc == KC - 1)),
                     reads=[b_W1, b_nTb], writes=[PSB[bank]])
            k.op("act", lambda e, ct=ct, bank=bank: e.activation(out=xrp[:, ct, 3:3 + ST], in_=PS[bank][:, :], func=AF.Copy), reads=[PSB[bank]], writes=[b_xrp[ct]])
            k.op("pool", lambda e, ct=ct, ri=ri: e.tensor_scalar(out=xc[ri][:, :], in0=xrp[:, ct, 0:ST], scalar1=convw[:, ct, 0:1], scalar2=recv[:, 0, ct:ct + 1], op0=ALU.mult, op1=ALU.add),
                 reads=[b_xrp[ct], b_convw, b_recv], writes=[b_xc[ri]])
            for j in (1, 2, 3):
                k.op("dve", lambda e, ct=ct, ri=ri, j=j: e.scalar_tensor_tensor(out=xc[ri][:, :], in0=xrp[:, ct, j:j + ST], scalar=convw[:, ct, j:j + 1], in1=xc[ri][:, :], op0=ALU.mult, op1=ALU.add),
                     reads=[b_xrp[ct], b_convw, b_xc[ri]], writes=[b_xc[ri]])
            k.op("pool", lambda e, ct=ct: e.tensor_copy(out=xrp[:, ct, 0:3], in_=xrp[:, ct, ST:ST + 3]), reads=[b_xrp[ct]], writes=[b_xrp[ct]])
            k.op("pool", lambda e, ri=ri: e.tensor_copy(out=xcb[ri][:, :], in_=xc[ri][:, :]), reads=[b_xc[ri]], writes=[b_xcb[ri]])
            k.op("pe", lambda e, ct=ct, ri=ri: e.matmul(PS[6][:, :], lhsT=wa[:, ct, :], rhs=xcb[ri][:, :], start=True, stop=True), reads=[b_wa, b_xcb[ri]], writes=[PSB[6]])
            k.op("pe", lambda e, ct=ct, ri=ri: e.matmul(PS[7][:, :], lhsT=wx[:, ct, :], rhs=xcb[ri][:, :], start=True, stop=True), reads=[b_wx, b_xcb[ri]], writes=[PSB[7]])
            k.op("act", lambda e, ct=ct, ri=ri: e.activation(out=rr[ri][:, :], in_=PS[6][:, :], func=AF.Sigmoid, bias=recv[:, 1, ct:ct + 1]), reads=[PSB[6], b_recv], writes=[b_rr[ri]])
            k.op("act", lambda e, ct=ct, ri=ri: e.activation(out=ii[ri][:, :], in_=PS[7][:, :], func=AF.Sigmoid, bias=recv[:, 2, ct:ct + 1]), reads=[PSB[7], b_recv], writes=[b_ii[ri]])
            k.op("act", lambda e, ct=ct, ri=ri: e.activation(out=aa[ri][:, :], in_=rr[ri][:, :], func=AF.Exp, scale=cneg[:, ct:ct + 1]), reads=[b_rr[ri], b_cneg], writes=[b_aa[ri]])
            k.op("act", lambda e, ct=ct, ri=ri: e.activation(out=a2[ri][:, :], in_=rr[ri][:, :], func=AF.Exp, scale=cneg2[:, ct:ct + 1]), reads=[b_rr[ri], b_cneg2], writes=[b_a2[ri]])
            k.op("act", lambda e, ri=ri: e.activation(out=a2[ri][:, :], in_=a2[ri][:, :], func=AF.Sqrt, scale=-1.0, bias=epst[1.0]), reads=[b_a2[ri]], writes=[b_a2[ri]])
            k.op("dve", lambda e, ri=ri: e.tensor_tensor(out=bb[ri][:, :], in0=ii[ri][:, :], in1=xc[ri][:, :], op=ALU.mult), reads=[b_ii[ri], b_xc[ri]], writes=[b_bb[ri]])
            k.op("dve", lambda e, ri=ri: e.tensor_tensor(out=bb[ri][:, :], in0=bb[ri][:, :], in1=a2[ri][:, :], op=ALU.mult), reads=[b_bb[ri], b_a2[ri]], writes=[b_bb[ri]])
            if not own:
                k.op("dve", lambda e, ri=ri, vi=s % 2: e.tensor_tensor(out=bb[ri][:, :], in0=bb[ri][:, :], in1=vrow[vi][:, :], op=ALU.mult), reads=[b_bb[ri], b_vrow[s % 2]], writes=[b_bb[ri]])
            k.op("dve", lambda e, ri=ri, ct=ct: e.tensor_tensor_scan(out=hh[ri][:, :], data0=aa[ri][:, :], data1=bb[ri][:, :], initial=hlast[:, ct:ct + 1], op0=ALU.mult, op1=ALU.add),
                 reads=[b_aa[ri], b_bb[ri], b_hlast[ct]], writes=[b_hh[ri]])
            k.op("dve", lambda e, ri=ri, ct=ct: e.tensor_copy(out=hlast[:, ct:ct + 1], in_=hh[ri][:, ST - 1:ST]), reads=[b_hh[ri]], writes=[b_hlast[ct]])
            if own:
                j = s - S0
                k.op("sp", lambda e, ri=ri, ct=ct, j=j: e.dma_start(out=hown_d[ct * 128:(ct + 1) * 128, j * ST:(j + 1) * ST], in_=hh[ri][:, :]), reads=[b_hh[ri]], writes=[B_hown[ct][j]], dma=True)

    k.barrier()

    W2 = W1
    b_W2 = Buf()
    load_w(W2, b_W2, w_in, 0, 1024, dcol0=0)
    load_w(W2, b_W2, w_in, 4096, 5120, dcol0=1024)
    ybuf = W1[:, 0:8, 2048:3072].bitcast(F32); b_ybuf = [b_W2 for _ in range(8)]
    ysq = W1[:, 8:16, 2048:2048 + ST]; b_ysq = [b_W2 for _ in range(8)]
    rs = a2[0]; b_rs = b_a2[0]
    ynb = [xcb[0], xcb[0]]; b_ynb = [b_xcb[0], b_xcb[0]]
    for j in range(NOWN):
        s = S0 + j
        nTb, b_nTb = nT[s % 2], b_nT[s % 2]
        norm_transpose_supertile(s, gmix, b_gmix, nTb, b_nTb, lambda tt, s=s: xseq[s * ST + tt * 128: s * ST + (tt + 1) * 128, :])
        for m in range(8):
            bank = 2 + (m % 2)
            for kc in range(KC):
                k.op("pe", lambda e, kc=kc, m=m, bank=bank: e.matmul(PS[bank][:, :], lhsT=W2[:, kc, m * 128:(m + 1) * 128], rhs=nTb[:, kc, :], start=(kc == 0), stop=(kc == KC - 1)),
                     reads=[b_W2, b_nTb], writes=[PSB[bank]])
            ki = m % 2
            k.op("act", lambda e, ki=ki, bank=bank: e.activation(out=kst[ki][:, :], in_=PS[bank][:, :], func=AF.Copy), reads=[PSB[bank]], writes=[b_kst[ki]])
            k.op("sp", lambda e, ki=ki, m=m, j=j: e.dma_start(out=qT_d[m * 128:(m + 1) * 128, j * ST:(j + 1) * ST], in_=kst[ki][:, :]),
                 reads=[b_kst[ki]], writes=[B_qT[m][j]], dma=True)
        for ct in range(8):
            ri = ct % NR
            bank = 4 + (ct % 2)
            for kc in range(KC):
                k.op("pe", lambda e, kc=kc, ct=ct, bank=bank: e.matmul(PS[bank][:, :], lhsT=W2[:, kc, 1024 + ct * 128:1024 + (ct + 1) * 128], rhs=nTb[:, kc, :], start=(kc == 0), stop=(kc == KC - 1)),
                     reads=[b_W2, b_nTb], writes=[PSB[bank]])
            k.op("act", lambda e, ri=ri, bank=bank: e.activation(out=xc[ri][:, :], in_=PS[bank][:, :], func=AF.Copy), reads=[PSB[bank]], writes=[b_xc[ri]])
            k.op("dve", lambda e, ri=ri: e.tensor_tensor(out=rr[ri][:, :], in0=xc[ri][:, :], in1=xc[ri][:, :], op=ALU.mult), reads=[b_xc[ri]], writes=[b_rr[ri]])
            k.op("dve", lambda e, ri=ri: e.tensor_scalar(out=rr[ri][:, :], in0=rr[ri][:, :], scalar1=0.044715, scalar2=1.0, op0=ALU.mult, op1=ALU.add), reads=[b_rr[ri]], writes=[b_rr[ri]])
            k.op("dve", lambda e, ri=ri: e.tensor_tensor(out=rr[ri][:, :], in0=rr[ri][:, :], in1=xc[ri][:, :], op=ALU.mult), reads=[b_rr[ri], b_xc[ri]], writes=[b_rr[ri]])
            k.op("act", lambda e, ri=ri: e.activation(out=ii[ri][:, :], in_=rr[ri][:, :], func=AF.Sigmoid, scale=1.5957691216057308), reads=[b_rr[ri]], writes=[b_ii[ri]])
            k.op("sp", lambda e, ri=ri, ct=ct, j=j: e.dma_start(out=hh[ri][:, :], in_=hown_d[ct * 128:(ct + 1) * 128, j * ST:(j + 1) * ST]), reads=[B_hown[ct][j]], writes=[b_hh[ri]], dma=True)
            k.op("dve", lambda e, ri=ri: e.tensor_tensor(out=bb[ri][:, :], in0=xc[ri][:, :], in1=ii[ri][:, :], op=ALU.mult), reads=[b_xc[ri], b_ii[ri]], writes=[b_bb[ri]])
            k.op("dve", lambda e, ri=ri, ct=ct: e.tensor_tensor(out=ybuf[:, ct, :], in0=bb[ri][:, :], in1=hh[ri][:, :], op=ALU.mult), reads=[b_bb[ri], b_hh[ri]], writes=[b_ybuf[ct]])
            k.op("act", lambda e, ct=ct: e.activation(out=ysq[:, ct, :], in_=ybuf[:, ct, :], func=AF.Square), reads=[b_ybuf[ct]], writes=[b_ysq[ct]])
        for ct in range(8):
            k.op("pe", lambda e, ct=ct: e.matmul(PS[6][:, :], lhsT=ones_bf[:, :], rhs=ysq[:, ct, :], start=(ct == 0), stop=(ct == 7)), reads=[b_ones, b_ysq[ct]], writes=[PSB[6]])
        k.op("act", lambda e: e.activation(out=rs[:, :], in_=PS[6][:, :], func=AF.Sqrt, scale=1.0 / 1024, bias=epst[EPS]), reads=[PSB[6]], writes=[b_rs])
        k.op("dve", lambda e: e.reciprocal(out=rs[:, :], in_=rs[:, :]), reads=[b_rs], writes=[b_rs])
        for ct in range(8):
            yi = ct % 2
            k.op("dve", lambda e, ct=ct, yi=yi: e.scalar_tensor_tensor(out=ynb[yi][:, :], in0=ybuf[:, ct, :], scalar=recv[:, 4, ct:ct + 1], in1=rs[:, :], op0=ALU.mult, op1=ALU.mult),
                 reads=[b_ybuf[ct], b_recv, b_rs], writes=[b_ynb[yi]])
            k.op("sp", lambda e, ct=ct, yi=yi, j=j: e.dma_start(out=cat_d[1024 + ct * 128:1024 + (ct + 1) * 128, j * ST:(j + 1) * ST], in_=ynb[yi][:, :]), reads=[b_ynb[yi]], writes=[B_cat[8 + ct][j]], dma=True)

    k.barrier()
    ph1.close()

    ph2 = ExitStack()
    cur[0] = ph2
    kT2 = sb("kT2", [128, 2, SEQ], BF); b_kT2 = [Buf(), Buf()]
    vv = sb("vv", [128, NKT, 256], BF); b_vv = Buf()
    qh = sb("qh", [128, 2, OWN], BF); b_qh = Buf()
    kbias = sb("kbias", [128, NKT], F32); b_kbias = Buf()
    dmask = sb("dmask", [128, 4, ST], BF); b_dmask = Buf()
    lamv = sb("lamv", [128, 4, 128], F32); b_lamv = Buf()
    subg = sb("subg", [128, 2], F32); b_subg = Buf()
    lam_t = sb("lam_t", [128, 4], F32); b_lam = Buf()
    prod = sb("prod", [128, 128], F32); b_prod = Buf()
    pT = [sb("pT%d" % i, [128, ST], BF) for i in range(3)]; b_pT = [Buf() for _ in range(3)]
    rinv = sb("rinv", [128, ST], F32); b_rinv = Buf()
    osb = sb("osb", [128, 2, 2, ST], F32); b_osb = [Buf(), Buf()]
    od = sb("od", [128, 2, ST], F32); b_od = Buf()
    osq = sb("osq", [128, 2, ST], BF); b_osq = Buf()
    rs2 = sb("rs2", [128, ST], F32); b_rs2 = Buf()
    onb = [sb("onb%d" % i, [128, ST], BF) for i in range(2)]; b_onb = [Buf(), Buf()]
    k.op("sp", lambda e: e.dma_start(out=kbias[:, :], in_=kbias_d[:, :]), writes=[b_kbias], dma=True)
    k.op("sp", lambda e: e.dma_start(out=dmask[:, :, :], in_=dmask_d[:, :, :]), writes=[b_dmask], dma=True)
    k.op("sp", lambda e: e.dma_start(out=lamv[:, :, :], in_=lamv_d[:, :, :]), writes=[b_lamv], dma=True)
    k.op("sp", lambda e: e.dma_start(out=subg[:, :], in_=subg_d[:, :]), writes=[b_subg], dma=True)
    for t in range(2):
        k.op("dve", lambda e, t=t: e.tensor_tensor(out=prod[:, :], in0=lamv[:, 2 * t, :], in1=lamv[:, 2 * t + 1, :], op=ALU.mult), reads=[b_lamv], writes=[b_prod])
        k.op("dve", lambda e, t=t: e.reduce_sum(out=lam_t[:, t:t + 1], in_=prod[:, :], axis=AX.X), reads=[b_prod], writes=[b_lam])
    k.op("act", lambda e: e.activation(out=lam_t[:, 0:2], in_=lam_t[:, 0:2], func=AF.Exp), reads=[b_lam], writes=[b_lam])
    k.op("dve", lambda e: e.tensor_tensor(out=lam_t[:, 2:3], in0=lam_t[:, 1:2], in1=lam_t[:, 0:1], op=ALU.subtract), reads=[b_lam], writes=[b_lam])
    k.op("dve", lambda e: e.tensor_scalar(out=lam_t[:, 2:3], in0=lam_t[:, 2:3], scalar1=-LAMBDA_INIT, scalar2=None, op0=ALU.add), reads=[b_lam], writes=[b_lam])
    k.op("dve", lambda e: e.tensor_scalar(out=subg[:, :], in0=subg[:, :], scalar1=1.0 - LAMBDA_INIT, scalar2=None, op0=ALU.mult), reads=[b_subg], writes=[b_subg])
    SCALE = 128.0 ** -0.5
    pctr = 0
    for hd in range(4):
        for m in range(2):
            gm = hd * 2 + m
            for s in range(NST):
                k.op("sp", lambda e, m=m, gm=gm, s=s: e.dma_start(out=kT2[:, m, s * ST:(s + 1) * ST], in_=kT_d[gm * 128:(gm + 1) * 128, s * ST:(s + 1) * ST]),
                     reads=[B_kT[gm][s]], writes=[b_kT2[m]], dma=True)
            k.op("sp", lambda e, m=m, gm=gm: e.dma_start(out=qh[:, m, :], in_=qT_d[gm * 128:(gm + 1) * 128, :]), reads=B_qT[gm], writes=[b_qh], dma=True)
        for s in range(NST):
            k.op("sp", lambda e, s=s, hd=hd: e.dma_start(out=vv[:, s * 4:(s + 1) * 4, :], in_=v_d[s * ST:(s + 1) * ST, hd * 256:(hd + 1) * 256].rearrange("(t p) c -> p t c", p=128)),
                 reads=B_v[s * 4:(s + 1) * 4], writes=[b_vv], dma=True)
        for j in range(NOWN):
            nkb = (S0 + j + 1) * 4
            for m in range(2):
                for kb in range(nkb):
                    sbk = kb % 2
                    pi = pctr % 3
                    pctr += 1
                    k.op("pe", lambda e, m=m, kb=kb, j=j, sbk=sbk: e.matmul(PS[sbk][:, :], lhsT=kT2[:, m, kb * 128:(kb + 1) * 128], rhs=qh[:, m, j * ST:(j + 1) * ST], start=True, stop=True),
                         reads=[b_kT2[m], b_qh], writes=[PSB[sbk]])
                    k.op("act", lambda e, kb=kb, sbk=sbk, pi=pi: e.activation(out=pT[pi][:, :], in_=PS[sbk][:, :], func=AF.Exp, scale=SCALE, bias=kbias[:, kb:kb + 1]),
                         reads=[PSB[sbk], b_kbias], writes=[b_pT[pi]])
                    kr = kb - (nkb - 4)
                    if kr >= 0:
                        k.op("dve", lambda e, pi=pi, kr=kr: e.tensor_tensor(out=pT[pi][:, :], in0=pT[pi][:, :], in1=dmask[:, kr, :], op=ALU.mult), reads=[b_pT[pi], b_dmask], writes=[b_pT[pi]])
                    for dvc in range(2):
                        k.op("pe", lambda e, kb=kb, dvc=dvc, pi=pi, nkb=nkb: e.matmul(PS[2 + dvc][:, :], lhsT=vv[:, kb, dvc * 128:(dvc + 1) * 128], rhs=pT[pi][:, :], start=(kb == 0), stop=(kb == nkb - 1)),
                             reads=[b_vv, b_pT[pi]], writes=[PSB[2 + dvc]])
                    k.op("pe", lambda e, kb=kb, pi=pi, nkb=nkb: e.matmul(PS[4][:, :], lhsT=ones_bf[:, :], rhs=pT[pi][:, :], start=(kb == 0), stop=(kb == nkb - 1)),
                         reads=[b_ones, b_pT[pi]], writes=[PSB[4]])
                k.op("dve", lambda e: e.reciprocal(out=rinv[:, :], in_=PS[4][:, :]), reads=[PSB[4]], writes=[b_rinv])
                for dvc in range(2):
                    k.op("dve", lambda e, m=m, dvc=dvc: e.tensor_tensor(out=osb[:, m, dvc, :], in0=PS[2 + dvc][:, :], in1=rinv[:, :], op=ALU.mult), reads=[PSB[2 + dvc], b_rinv], writes=[b_osb[m]])
            k.op("dve", lambda e: e.scalar_tensor_tensor(out=od[:, :, :], in0=osb[:, 1, :, :], scalar=lam_t[:, 2:3], in1=osb[:, 0, :, :], op0=ALU.mult, op1=ALU.add),
                 reads=[b_osb[0], b_osb[1], b_lam], writes=[b_od])
            k.op("act", lambda e: e.activation(out=osq[:, :, :], in_=od[:, :, :], func=AF.Square), reads=[b_od], writes=[b_osq])
            for dvc in range(2):
                k.op("pe", lambda e, dvc=dvc: e.matmul(PS[5][:, :], lhsT=ones_bf[:, :], rhs=osq[:, dvc, :], start=(dvc == 0), stop=(dvc == 1)), reads=[b_ones, b_osq], writes=[PSB[5]])
            k.op("act", lambda e: e.activation(out=rs2[:, :], in_=PS[5][:, :], func=AF.Sqrt, scale=1.0 / 256, bias=epst[DA_EPS]), reads=[PSB[5]], writes=[b_rs2])
            k.op("dve", lambda e: e.reciprocal(out=rs2[:, :], in_=rs2[:, :]), reads=[b_rs2], writes=[b_rs2])
            for dvc in range(2):
                k.op("dve", lambda e, dvc=dvc: e.scalar_tensor_tensor(out=onb[dvc][:, :], in0=od[:, dvc, :], scalar=subg[:, dvc:dvc + 1], in1=rs2[:, :], op0=ALU.mult, op1=ALU.mult),
                     reads=[b_od, b_subg, b_rs2], writes=[b_onb[dvc]])
                kc = hd * 2 + dvc
                k.op("sp", lambda e, dvc=dvc, kc=kc, j=j: e.dma_start(out=cat_d[kc * 128:(kc + 1) * 128, j * ST:(j + 1) * ST], in_=onb[dvc][:, :]), reads=[b_onb[dvc]], writes=[B_cat[kc][j]], dma=True)
    k.barrier()
    ph2.close()

    ph3 = ExitStack()
    cur[0] = ph3
    Wb = sb("Wb", [128, KC, D], BF); b_Wb = Buf()
    gv = sb("gv", [128, D], F32); b_gv = Buf()
    xt = [sb("xt%d" % i, [128, D], F32) for i in range(2)]; b_xt = [Buf(), Buf()]
    nb = [sb("nb%d" % i, [128, D], BF) for i in range(2)]; b_nb = [Buf(), Buf()]
    ss = [sb("ss%d" % i, [128, 1], F32) for i in range(2)]; b_ss = [Buf(), Buf()]
    rstd = [sb("rstd%d" % i, [128, 1], F32) for i in range(2)]; b_rstd = [Buf(), Buf()]
    nT0 = sb("nT0", [128, KC, ST], BF); b_nT0 = Buf()
    catT = sb("catT", [128, KC, ST], BF); b_catT = Buf()
    ht = [sb("ht%d" % i, [128, D], F32) for i in range(2)]; b_ht = [Buf(), Buf()]
    memT = sb("memT", [128, KC, NMEM], BF); b_memT = Buf()
    KmT = sb("KmT", [128, KC, NMEM], BF); b_KmT = Buf()
    Vm = sb("Vm", [128, 2, D], BF); b_Vm = Buf()
    pc = [sb("pc%d" % i, [128, ST], BF) for i in range(2)]; b_pc = [Buf(), Buf()]
    rinv = sb("rinv", [128, ST], F32); b_rinv = Buf()

    load_w(Wb, b_Wb, w_out, 0, D)
    hctr = 0
    for j in range(NOWN):
        for kc in range(KC):
            k.op("sp", lambda e, kc=kc, j=j: e.dma_start(out=catT[:, kc, :], in_=cat_d[kc * 128:(kc + 1) * 128, j * ST:(j + 1) * ST]), reads=[B_cat[kc][j]], writes=[b_catT], dma=True)
        for tt in range(4):
            hi = hctr % 2
            hctr += 1
            row0 = (S0 + j) * ST + tt * 128
            k.op("sp", lambda e, hi=hi, row0=row0: e.dma_start(out=xt[hi][:, :], in_=xseq[row0:row0 + 128, :]), writes=[b_xt[hi]], dma=True)
            for n4 in range(4):
                bank = n4 % 2
                for kc in range(KC):
                    k.op("pe", lambda e, kc=kc, tt=tt, n4=n4, bank=bank: e.matmul(PS[bank][:, :], lhsT=catT[:, kc, tt * 128:(tt + 1) * 128], rhs=Wb[:, kc, n4 * 512:(n4 + 1) * 512], start=(kc == 0), stop=(kc == KC - 1)),
                         reads=[b_catT, b_Wb], writes=[PSB[bank]])
                k.op("dve", lambda e, hi=hi, n4=n4, bank=bank: e.tensor_tensor(out=ht[hi][:, n4 * 512:(n4 + 1) * 512], in0=PS[bank][:, :], in1=xt[hi][:, n4 * 512:(n4 + 1) * 512], op=ALU.add),
                     reads=[PSB[bank], b_xt[hi]], writes=[b_ht[hi]])
            ti = j * 4 + tt
            k.op("sp", lambda e, hi=hi, ti=ti: e.dma_start(out=h_d[ti * 128:(ti + 1) * 128, :], in_=ht[hi][:, :]), reads=[b_ht[hi]], writes=[B_h[ti]], dma=True)

    k.op("sp", lambda e: e.dma_start(out=gv[:, :], in_=g_mem[:, :]), writes=[b_gv], dma=True)
    tile_ctr[0] = 0
    for mt in range(2):
        i = mt
        k.op("sp", lambda e, mt=mt, i=i: e.dma_start(out=xt[i][:, :], in_=mem_d[mt * 128:(mt + 1) * 128, :]), writes=[b_xt[i]], dma=True)
        rms_tile(xt[i][:, :], b_xt[i], gv[:, :], b_gv, nb[i][:, :], b_nb[i], nb[i][:, :], b_nb[i], ss[i][:, :], b_ss[i], rstd[i][:, :], b_rstd[i], EPS)
        transpose_tile(nb[i], b_nb[i], memT, b_memT, mt * 128)
    load_w(Wb, b_Wb, w_ckv, 0, D)
    for fc in range(KC):
        bank = 2 + fc % 2
        for kc in range(KC):
            k.op("pe", lambda e, kc=kc, fc=fc, bank=bank: e.matmul(PS[bank][:, 0:NMEM], lhsT=Wb[:, kc, fc * 128:(fc + 1) * 128], rhs=memT[:, kc, :], start=(kc == 0), stop=(kc == KC - 1)),
                 reads=[b_Wb, b_memT], writes=[PSB[bank]])
        k.op("act", lambda e, fc=fc, bank=bank: e.activation(out=KmT[:, fc, :], in_=PS[bank][:, 0:NMEM], func=AF.Copy), reads=[PSB[bank]], writes=[b_KmT])
    load_w(Wb, b_Wb, w_ckv, D, 2 * D)
    for mt in range(2):
        for n4 in range(4):
            bank = 2 + n4 % 2
            for kc in range(KC):
                k.op("pe", lambda e, kc=kc, mt=mt, n4=n4, bank=bank: e.matmul(PS[bank][:, :], lhsT=memT[:, kc, mt * 128:(mt + 1) * 128], rhs=Wb[:, kc, n4 * 512:(n4 + 1) * 512], start=(kc == 0), stop=(kc == KC - 1)),
                     reads=[b_Wb, b_memT], writes=[PSB[bank]])
            k.op("act", lambda e, mt=mt, n4=n4, bank=bank: e.activation(out=Vm[:, mt, n4 * 512:(n4 + 1) * 512], in_=PS[bank][:, :], func=AF.Copy), reads=[PSB[bank]], writes=[b_Vm])
    load_w(Wb, b_Wb, w_cq, 0, D)
    k.op("sp", lambda e: e.dma_start(out=gv[:, :], in_=g_cross[:, :]), writes=[b_gv], dma=True)
    qcT = catT; b_qcT = b_catT
    CS = 512.0 ** -0.5
    for j in range(NOWN):
        norm_transpose_supertile(j, gv, b_gv, nT0, b_nT0, lambda tt, j=j: h_d[(j * 4 + tt) * 128:(j * 4 + tt + 1) * 128, :], b_src=B_h[j * 4:(j + 1) * 4])
        for fc in range(KC):
            bank = 2 + fc % 2
            for kc in range(KC):
                k.op("pe", lambda e, kc=kc, fc=fc, bank=bank: e.matmul(PS[bank][:, :], lhsT=Wb[:, kc, fc * 128:(fc + 1) * 128], rhs=nT0[:, kc, :], start=(kc == 0), stop=(kc == KC - 1)),
                     reads=[b_Wb, b_nT0], writes=[PSB[bank]])
            k.op("act", lambda e, fc=fc, bank=bank: e.activation(out=qcT[:, fc, :], in_=PS[bank][:, :], func=AF.Copy), reads=[PSB[bank]], writes=[b_qcT])
        ocT = nT0; b_ocT = b_nT0
        for hc in range(4):
            for mc in range(2):
                bank = 4 + mc
                for dc in range(4):
                    k.op("pe", lambda e, hc=hc, mc=mc, dc=dc, bank=bank: e.matmul(PS[bank][:, :], lhsT=KmT[:, 4 * hc + dc, mc * 128:(mc + 1) * 128], rhs=qcT[:, 4 * hc + dc, :], start=(dc == 0), stop=(dc == 3)),
                         reads=[b_KmT, b_qcT], writes=[PSB[bank]])
                k.op("act", lambda e, mc=mc, bank=bank: e.activation(out=pc[mc][:, :], in_=PS[bank][:, :], func=AF.Exp, scale=CS), reads=[PSB[bank]], writes=[b_pc[mc]])
            for mc in range(2):
                k.op("pe", lambda e, mc=mc: e.matmul(PS[6][:, :], lhsT=ones_bf[:, :], rhs=pc[mc][:, :], start=(mc == 0), stop=(mc == 1)), reads=[b_ones, b_pc[mc]], writes=[PSB[6]])
            k.op("dve", lambda e: e.reciprocal(out=rinv[:, :], in_=PS[6][:, :]), reads=[PSB[6]], writes=[b_rinv])
            for dvc in range(4):
                bank = dvc % 2
                for mc in range(2):
                    k.op("pe", lambda e, hc=hc, mc=mc, dvc=dvc, bank=bank: e.matmul(PS[bank][:, :], lhsT=Vm[:, mc, hc * 512 + dvc * 128:hc * 512 + (dvc + 1) * 128], rhs=pc[mc][:, :], start=(mc == 0), stop=(mc == 1)),
                         reads=[b_Vm, b_pc[mc]], writes=[PSB[bank]])
                k.op("dve", lambda e, hc=hc, dvc=dvc, bank=bank: e.tensor_tensor(out=ocT[:, 4 * hc + dvc, :], in0=PS[bank][:, :], in1=rinv[:, :], op=ALU.mult), reads=[PSB[bank], b_rinv], writes=[b_ocT])
        for kc in range(KC):
            k.op("sp", lambda e, kc=kc, j=j: e.dma_start(out=oc_d[kc * 128:(kc + 1) * 128, j * ST:(j + 1) * ST], in_=ocT[:, kc, :]), reads=[b_ocT], writes=[B_oc[j]], dma=True)

    load_w(Wb, b_Wb, w_co, 0, D)
    for j in range(NOWN):
        for kc in range(KC):
            k.op("sp", lambda e, kc=kc, j=j: e.dma_start(out=catT[:, kc, :], in_=oc_d[kc * 128:(kc + 1) * 128, j * ST:(j + 1) * ST]), reads=[B_oc[j]], writes=[b_catT], dma=True)
        for tt in range(4):
            hi = hctr % 2
            hctr += 1
            ti = j * 4 + tt
            k.op("sp", lambda e, hi=hi, ti=ti: e.dma_start(out=xt[hi][:, :], in_=h_d[ti * 128:(ti + 1) * 128, :]), reads=[B_h[ti]], writes=[b_xt[hi]], dma=True)
            for n4 in range(4):
                bank = n4 % 2
                for kc in range(KC):
                    k.op("pe", lambda e, kc=kc, tt=tt, n4=n4, bank=bank: e.matmul(PS[bank][:, :], lhsT=catT[:, kc, tt * 128:(tt + 1) * 128], rhs=Wb[:, kc, n4 * 512:(n4 + 1) * 512], start=(kc == 0), stop=(kc == KC - 1)),
                         reads=[b_catT, b_Wb], writes=[PSB[bank]])
                k.op("dve", lambda e, hi=hi, n4=n4, bank=bank: e.tensor_tensor(out=ht[hi][:, n4 * 512:(n4 + 1) * 512], in0=PS[bank][:, :], in1=xt[hi][:, n4 * 512:(n4 + 1) * 512], op=ALU.add),
                     reads=[PSB[bank], b_xt[hi]], writes=[b_ht[hi]])
            k.op("sp", lambda e, hi=hi, ti=ti: e.dma_start(out=h_d[ti * 128:(ti + 1) * 128, :], in_=ht[hi][:, :]), reads=[b_ht[hi]], writes=[B_h[ti]], dma=True)
    k.barrier()
    ph3.close()

    ph4 = ExitStack()
    cur[0] = ph4
    gv = sb("gv", [128, D], F32); b_gv = Buf()
    gfin = sb("gfin", [128, D], F32); b_gfin = Buf()
    k.op("sp", lambda e: e.dma_start(out=gv[:, :], in_=g_ffn[:, :]), writes=[b_gv], dma=True)
    k.op("sp", lambda e: e.dma_start(out=gfin[:, :], in_=g_final[:, :]), writes=[b_gfin], dma=True)
    acc = sb("acc", [128, 4, D], F32); b_acc = [Buf() for _ in range(4)]
    nb = [sb("nb%d" % i, [128, D], BF) for i in range(2)]; b_nb = [Buf(), Buf()]
    ss = [sb("ss%d" % i, [128, 1], F32) for i in range(2)]; b_ss = [Buf(), Buf()]
    rstd = [sb("rstd%d" % i, [128, 1], F32) for i in range(2)]; b_rstd = [Buf(), Buf()]
    n2T = sb("n2T", [128, KC, ST], BF); b_n2T = Buf()
    Wr = sb("Wr", [128, KC, NEXP_], BF); b_Wr = Buf()
    k.op("pool", lambda e: e.dma_start(out=Wr[:, :, :], in_=w_router.rearrange("(kc p) n -> p kc n", p=128)), writes=[b_Wr], dma=True)
    brt = sb("brt", [128, NEXP_], F32); b_brt = Buf()
    k.op("sp", lambda e: e.dma_start(out=brt[:, :], in_=brt_d[:, :]), writes=[b_brt], dma=True)
    bgu = sb("bgu", [128, NEXP_, 32], F32); b_bgu = Buf()
    k.op("sp", lambda e: e.dma_start(out=bgu[:, :, :], in_=bgu_d[:, :, :]), writes=[b_bgu], dma=True)
    bdn = sb("bdn", [NEXP_, D], F32); b_bdn = Buf()
    k.op("sp", lambda e: e.dma_start(out=bdn[:, :], in_=bdown_d[:, :]), writes=[b_bdn], dma=True)
    logit = sb("logit", [128, NEXP_], F32); b_logit = Buf()
    top8 = sb("top8", [128, 8], F32); b_top8 = Buf()
    msk = sb("msk", [128, NEXP_], F32); b_msk = Buf()
    den = sb("den", [128, 2], F32); b_den = Buf()
    gates = sb("gates", [128, 4, NEXP_], F32); b_gates = [Buf() for _ in range(4)]
    gT = sb("gT", [NEXP_, 4, 128], F32); b_gT = Buf()
    wgu = [sb("wgu%d" % i, [128, KC, 2, 256], BF) for i in range(2)]; b_wgu = [Buf(), Buf()]
    wd = [sb("wd%d" % i, [128, KC, 512], BF) for i in range(2)]; b_wd = [Buf(), Buf()]
    actT = sb("actT", [128, KC, ST], BF); b_actT = [Buf() for _ in range(KC)]
    g32 = [sb("g32%d" % i, [128, ST], F32) for i in range(2)]; b_g32 = [Buf(), Buf()]
    sg = [sb("sg%d" % i, [128, ST], F32) for i in range(2)]; b_sg = [Buf(), Buf()]
    u32 = [sb("u32%d" % i, [128, ST], F32) for i in range(2)]; b_u32 = [Buf(), Buf()]
    wctr = 0
    dctr = 0
    ectr = 0
    for j in range(NOWN):
        for tt in range(4):
            ti = j * 4 + tt
            i = tt % 2
            k.op("sp", lambda e, tt=tt, ti=ti: e.dma_start(out=acc[:, tt, :], in_=h_d[ti * 128:(ti + 1) * 128, :]), reads=[B_h[ti]], writes=[b_acc[tt]], dma=True)
            rms_tile(acc[:, tt, :], b_acc[tt], gv[:, :], b_gv, nb[i][:, :], b_nb[i], nb[i][:, :], b_nb[i], ss[i][:, :], b_ss[i], rstd[i][:, :], b_rstd[i], EPS)
            transpose_tile(nb[i], b_nb[i], n2T, b_n2T, tt * 128)
        for tt in range(4):
            for kc in range(KC):
                k.op("pe", lambda e, kc=kc, tt=tt: e.matmul(PS[2][:, 0:NEXP_], lhsT=n2T[:, kc, tt * 128:(tt + 1) * 128], rhs=Wr[:, kc, :], start=(kc == 0), stop=(kc == KC - 1)),
                     reads=[b_n2T, b_Wr], writes=[PSB[2]])
            k.op("dve", lambda e: e.tensor_tensor(out=logit[:, :], in0=PS[2][:, 0:NEXP_], in1=brt[:, :], op=ALU.add), reads=[PSB[2], b_brt], writes=[b_logit])
            k.op("dve", lambda e: e.max(out=top8[:, :], in_=logit[:, :]), reads=[b_logit], writes=[b_top8])
            k.op("dve", lambda e: e.tensor_scalar(out=msk[:, :], in0=logit[:, :], scalar1=top8[:, 3:4], scalar2=None, op0=ALU.is_ge), reads=[b_logit, b_top8], writes=[b_msk])
            k.op("dve", lambda e: e.tensor_scalar(out=den[:, 0:1], in0=top8[:, 0:1], scalar1=-1.0, scalar2=None, op0=ALU.mult), reads=[b_top8], writes=[b_den])
            k.op("act", lambda e: e.activation(out=logit[:, :], in_=logit[:, :], func=AF.Exp, bias=den[:, 0:1]), reads=[b_logit, b_den], writes=[b_logit])
            k.op("dve", lambda e: e.tensor_tensor(out=msk[:, :], in0=msk[:, :], in1=logit[:, :], op=ALU.mult), reads=[b_msk, b_logit], writes=[b_msk])
            k.op("dve", lambda e: e.reduce_sum(out=den[:, 1:2], in_=msk[:, :], axis=AX.X), reads=[b_msk], writes=[b_den])
            k.op("dve", lambda e: e.reciprocal(out=den[:, 1:2], in_=den[:, 1:2]), reads=[b_den], writes=[b_den])
            k.op("dve", lambda e, tt=tt: e.tensor_scalar(out=gates[:, tt, :], in0=msk[:, :], scalar1=den[:, 1:2], scalar2=None, op0=ALU.mult), reads=[b_msk, b_den], writes=[b_gates[tt]])
            k.op("pe", lambda e, tt=tt: e.transpose(out=PS[3][0:NEXP_, 0:128], in_=gates[:, tt, :], identity=identf[:, :]), reads=[b_gates[tt], b_identf], writes=[PSB[3]])
            k.op("act", lambda e, tt=tt: e.activation(out=gT[:, tt, :], in_=PS[3][0:NEXP_, 0:128], func=AF.Copy), reads=[PSB[3]], writes=[b_gT])
        for tt in range(4):
            for n4 in range(4):
                bank = 2 + n4 % 2
                k.op("pe", lambda e, tt=tt, n4=n4, bank=bank: e.matmul(PS[bank][:, :], lhsT=gT[:, tt, :], rhs=bdn[:, n4 * 512:(n4 + 1) * 512], start=True, stop=True), reads=[b_gT, b_bdn], writes=[PSB[bank]])
                k.op("dve", lambda e, tt=tt, n4=n4, bank=bank: e.tensor_tensor(out=acc[:, tt, n4 * 512:(n4 + 1) * 512], in0=PS[bank][:, :], in1=acc[:, tt, n4 * 512:(n4 + 1) * 512], op=ALU.add),
                     reads=[PSB[bank], b_acc[tt]], writes=[b_acc[tt]])
        chunks = []
        for ex in range(NEXP_):
            for fcb in range(8):
                chunks.append((ex, "gu", fcb))
            for dc in range(4):
                chunks.append((ex, "d", dc))
        cbuf = {}

        def issue(ch):
            nonlocal wctr, dctr
            ex, kind, idx = ch
            if kind == "gu":
                wi = wctr % 2
                wctr += 1
                cbuf[ch] = wi
                wguv = w_gu[ex].rearrange("(kc p) n -> p kc n", p=128)
                for gu in range(2):
                    for kh in range(2):
                        k.op("pool", lambda e, wi=wi, gu=gu, idx=idx, kh=kh, wguv=wguv: e.dma_start(out=wgu[wi][:, kh * 8:(kh + 1) * 8, gu, :], in_=wguv[:, kh * 8:(kh + 1) * 8, gu * D + idx * 256:gu * D + (idx + 1) * 256]),
                             writes=[b_wgu[wi]], dma=True)
            else:
                di = dctr % 2
                dctr += 1
                cbuf[ch] = di
                wdv = w_down[ex].rearrange("(fc p) n -> p fc n", p=128)
                for fh in range(2):
                    k.op("pool", lambda e, di=di, idx=idx, fh=fh, wdv=wdv: e.dma_start(out=wd[di][:, fh * 8:(fh + 1) * 8, :], in_=wdv[:, fh * 8:(fh + 1) * 8, idx * 512:(idx + 1) * 512]), writes=[b_wd[di]], dma=True)

        def compute(ch):
            nonlocal ectr
            ex, kind, idx = ch
            if kind == "gu":
                wi = cbuf[ch]
                fcb = idx
                for f2 in range(2):
                    fc = fcb * 2 + f2
                    ei = ectr % 2
                    ectr += 1
                    for gu in range(2):
                        bank = 4 + 2 * gu + ei
                        for kc in range(KC):
                            k.op("pe", lambda e, kc=kc, wi=wi, gu=gu, f2=f2, bank=bank: e.matmul(PS[bank][:, :], lhsT=wgu[wi][:, kc, gu, f2 * 128:(f2 + 1) * 128], rhs=n2T[:, kc, :], start=(kc == 0), stop=(kc == KC - 1)),
                                 reads=[b_wgu[wi], b_n2T], writes=[PSB[bank]])
                    bg, bu = 4 + ei, 6 + ei
                    k.op("dve", lambda e, ei=ei, bg=bg, ex=ex, fc=fc: e.tensor_scalar(out=g32[ei][:, :], in0=PS[bg][:, :], scalar1=bgu[:, ex, fc:fc + 1], scalar2=7.0, op0=ALU.add, op1=ALU.min),
                         reads=[PSB[bg], b_bgu], writes=[b_g32[ei]])
                    k.op("act", lambda e, ei=ei: e.activation(out=sg[ei][:, :], in_=g32[ei][:, :], func=AF.Sigmoid, scale=1.702), reads=[b_g32[ei]], writes=[b_sg[ei]])
                    k.op("dve", lambda e, ei=ei, bu=bu, ex=ex, fc=fc: e.tensor_scalar(out=u32[ei][:, :], in0=PS[bu][:, :], scalar1=bgu[:, ex, 16 + fc:16 + fc + 1], scalar2=7.0, op0=ALU.add, op1=ALU.min),
                         reads=[PSB[bu], b_bgu], writes=[b_u32[ei]])
                    k.op("pool", lambda e, ei=ei: e.tensor_scalar(out=u32[ei][:, :], in0=u32[ei][:, :], scalar1=-7.0, scalar2=1.0, op0=ALU.max, op1=ALU.add), reads=[b_u32[ei]], writes=[b_u32[ei]])
                    k.op("pool", lambda e, ei=ei: e.tensor_tensor(out=g32[ei][:, :], in0=g32[ei][:, :], in1=sg[ei][:, :], op=ALU.mult), reads=[b_g32[ei], b_sg[ei]], writes=[b_g32[ei]])
                    k.op("pool", lambda e, ei=ei, fc=fc: e.tensor_tensor(out=actT[:, fc, :], in0=g32[ei][:, :], in1=u32[ei][:, :], op=ALU.mult), reads=[b_g32[ei], b_u32[ei]], writes=[b_actT[fc]])
            else:
                di = cbuf[ch]
                dc = idx
                for tt in range(4):
                    bank = (tt % 2)
                    for fc in range(KC):
                        k.op("pe", lambda e, fc=fc, tt=tt, di=di, bank=bank: e.matmul(PS[bank][:, :], lhsT=actT[:, fc, tt * 128:(tt + 1) * 128], rhs=wd[di][:, fc, :], start=(fc == 0), stop=(fc == KC - 1)),
                             reads=[b_actT[fc], b_wd[di]], writes=[PSB[bank]])
                    k.op("dve", lambda e, tt=tt, dc=dc, ex=ex, bank=bank: e.scalar_tensor_tensor(out=acc[:, tt, dc * 512:(dc + 1) * 512], in0=PS[bank][:, :], scalar=gates[:, tt, ex:ex + 1], in1=acc[:, tt, dc * 512:(dc + 1) * 512], op0=ALU.mult, op1=ALU.add),
                         reads=[PSB[bank], b_gates[tt], b_acc[tt]], writes=[b_acc[tt]])

        issue(chunks[0])
        for ci in range(len(chunks)):
            if ci + 1 < len(chunks):
                issue(chunks[ci + 1])
            compute(chunks[ci])
        for tt in range(4):
            i = tt % 2
            ti = j * 4 + tt
            k.op("act", lambda e, tt=tt, i=i: e.activation(out=nb[i][:, :], in_=acc[:, tt, :], func=AF.Square, accum_out=ss[i][:, :]), reads=[b_acc[tt]], writes=[b_nb[i], b_ss[i]])
            k.op("act", lambda e, i=i: e.activation(out=rstd[i][:, :], in_=ss[i][:, :], func=AF.Sqrt, scale=1.0 / D, bias=epst[EPS]), reads=[b_ss[i]], writes=[b_rstd[i]])
            k.op("dve", lambda e, i=i: e.reciprocal(out=rstd[i][:, :], in_=rstd[i][:, :]), reads=[b_rstd[i]], writes=[b_rstd[i]])
            k.op("dve", lambda e, tt=tt, i=i: e.scalar_tensor_tensor(out=acc[:, tt, :], in0=acc[:, tt, :], scalar=rstd[i][:, :], in1=gfin[:, :], op0=ALU.mult, op1=ALU.mult),
                 reads=[b_acc[tt], b_rstd[i], b_gfin], writes=[b_acc[tt]])
            k.op("sp", lambda e, tt=tt, ti=ti: e.dma_start(out=out_d[ti * 128:(ti + 1) * 128, :], in_=acc[:, tt, :]), reads=[b_acc[tt]], writes=[B_out], dma=True)
        k.barrier()
    ph4.close()
    stack_outer.close()
    return nc, k, None


def _bf(a):
    return np.ascontiguousarray(a).astype(ml_dtypes.bfloat16)


def prep_inputs(inp, SEQ):
    OWN = SEQ // NCORES
    NKT = SEQ // 128
    f = lambda a: np.ascontiguousarray(np.asarray(a, dtype=np.float32))
    x = f(inp["x"])[0]
    rep = lambda v: np.ascontiguousarray(np.broadcast_to(f(v).reshape(1, -1), (128, f(v).size)))
    shared = {
        "mem": f(inp["mem"])[0],
        "w_in": f(inp["w_in"])[0], "w_out": f(inp["w_out"])[0], "w_cq": f(inp["w_cq"])[0],
        "w_ckv": f(inp["w_ckv"])[0], "w_co": f(inp["w_co"])[0], "w_router": f(inp["w_router"])[0],
        "w_gu": f(inp["w_gate_up"])[0], "w_down": f(inp["w_down"])[0],
        "w_rga": f(inp["w_rg_a"])[0], "w_rgx": f(inp["w_rg_x"])[0],
        "g_mix": rep(inp["norm_mix_g"][0]), "g_cross": rep(inp["norm_cross_g"][0]), "g_mem": rep(inp["norm_mem_g"][0]),
        "g_ffn": rep(inp["norm_ffn_g"][0]), "g_final": rep(inp["norm_final_g"]),
        "convw": np.ascontiguousarray(f(inp["conv_w"])[0].reshape(4, 8, 128).transpose(2, 1, 0)),
        "recv": np.ascontiguousarray(np.stack([f(inp[n])[0].reshape(8, 128) for n in ("conv_b", "b_rg_a", "b_rg_x", "rg_lambda", "rec_norm_g")], 0).transpose(2, 0, 1)),
        "lamv": np.ascontiguousarray(np.broadcast_to(np.stack([f(inp[n])[0] for n in ("lambda_q1", "lambda_k1", "lambda_q2", "lambda_k2")], 0)[None], (128, 4, 128))),
        "subg": np.ascontiguousarray(f(inp["subln_g"])[0].reshape(2, 128).T),
        "brt": rep(inp["b_router"][0]),
        "bgu": np.ascontiguousarray(f(inp["b_gate_up"])[0].reshape(-1, 32, 128).transpose(2, 0, 1)),
        "bdown": f(inp["b_down"])[0],
        "ident": _bf(np.eye(128, dtype=np.float32)),
        "identf": np.eye(128, dtype=np.float32),
    }
    kk = np.arange(128)[:, None, None]
    kr = np.arange(4)[None, :, None]
    qq = np.arange(ST)[None, None, :]
    shared["dmask"] = _bf(((kr * 128 + kk) <= qq).astype(np.float32))
    maps = []
    for c in range(NCORES):
        npad = SEQ - OWN * (c + 1)
        xs = np.zeros((SEQ, D), np.float32)
        xs[npad:] = x[: OWN * (c + 1)]
        valid = (np.arange(SEQ) >= npad).astype(np.float32)
        m = dict(shared)
        m["xseq"] = xs
        m["validrow"] = np.ascontiguousarray(np.broadcast_to(valid[None, :], (128, SEQ)))
        m["kbias"] = np.ascontiguousarray(((valid - 1.0) * 30000.0).reshape(NKT, 128).T)
        maps.append(m)
    return maps


def kernel(**inputs):
    SEQ = 16384
    nc, k, L = build(SEQ)
    maps = prep_inputs(inputs, SEQ)
    res = run_bass_kernel_spmd(nc, maps, core_ids=list(range(NCORES)))
    out = np.concatenate([np.asarray(r["out"]) for r in res.results], 0)[None]
    return np.ascontiguousarray(out.astype(np.float32))
```

```python
import math
from contextlib import ExitStack
import numpy as np
import ml_dtypes
import concourse.bass as bass
import concourse.mybir as mybir
from concourse.bass_utils import run_bass_kernel_spmd

F32 = mybir.dt.float32
BF = mybir.dt.bfloat16
AF = mybir.ActivationFunctionType
ALU = mybir.AluOpType
AX = mybir.AxisListType

D = 2048
KC = 16
ST = 512
NCORES = 8
NEXP = 32
NMEM = 256
EPS = 1e-6
DA_EPS = 1e-5
LAMBDA_INIT = 0.8 - 0.6 * math.exp(0.0)


class Buf:
    __slots__ = ("w", "r")

    def __init__(self):
        self.w = None
        self.r = {}


class EngQ:
    def __init__(self, name, sem, dsems):
        self.name = name
        self.sem = sem
        self.cnt = 0
        self.items = []
        self.dsems = dsems
        self.dn = 0
        self.waited = {}


class K:
    def __init__(self, nc, nds=6):
        self.nc = nc
        self.epoch = 0
        self.q = {}
        self.h = {"pe": nc.tensor, "act": nc.scalar, "dve": nc.vector, "pool": nc.gpsimd, "sp": nc.sync}
        for name in ("pe", "act", "dve", "pool", "sp"):
            sem = nc.alloc_semaphore("prog_" + name)
            dsems = []
            if name in ("sp", "pool", "act"):
                dsems = [nc.alloc_semaphore("dma_%s_%d" % (name, i)) for i in range(nds)]
            self.q[name] = EngQ(name, sem, dsems)

    def _waits(self, e, deps):
        out = []
        for (sem, val, src) in deps:
            if src == "pe" and e.name == "pe":
                continue
            key = id(sem)
            if e.waited.get(key, (None, 0))[1] >= val:
                continue
            e.waited[key] = (sem, val)
            out.append((sem, val))
        return out

    def op(self, eng, fn, reads=(), writes=(), dma=False):
        e = self.q[eng]
        deps = []
        for b in reads:
            if b.w is not None:
                deps.append(b.w)
        for b in writes:
            if b.w is not None:
                deps.append(b.w)
            deps.extend(b.r.values())
        if dma:
            ns = len(e.dsems)
            slot = e.dn % ns
            rnd = e.dn // ns
            e.dn += 1
            sem = e.dsems[slot]
            if rnd > 0:
                deps.append((sem, 16 * rnd, "dma"))
            tok = (sem, 16 * (rnd + 1), "dma")
            inc = (sem, 16)
        else:
            e.cnt += 1
            tok = (e.sem, e.cnt, eng)
            inc = (e.sem, 1)
        waits = self._waits(e, deps)
        h = self.h[eng]
        for (wsem, wval) in waits:
            h.wait_ge(wsem, wval)
        fn(h).then_inc(inc[0], inc[1])
        for b in reads:
            k = id(tok[0])
            if k not in b.r or b.r[k][1] < tok[1]:
                b.r[k] = tok
        for b in writes:
            b.w = tok
            b.r = {}
        return tok

    def all_tokens(self):
        toks = []
        for e in self.q.values():
            if e.cnt > 0:
                toks.append((e.sem, e.cnt, e.name))
            ns = len(e.dsems)
            for slot in range(min(ns, e.dn)):
                n_uses = (e.dn - slot + ns - 1) // ns
                toks.append((e.dsems[slot], 16 * n_uses, "dma"))
        return toks

    def barrier(self, engines=("pe", "act", "dve", "pool", "sp")):
        toks = self.all_tokens()
        for name in engines:
            e = self.q[name]
            waits = []
            for (sem, val, src) in toks:
                key = id(sem)
                if e.waited.get(key, (None, 0))[1] >= val:
                    continue
                e.waited[key] = (sem, val)
                waits.append((sem, val))
            for (wsem, wval) in waits:
                self.h[name].wait_ge(wsem, wval)
        self.epoch += 1
        for name in engines:
            e = self.q[name]
            if e.cnt > 0:
                e.sem = self.nc.alloc_semaphore("prog%d_%s" % (self.epoch, name))
                e.cnt = 0


def build(SEQ, dbg=False, nexp=NEXP):
    NEXP_ = nexp
    OWN = SEQ // NCORES
    NST = SEQ // ST
    NOWN = OWN // ST
    S0 = NST - NOWN
    NKT = SEQ // 128

    nc = bass.Bass("TRN2", target_bir_lowering=False)

    def din(name, shape, dt=F32):
        return nc.dram_tensor(name, list(shape), dt, kind="ExternalInput").ap()

    xseq = din("xseq", [SEQ, D])
    validrow = din("validrow", [128, SEQ])
    kbias_d = din("kbias", [128, NKT])
    mem_d = din("mem", [NMEM, D])
    w_in = din("w_in", [D, 5120])
    w_out = din("w_out", [D, D])
    w_cq = din("w_cq", [D, D])
    w_ckv = din("w_ckv", [D, 2 * D])
    w_co = din("w_co", [D, D])
    w_router = din("w_router", [D, NEXP_])
    w_gu = din("w_gu", [NEXP_, D, 2 * D])
    w_down = din("w_down", [NEXP_, D, D])
    w_rga = din("w_rga", [8, 128, 128])
    w_rgx = din("w_rgx", [8, 128, 128])
    g_mix = din("g_mix", [128, D])
    g_cross = din("g_cross", [128, D])
    g_mem = din("g_mem", [128, D])
    g_ffn = din("g_ffn", [128, D])
    g_final = din("g_final", [128, D])
    convw_d = din("convw", [128, 8, 4])
    recv_d = din("recv", [128, 5, 8])
    lamv_d = din("lamv", [128, 4, 128])
    subg_d = din("subg", [128, 2])
    brt_d = din("brt", [128, NEXP_])
    bgu_d = din("bgu", [128, NEXP_, 32])
    bdown_d = din("bdown", [NEXP_, D])
    ident_d = din("ident", [128, 128], BF)
    identf_d = din("identf", [128, 128])
    dmask_d = din("dmask", [128, 4, ST], BF)

    out_d = nc.dram_tensor("out", [OWN, D], F32, kind="ExternalOutput").ap()

    skind = "ExternalOutput" if dbg else "Internal"

    def dscr(name, shape, dt):
        return nc.dram_tensor(name, list(shape), dt, kind=skind).ap()

    kT_d = dscr("kT_s", [1024, SEQ], BF)
    v_d = dscr("v_s", [SEQ, 1024], BF)
    hown_d = dscr("hown_s", [1024, OWN], F32)
    qT_d = dscr("qT_s", [1024, OWN], BF)
    cat_d = dscr("cat_s", [D, OWN], BF)
    h_d = dscr("h_s", [OWN, D], F32)
    oc_d = dscr("oc_s", [D, OWN], BF)

    wgu_b = [nc.dram_tensor("wgu_b%d" % ex, [D, 2 * D], BF, kind="Internal").ap() for ex in range(NEXP_)]
    wd_b = [nc.dram_tensor("wd_b%d" % ex, [D, D], BF, kind="Internal").ap() for ex in range(NEXP_)]
    k = K(nc)
    B_wgub = [[Buf() for _ in range(4)] for _ in range(NEXP_)]
    B_wdb = [[Buf() for _ in range(2)] for _ in range(NEXP_)]
    cast_list = []
    for ex in range(NEXP_):
        for a in range(4):
            cast_list.append((wgu_b[ex][a * 512:(a + 1) * 512, :].rearrange("(r p) n -> p r n", p=128),
                              w_gu[ex, a * 512:(a + 1) * 512, :].rearrange("(r p) n -> p r n", p=128), B_wgub[ex][a]))
        for a in range(2):
            cast_list.append((wd_b[ex][a * 1024:(a + 1) * 1024, :].rearrange("(r p) n -> p r n", p=128),
                              w_down[ex, a * 1024:(a + 1) * 1024, :].rearrange("(r p) n -> p r n", p=128), B_wdb[ex][a]))
    cast_pos = [0]

    def cast_some(n):
        for _ in range(n):
            if cast_pos[0] >= len(cast_list):
                return
            dst, srcap, bf_ = cast_list[cast_pos[0]]
            cast_pos[0] += 1
            k.op("pool", lambda e, dst=dst, srcap=srcap: e.dma_start(out=dst, in_=srcap), writes=[bf_], dma=True)

    B_kT = [[Buf() for _ in range(NST)] for _ in range(8)]
    B_v = [Buf() for _ in range(NKT)]
    B_hown = [[Buf() for _ in range(NOWN)] for _ in range(8)]
    B_qT = [[Buf() for _ in range(NOWN)] for _ in range(8)]
    B_cat = [[Buf() for _ in range(NOWN)] for _ in range(KC)]
    B_h = [Buf() for _ in range(OWN // 128)]
    B_oc = [Buf() for _ in range(NOWN)]
    B_out = Buf()

    stack_outer = ExitStack()
    cur = [stack_outer]

    sbn = [0]

    def sb(name, shape, dt):
        sbn[0] += 1
        return cur[0].enter_context(nc.sbuf_tensor("sb%d_%s" % (sbn[0], name), list(shape), dt))

    ident = sb("ident", [128, 128], BF); b_ident = Buf()
    identf = sb("identf", [128, 128], F32); b_identf = Buf()
    ones_bf = sb("ones_bf", [128, 128], BF); b_ones = Buf()
    k.op("sp", lambda e: e.dma_start(out=ident[:, :], in_=ident_d[:, :]), writes=[b_ident], dma=True)
    k.op("sp", lambda e: e.dma_start(out=identf[:, :], in_=identf_d[:, :]), writes=[b_identf], dma=True)
    k.op("dve", lambda e: e.memset(ones_bf[:, :], 1.0), writes=[b_ones])
    epst = {}
    for ci, cv in enumerate((EPS, DA_EPS, 1.0)):
        tcst = sb("cst%d" % ci, [128, 1], F32)
        k.op("dve", lambda e, tcst=tcst, cv=cv: e.memset(tcst[:, :], cv), writes=[Buf()])
        epst[cv] = tcst[:, :]
    k.barrier()

    PS = [nc.alloc_psum_tensor("ps%d" % i, [128, 512], F32) for i in range(8)]
    PSB = [Buf() for _ in range(8)]

    def psbf(i):
        return PS[i][:, :].bitcast(BF)

    def load_w(dst, dst_buf, src2d, c0, c1, kcn=KC, dcol0=0):
        srcv = src2d.rearrange("(kc p) n -> p kc n", p=128)
        for kc in range(kcn):
            k.op("pool", lambda e, kc=kc: e.dma_start(out=dst[:, kc, dcol0:dcol0 + (c1 - c0)], in_=srcv[:, kc, c0:c1]),
                 writes=[dst_buf], dma=True)

    def rms_tile(xt_ap, b_x, grep, b_g, nb_ap, b_nb, junk, b_junk, ss, b_ss, rstd, b_rstd, eps=EPS):
        k.op("act", lambda e: e.activation(out=junk, in_=xt_ap, func=AF.Square, accum_out=ss),
             reads=[b_x], writes=[b_junk, b_ss])
        k.op("act", lambda e: e.activation(out=rstd, in_=ss, func=AF.Sqrt, scale=1.0 / D, bias=epst[eps]),
             reads=[b_ss], writes=[b_rstd])
        k.op("dve", lambda e: e.reciprocal(out=rstd, in_=rstd),
             reads=[b_rstd], writes=[b_rstd])
        k.op("dve", lambda e: e.scalar_tensor_tensor(out=nb_ap, in0=xt_ap, scalar=rstd, in1=grep, op0=ALU.mult, op1=ALU.mult),
             reads=[b_x, b_rstd, b_g], writes=[b_nb])

    def transpose_tile(nb_t, b_nb, nT, b_nT, col0, banks=(0, 1)):
        for half in range(2):
            bank = banks[half]
            pv = psbf(bank)
            for i in range(8):
                kc = half * 8 + i
                k.op("pe", lambda e, kc=kc, i=i, pv=pv: e.transpose(out=pv[:, i * 128:(i + 1) * 128], in_=nb_t[:, kc * 128:(kc + 1) * 128], identity=ident[:, :]),
                     reads=[b_nb, b_ident], writes=[PSB[bank]])
            k.op("act", lambda e, half=half, pv=pv: e.activation(out=nT[:, half * 8:(half + 1) * 8, col0:col0 + 128],
                                                                 in_=pv.rearrange("p (a b) -> p a b", a=8), func=AF.Copy),
                 reads=[PSB[bank]], writes=[b_nT])

    ph1 = ExitStack()
    cur[0] = ph1
    W1 = sb("W1", [128, KC, 3072], BF); b_W1 = Buf()
    load_w(W1, b_W1, w_in, 1024, 4096)
    gmix = sb("gmix", [128, D], F32); b_gmix = Buf()
    k.op("sp", lambda e: e.dma_start(out=gmix[:, :], in_=g_mix[:, :]), writes=[b_gmix], dma=True)
    wa = sb("wa", [128, 8, 128], BF); b_wa = Buf()
    wx = sb("wx", [128, 8, 128], BF); b_wx = Buf()
    k.op("pool", lambda e: e.dma_start(out=wa[:, :, :], in_=w_rga.rearrange("n c d -> c n d")), writes=[b_wa], dma=True)
    k.op("pool", lambda e: e.dma_start(out=wx[:, :, :], in_=w_rgx.rearrange("n c d -> c n d")), writes=[b_wx], dma=True)
    convw = sb("convw", [128, 8, 4], F32); b_convw = Buf()
    recv = sb("recv", [128, 5, 8], F32); b_recv = Buf()
    k.op("sp", lambda e: e.dma_start(out=convw[:, :, :], in_=convw_d[:, :, :]), writes=[b_convw], dma=True)
    k.op("sp", lambda e: e.dma_start(out=recv[:, :, :], in_=recv_d[:, :, :]), writes=[b_recv], dma=True)

    uu = sb("uu", [128, 8], F32); b_uu = Buf()
    pp = sb("pp", [128, 8], F32); b_pp = Buf()
    cneg = sb("cneg", [128, 8], F32); b_cneg = Buf()
    cneg2 = sb("cneg2", [128, 8], F32); b_cneg2 = Buf()
    k.op("act", lambda e: e.activation(out=uu[:, :], in_=recv[:, 3, :], func=AF.Exp, scale=-1.0), reads=[b_recv], writes=[b_uu])
    k.op("dve", lambda e: e.tensor_scalar(out=pp[:, :], in0=uu[:, :], scalar1=-1.0 / 8, scalar2=1.0 / 7, op0=ALU.mult, op1=ALU.add), reads=[b_uu], writes=[b_pp])
    for c in (6, 5, 4, 3, 2, 1):
        k.op("dve", lambda e: e.tensor_tensor(out=pp[:, :], in0=pp[:, :], in1=uu[:, :], op=ALU.mult), reads=[b_pp, b_uu], writes=[b_pp])
        k.op("dve", lambda e, c=c: e.tensor_scalar(out=pp[:, :], in0=pp[:, :], scalar1=-1.0, scalar2=1.0 / c, op0=ALU.mult, op1=ALU.add), reads=[b_pp], writes=[b_pp])
    k.op("dve", lambda e: e.tensor_tensor(out=pp[:, :], in0=pp[:, :], in1=uu[:, :], op=ALU.mult), reads=[b_pp, b_uu], writes=[b_pp])
    k.op("dve", lambda e: e.tensor_scalar(out=cneg[:, :], in0=pp[:, :], scalar1=-8.0, scalar2=None, op0=ALU.mult), reads=[b_pp], writes=[b_cneg])
    k.op("dve", lambda e: e.tensor_scalar(out=cneg2[:, :], in0=pp[:, :], scalar1=-16.0, scalar2=None, op0=ALU.mult), reads=[b_pp], writes=[b_cneg2])

    xt = [sb("xt%d" % i, [128, D], F32) for i in range(2)]; b_xt = [Buf(), Buf()]
    nb = [sb("nb%d" % i, [128, D], BF) for i in range(2)]; b_nb = [Buf(), Buf()]
    ss = [sb("ss%d" % i, [128, 1], F32) for i in range(2)]; b_ss = [Buf(), Buf()]
    rstd = [sb("rstd%d" % i, [128, 1], F32) for i in range(2)]; b_rstd = [Buf(), Buf()]
    nT0 = sb("nT0", [128, KC, ST], BF); nT = [nT0, nT0]; b_nT0 = Buf(); b_nT = [b_nT0, b_nT0]

    tile_ctr = [0]

    def norm_transpose_supertile(s, grep, b_g, nTbuf, b_nTbuf, src_rows, b_src=None, eps=EPS):
        for tt in range(4):
            i = tile_ctr[0] % 2
            tile_ctr[0] += 1
            rd = [b_src[tt]] if b_src is not None else []
            k.op("sp", lambda e, tt=tt, i=i: e.dma_start(out=xt[i][:, :], in_=src_rows(tt)), reads=rd, writes=[b_xt[i]], dma=True)
            rms_tile(xt[i][:, :], b_xt[i], grep[:, :], b_g, nb[i][:, :], b_nb[i], nb[i][:, :], b_nb[i], ss[i][:, :], b_ss[i], rstd[i][:, :], b_rstd[i], eps)
            transpose_tile(nb[i], b_nb[i], nTbuf, b_nTbuf, tt * 128)

    xrp = sb("xrp", [128, 8, 3 + ST], F32); b_xrp = [Buf() for _ in range(8)]
    k.op("pool", lambda e: e.memset(xrp[:, :, :], 0.0), writes=b_xrp)
    hlast = sb("hlast", [128, 8], F32); b_hlast = [Buf() for _ in range(8)]
    k.op("pool", lambda e: e.memset(hlast[:, :], 0.0), writes=b_hlast)
    NR = 1
    xc = [sb("xc%d" % i, [128, ST], F32) for i in range(NR)]; b_xc = [Buf() for _ in range(NR)]
    xcb = [sb("xcb%d" % i, [128, ST], BF) for i in range(NR)]; b_xcb = [Buf() for _ in range(NR)]
    rr = [sb("rr%d" % i, [128, ST], F32) for i in range(NR)]; b_rr = [Buf() for _ in range(NR)]
    ii = [sb("ii%d" % i, [128, ST], F32) for i in range(NR)]; b_ii = [Buf() for _ in range(NR)]
    aa = [sb("aa%d" % i, [128, ST], F32) for i in range(NR)]; b_aa = [Buf() for _ in range(NR)]
    a2 = [sb("a2%d" % i, [128, ST], F32) for i in range(NR)]; b_a2 = [Buf() for _ in range(NR)]
    bb = [sb("bb%d" % i, [128, ST], F32) for i in range(NR)]; b_bb = [Buf() for _ in range(NR)]
    hh = [sb("hh%d" % i, [128, ST], F32) for i in range(NR)]; b_hh = [Buf() for _ in range(NR)]
    vrow = [sb("vrow%d" % i, [128, ST], F32) for i in range(2)]; b_vrow = [Buf(), Buf()]
    kst = [sb("kst%d" % i, [128, ST], BF) for i in range(2)]; b_kst = [Buf(), Buf()]
    vst = [sb("vst%d" % i, [128, 1024], BF) for i in range(2)]; b_vst = [Buf(), Buf()]

    rec_ctr = [0]
    for s in range(NST):
        nTb, b_nTb = nT[s % 2], b_nT[s % 2]
        norm_transpose_supertile(s, gmix, b_gmix, nTb, b_nTb, lambda tt, s=s: xseq[s * ST + tt * 128: s * ST + (tt + 1) * 128, :])
        own = s >= S0
        if not own:
            vi = s % 2
            k.op("sp", lambda e, s=s, vi=vi: e.dma_start(out=vrow[vi][:, :], in_=validrow[:, s * ST:(s + 1) * ST]), writes=[b_vrow[vi]], dma=True)
        for m in range(8):
            bank = 2 + (m % 2)
            for kc in range(KC):
                k.op("pe", lambda e, kc=kc, m=m, bank=bank: e.matmul(PS[bank][:, :], lhsT=W1[:, kc, m * 128:(m + 1) * 128], rhs=nTb[:, kc, :], start=(kc == 0), stop=(kc == KC - 1)),
                     reads=[b_W1, b_nTb], writes=[PSB[bank]])
            ki = m % 2
            k.op("act", lambda e, ki=ki, bank=bank: e.activation(out=kst[ki][:, :], in_=PS[bank][:, :], func=AF.Copy), reads=[PSB[bank]], writes=[b_kst[ki]])
            k.op("sp", lambda e, ki=ki, m=m, s=s: e.dma_start(out=kT_d[m * 128:(m + 1) * 128, s * ST:(s + 1) * ST], in_=kst[ki][:, :]),
                 reads=[b_kst[ki]], writes=[B_kT[m][s]], dma=True)
        for tt in range(4):
            vi = tt % 2
            for half in range(2):
                bank = 2 + half
                for kc in range(KC):
                    k.op("pe", lambda e, kc=kc, tt=tt, half=half, bank=bank: e.matmul(PS[bank][:, :], lhsT=nTb[:, kc, tt * 128:(tt + 1) * 128], rhs=W1[:, kc, 1024 + half * 512:1024 + (half + 1) * 512], start=(kc == 0), stop=(kc == KC - 1)),
                         reads=[b_W1, b_nTb], writes=[PSB[bank]])
                k.op("act", lambda e, vi=vi, half=half, bank=bank: e.activation(out=vst[vi][:, half * 512:(half + 1) * 512], in_=PS[bank][:, :], func=AF.Copy), reads=[PSB[bank]], writes=[b_vst[vi]])
            kt = s * 4 + tt
            k.op("sp", lambda e, vi=vi, kt=kt: e.dma_start(out=v_d[kt * 128:(kt + 1) * 128, :], in_=vst[vi][:, :]), reads=[b_vst[vi]], writes=[B_v[kt]], dma=True)
        cast_some((len(cast_list) // 2 + NST - 1) // NST)
        for ct in range(8):
            ri = rec_ctr[0] % NR
            rec_ctr[0] += 1
            bank = 4 + (ct % 2)
            for kc in range(KC):
                k.op("pe", lambda e, kc=kc, ct=ct, bank=bank: e.matmul(PS[bank][:, :], lhsT=W1[:, kc, 2048 + ct * 128:2048 + (ct + 1) * 128], rhs=nTb[:, kc, :], start=(kc == 0), stop=(kc == KC - 1)),
                     reads=[b_W1, b_nTb], writes=[PSB[bank]])
            k.op("act", lambda e, ct=ct, bank=bank: e.activation(out=xrp[:, ct, 3:3 + ST], in_=PS[bank][:, :], func=AF.Copy), reads=[PSB[bank]], writes=[b_xrp[ct]])
            k.op("pool", lambda e, ct=ct, ri=ri: e.tensor_scalar(out=xc[ri][:, :], in0=xrp[:, ct, 0:ST], scalar1=convw[:, ct, 0:1], scalar2=recv[:, 0, ct:ct + 1], op0=ALU.mult, op1=ALU.add),
                 reads=[b_xrp[ct], b_convw, b_recv], writes=[b_xc[ri]])
            for j in (1, 2, 3):
                k.op("dve", lambda e, ct=ct, ri=ri, j=j: e.scalar_tensor_tensor(out=xc[ri][:, :], in0=xrp[:, ct, j:j + ST], scalar=convw[:, ct, j:j + 1], in1=xc[ri][:, :], op0=ALU.mult, op1=ALU.add),
                     reads=[b_xrp[ct], b_convw, b_xc[ri]], writes=[b_xc[ri]])
            k.op("pool", lambda e, ct=ct: e.tensor_copy(out=xrp[:, ct, 0:3], in_=xrp[:, ct, ST:ST + 3]), reads=[b_xrp[ct]], writes=[b_xrp[ct]])
            k.op("pool", lambda e, ri=ri: e.tensor_copy(out=xcb[ri][:, :], in_=xc[ri][:, :]), reads=[b_xc[ri]], writes=[b_xcb[ri]])
            k.op("pe", lambda e, ct=ct, ri=ri: e.matmul(PS[6][:, :], lhsT=wa[:, ct, :], rhs=xcb[ri][:, :], start=True, stop=True), reads=[b_wa, b_xcb[ri]], writes=[PSB[6]])
            k.op("pe", lambda e, ct=ct, ri=ri: e.matmul(PS[7][:, :], lhsT=wx[:, ct, :], rhs=xcb[ri][:, :], start=True, stop=True), reads=[b_wx, b_xcb[ri]], writes=[PSB[7]])
            k.op("act", lambda e, ct=ct, ri=ri: e.activation(out=rr[ri][:, :], in_=PS[6][:, :], func=AF.Sigmoid, bias=recv[:, 1, ct:ct + 1]), reads=[PSB[6], b_recv], writes=[b_rr[ri]])
            k.op("act", lambda e, ct=ct, ri=ri: e.activation(out=ii[ri][:, :], in_=PS[7][:, :], func=AF.Sigmoid, bias=recv[:, 2, ct:ct + 1]), reads=[PSB[7], b_recv], writes=[b_ii[ri]])
            k.op("act", lambda e, ct=ct, ri=ri: e.activation(out=aa[ri][:, :], in_=rr[ri][:, :], func=AF.Exp, scale=cneg[:, ct:ct + 1]), reads=[b_rr[ri], b_cneg], writes=[b_aa[ri]])
            k.op("act", lambda e, ct=ct, ri=ri: e.activation(out=a2[ri][:, :], in_=rr[ri][:, :], func=AF.Exp, scale=cneg2[:, ct:ct + 1]), reads=[b_rr[ri], b_cneg2], writes=[b_a2[ri]])
            k.op("act", lambda e, ri=ri: e.activation(out=a2[ri][:, :], in_=a2[ri][:, :], func=AF.Sqrt, scale=-1.0, bias=epst[1.0]), reads=[b_a2[ri]], writes=[b_a2[ri]])
            k.op("dve", lambda e, ri=ri: e.tensor_tensor(out=bb[ri][:, :], in0=ii[ri][:, :], in1=xc[ri][:, :], op=ALU.mult), reads=[b_ii[ri], b_xc[ri]], writes=[b_bb[ri]])
            k.op("dve", lambda e, ri=ri: e.tensor_tensor(out=bb[ri][:, :], in0=bb[ri][:, :], in1=a2[ri][:, :], op=ALU.mult), reads=[b_bb[ri], b_a2[ri]], writes=[b_bb[ri]])
            if not own:
                k.op("dve", lambda e, ri=ri, vi=s % 2: e.tensor_tensor(out=bb[ri][:, :], in0=bb[ri][:, :], in1=vrow[vi][:, :], op=ALU.mult), reads=[b_bb[ri], b_vrow[s % 2]], writes=[b_bb[ri]])
            k.op("dve", lambda e, ri=ri, ct=ct: e.tensor_tensor_scan(out=hh[ri][:, :], data0=aa[ri][:, :], data1=bb[ri][:, :], initial=hlast[:, ct:ct + 1], op0=ALU.mult, op1=ALU.add),
                 reads=[b_aa[ri], b_bb[ri], b_hlast[ct]], writes=[b_hh[ri]])
            k.op("dve", lambda e, ri=ri, ct=ct: e.tensor_copy(out=hlast[:, ct:ct + 1], in_=hh[ri][:, ST - 1:ST]), reads=[b_hh[ri]], writes=[b_hlast[ct]])
            if own:
                j = s - S0
                k.op("sp", lambda e, ri=ri, ct=ct, j=j: e.dma_start(out=hown_d[ct * 128:(ct + 1) * 128, j * ST:(j + 1) * ST], in_=hh[ri][:, :]), reads=[b_hh[ri]], writes=[B_hown[ct][j]], dma=True)

    k.barrier()

    W2 = W1
    b_W2 = Buf()
    load_w(W2, b_W2, w_in, 0, 1024, dcol0=0)
    load_w(W2, b_W2, w_in, 4096, 5120, dcol0=1024)
    ybuf = W1[:, 0:8, 2048:3072].bitcast(F32); b_ybuf = [b_W2 for _ in range(8)]
    ysq = W1[:, 8:16, 2048:2048 + ST]; b_ysq = [b_W2 for _ in range(8)]
    rs = a2[0]; b_rs = b_a2[0]
    ynb = [xcb[0], xcb[0]]; b_ynb = [b_xcb[0], b_xcb[0]]
    for j in range(NOWN):
        s = S0 + j
        nTb, b_nTb = nT[s % 2], b_nT[s % 2]
        norm_transpose_supertile(s, gmix, b_gmix, nTb, b_nTb, lambda tt, s=s: xseq[s * ST + tt * 128: s * ST + (tt + 1) * 128, :])
        for m in range(8):
            bank = 2 + (m % 2)
            for kc in range(KC):
                k.op("pe", lambda e, kc=kc, m=m, bank=bank: e.matmul(PS[bank][:, :], lhsT=W2[:, kc, m * 128:(m + 1) * 128], rhs=nTb[:, kc, :], start=(kc == 0), stop=(kc == KC - 1)),
                     reads=[b_W2, b_nTb], writes=[PSB[bank]])
            ki = m % 2
            k.op("act", lambda e, ki=ki, bank=bank: e.activation(out=kst[ki][:, :], in_=PS[bank][:, :], func=AF.Copy), reads=[PSB[bank]], writes=[b_kst[ki]])
            k.op("sp", lambda e, ki=ki, m=m, j=j: e.dma_start(out=qT_d[m * 128:(m + 1) * 128, j * ST:(j + 1) * ST], in_=kst[ki][:, :]),
                 reads=[b_kst[ki]], writes=[B_qT[m][j]], dma=True)
        for ct in range(8):
            ri = ct % NR
            bank = 4 + (ct % 2)
            for kc in range(KC):
                k.op("pe", lambda e, kc=kc, ct=ct, bank=bank: e.matmul(PS[bank][:, :], lhsT=W2[:, kc, 1024 + ct * 128:1024 + (ct + 1) * 128], rhs=nTb[:, kc, :], start=(kc == 0), stop=(kc == KC - 1)),
                     reads=[b_W2, b_nTb], writes=[PSB[bank]])
            k.op("act", lambda e, ri=ri, bank=bank: e.activation(out=xc[ri][:, :], in_=PS[bank][:, :], func=AF.Copy), reads=[PSB[bank]], writes=[b_xc[ri]])
            k.op("dve", lambda e, ri=ri: e.tensor_tensor(out=rr[ri][:, :], in0=xc[ri][:, :], in1=xc[ri][:, :], op=ALU.mult), reads=[b_xc[ri]], writes=[b_rr[ri]])
            k.op("dve", lambda e, ri=ri: e.tensor_scalar(out=rr[ri][:, :], in0=rr[ri][:, :], scalar1=0.044715, scalar2=1.0, op0=ALU.mult, op1=ALU.add), reads=[b_rr[ri]], writes=[b_rr[ri]])
            k.op("dve", lambda e, ri=ri: e.tensor_tensor(out=rr[ri][:, :], in0=rr[ri][:, :], in1=xc[ri][:, :], op=ALU.mult), reads=[b_rr[ri], b_xc[ri]], writes=[b_rr[ri]])
            k.op("act", lambda e, ri=ri: e.activation(out=ii[ri][:, :], in_=rr[ri][:, :], func=AF.Sigmoid, scale=1.5957691216057308), reads=[b_rr[ri]], writes=[b_ii[ri]])
            k.op("sp", lambda e, ri=ri, ct=ct, j=j: e.dma_start(out=hh[ri][:, :], in_=hown_d[ct * 128:(ct + 1) * 128, j * ST:(j + 1) * ST]), reads=[B_hown[ct][j]], writes=[b_hh[ri]], dma=True)
            k.op("dve", lambda e, ri=ri: e.tensor_tensor(out=bb[ri][:, :], in0=xc[ri][:, :], in1=ii[ri][:, :], op=ALU.mult), reads=[b_xc[ri], b_ii[ri]], writes=[b_bb[ri]])
            k.op("dve", lambda e, ri=ri, ct=ct: e.tensor_tensor(out=ybuf[:, ct, :], in0=bb[ri][:, :], in1=hh[ri][:, :], op=ALU.mult), reads=[b_bb[ri], b_hh[ri]], writes=[b_ybuf[ct]])
            k.op("act", lambda e, ct=ct: e.activation(out=ysq[:, ct, :], in_=ybuf[:, ct, :], func=AF.Square), reads=[b_ybuf[ct]], writes=[b_ysq[ct]])
        for ct in range(8):
            k.op("pe", lambda e, ct=ct: e.matmul(PS[6][:, :], lhsT=ones_bf[:, :], rhs=ysq[:, ct, :], start=(ct == 0), stop=(ct == 7)), reads=[b_ones, b_ysq[ct]], writes=[PSB[6]])
        k.op("act", lambda e: e.activation(out=rs[:, :], in_=PS[6][:, :], func=AF.Sqrt, scale=1.0 / 1024, bias=epst[EPS]), reads=[PSB[6]], writes=[b_rs])
        k.op("dve", lambda e: e.reciprocal(out=rs[:, :], in_=rs[:, :]), reads=[b_rs], writes=[b_rs])
        for ct in range(8):
            yi = ct % 2
            k.op("dve", lambda e, ct=ct, yi=yi: e.scalar_tensor_tensor(out=ynb[yi][:, :], in0=ybuf[:, ct, :], scalar=recv[:, 4, ct:ct + 1], in1=rs[:, :], op0=ALU.mult, op1=ALU.mult),
                 reads=[b_ybuf[ct], b_recv, b_rs], writes=[b_ynb[yi]])
            k.op("sp", lambda e, ct=ct, yi=yi, j=j: e.dma_start(out=cat_d[1024 + ct * 128:1024 + (ct + 1) * 128, j * ST:(j + 1) * ST], in_=ynb[yi][:, :]), reads=[b_ynb[yi]], writes=[B_cat[8 + ct][j]], dma=True)

    k.barrier()
    ph1.close()

    ph2 = ExitStack()
    cur[0] = ph2
    cast_some(len(cast_list))
    kT2 = sb("kT2", [128, 2, SEQ], BF); b_kT2 = [Buf(), Buf()]
    vv = sb("vv", [128, NKT, 256], BF); b_vv = Buf()
    qh = sb("qh", [128, 2, OWN], BF); b_qh = Buf()
    kbias = sb("kbias", [128, NKT], F32); b_kbias = Buf()
    dmask = sb("dmask", [128, 4, ST], BF); b_dmask = Buf()
    lamv = sb("lamv", [128, 4, 128], F32); b_lamv = Buf()
    subg = sb("subg", [128, 2], F32); b_subg = Buf()
    lam_t = sb("lam_t", [128, 4], F32); b_lam = Buf()
    prod = sb("prod", [128, 128], F32); b_prod = Buf()
    pT = [sb("pT%d" % i, [128, ST], BF) for i in range(3)]; b_pT = [Buf() for _ in range(3)]
    rinv = sb("rinv", [128, ST], F32); b_rinv = Buf()
    osb = sb("osb", [128, 2, 2, ST], F32); b_osb = [Buf(), Buf()]
    od = sb("od", [128, 2, ST], F32); b_od = Buf()
    osq = sb("osq", [128, 2, ST], BF); b_osq = Buf()
    rs2 = sb("rs2", [128, ST], F32); b_rs2 = Buf()
    onb = [sb("onb%d" % i, [128, ST], BF) for i in range(2)]; b_onb = [Buf(), Buf()]
    k.op("sp", lambda e: e.dma_start(out=kbias[:, :], in_=kbias_d[:, :]), writes=[b_kbias], dma=True)
    k.op("sp", lambda e: e.dma_start(out=dmask[:, :, :], in_=dmask_d[:, :, :]), writes=[b_dmask], dma=True)
    k.op("sp", lambda e: e.dma_start(out=lamv[:, :, :], in_=lamv_d[:, :, :]), writes=[b_lamv], dma=True)
    k.op("sp", lambda e: e.dma_start(out=subg[:, :], in_=subg_d[:, :]), writes=[b_subg], dma=True)
    for t in range(2):
        k.op("dve", lambda e, t=t: e.tensor_tensor(out=prod[:, :], in0=lamv[:, 2 * t, :], in1=lamv[:, 2 * t + 1, :], op=ALU.mult), reads=[b_lamv], writes=[b_prod])
        k.op("dve", lambda e, t=t: e.reduce_sum(out=lam_t[:, t:t + 1], in_=prod[:, :], axis=AX.X), reads=[b_prod], writes=[b_lam])
    k.op("act", lambda e: e.activation(out=lam_t[:, 0:2], in_=lam_t[:, 0:2], func=AF.Exp), reads=[b_lam], writes=[b_lam])
    k.op("dve", lambda e: e.tensor_tensor(out=lam_t[:, 2:3], in0=lam_t[:, 1:2], in1=lam_t[:, 0:1], op=ALU.subtract), reads=[b_lam], writes=[b_lam])
    k.op("dve", lambda e: e.tensor_scalar(out=lam_t[:, 2:3], in0=lam_t[:, 2:3], scalar1=-LAMBDA_INIT, scalar2=None, op0=ALU.add), reads=[b_lam], writes=[b_lam])
    k.op("dve", lambda e: e.tensor_scalar(out=subg[:, :], in0=subg[:, :], scalar1=1.0 - LAMBDA_INIT, scalar2=None, op0=ALU.mult), reads=[b_subg], writes=[b_subg])
    SCALE = 128.0 ** -0.5
    pctr = 0
    for hd in range(4):
        for m in range(2):
            gm = hd * 2 + m
            for s in range(NST):
                k.op("sp", lambda e, m=m, gm=gm, s=s: e.dma_start(out=kT2[:, m, s * ST:(s + 1) * ST], in_=kT_d[gm * 128:(gm + 1) * 128, s * ST:(s + 1) * ST]),
                     reads=[B_kT[gm][s]], writes=[b_kT2[m]], dma=True)
            k.op("sp", lambda e, m=m, gm=gm: e.dma_start(out=qh[:, m, :], in_=qT_d[gm * 128:(gm + 1) * 128, :]), reads=B_qT[gm], writes=[b_qh], dma=True)
        for s in range(NST):
            k.op("sp", lambda e, s=s, hd=hd: e.dma_start(out=vv[:, s * 4:(s + 1) * 4, :], in_=v_d[s * ST:(s + 1) * ST, hd * 256:(hd + 1) * 256].rearrange("(t p) c -> p t c", p=128)),
                 reads=B_v[s * 4:(s + 1) * 4], writes=[b_vv], dma=True)
        for j in range(NOWN):
            nkb = (S0 + j + 1) * 4
            for m in range(2):
                for kb in range(nkb):
                    sbk = kb % 2
                    pi = pctr % 3
                    pctr += 1
                    k.op("pe", lambda e, m=m, kb=kb, j=j, sbk=sbk: e.matmul(PS[sbk][:, :], lhsT=kT2[:, m, kb * 128:(kb + 1) * 128], rhs=qh[:, m, j * ST:(j + 1) * ST], start=True, stop=True),
                         reads=[b_kT2[m], b_qh], writes=[PSB[sbk]])
                    k.op("act", lambda e, kb=kb, sbk=sbk, pi=pi: e.activation(out=pT[pi][:, :], in_=PS[sbk][:, :], func=AF.Exp, scale=SCALE, bias=kbias[:, kb:kb + 1]),
                         reads=[PSB[sbk], b_kbias], writes=[b_pT[pi]])
                    kr = kb - (nkb - 4)
                    if kr >= 0:
                        k.op("dve", lambda e, pi=pi, kr=kr: e.tensor_tensor(out=pT[pi][:, :], in0=pT[pi][:, :], in1=dmask[:, kr, :], op=ALU.mult), reads=[b_pT[pi], b_dmask], writes=[b_pT[pi]])
                    for dvc in range(2):
                        k.op("pe", lambda e, kb=kb, dvc=dvc, pi=pi, nkb=nkb: e.matmul(PS[2 + dvc][:, :], lhsT=vv[:, kb, dvc * 128:(dvc + 1) * 128], rhs=pT[pi][:, :], start=(kb == 0), stop=(kb == nkb - 1)),
                             reads=[b_vv, b_pT[pi]], writes=[PSB[2 + dvc]])
                    k.op("pe", lambda e, kb=kb, pi=pi, nkb=nkb: e.matmul(PS[4][:, :], lhsT=ones_bf[:, :], rhs=pT[pi][:, :], start=(kb == 0), stop=(kb == nkb - 1)),
                         reads=[b_ones, b_pT[pi]], writes=[PSB[4]])
                k.op("dve", lambda e: e.reciprocal(out=rinv[:, :], in_=PS[4][:, :]), reads=[PSB[4]], writes=[b_rinv])
                for dvc in range(2):
                    k.op("dve", lambda e, m=m, dvc=dvc: e.tensor_tensor(out=osb[:, m, dvc, :], in0=PS[2 + dvc][:, :], in1=rinv[:, :], op=ALU.mult), reads=[PSB[2 + dvc], b_rinv], writes=[b_osb[m]])
            k.op("dve", lambda e: e.scalar_tensor_tensor(out=od[:, :, :], in0=osb[:, 1, :, :], scalar=lam_t[:, 2:3], in1=osb[:, 0, :, :], op0=ALU.mult, op1=ALU.add),
                 reads=[b_osb[0], b_osb[1], b_lam], writes=[b_od])
            k.op("act", lambda e: e.activation(out=osq[:, :, :], in_=od[:, :, :], func=AF.Square), reads=[b_od], writes=[b_osq])
            for dvc in range(2):
                k.op("pe", lambda e, dvc=dvc: e.matmul(PS[5][:, :], lhsT=ones_bf[:, :], rhs=osq[:, dvc, :], start=(dvc == 0), stop=(dvc == 1)), reads=[b_ones, b_osq], writes=[PSB[5]])
            k.op("act", lambda e: e.activation(out=rs2[:, :], in_=PS[5][:, :], func=AF.Sqrt, scale=1.0 / 256, bias=epst[DA_EPS]), reads=[PSB[5]], writes=[b_rs2])
            k.op("dve", lambda e: e.reciprocal(out=rs2[:, :], in_=rs2[:, :]), reads=[b_rs2], writes=[b_rs2])
            for dvc in range(2):
                k.op("dve", lambda e, dvc=dvc: e.scalar_tensor_tensor(out=onb[dvc][:, :], in0=od[:, dvc, :], scalar=subg[:, dvc:dvc + 1], in1=rs2[:, :], op0=ALU.mult, op1=ALU.mult),
                     reads=[b_od, b_subg, b_rs2], writes=[b_onb[dvc]])
                kc = hd * 2 + dvc
                k.op("sp", lambda e, dvc=dvc, kc=kc, j=j: e.dma_start(out=cat_d[kc * 128:(kc + 1) * 128, j * ST:(j + 1) * ST], in_=onb[dvc][:, :]), reads=[b_onb[dvc]], writes=[B_cat[kc][j]], dma=True)
    k.barrier()
    ph2.close()

    ph3 = ExitStack()
    cur[0] = ph3
    Wb = sb("Wb", [128, KC, D], BF); b_Wb = Buf()
    gv = sb("gv", [128, D], F32); b_gv = Buf()
    xt = [sb("xt%d" % i, [128, D], F32) for i in range(2)]; b_xt = [Buf(), Buf()]
    nb = [sb("nb%d" % i, [128, D], BF) for i in range(2)]; b_nb = [Buf(), Buf()]
    ss = [sb("ss%d" % i, [128, 1], F32) for i in range(2)]; b_ss = [Buf(), Buf()]
    rstd = [sb("rstd%d" % i, [128, 1], F32) for i in range(2)]; b_rstd = [Buf(), Buf()]
    nT0 = sb("nT0", [128, KC, ST], BF); b_nT0 = Buf()
    catT = sb("catT", [128, KC, ST], BF); b_catT = Buf()
    ht = [sb("ht%d" % i, [128, D], F32) for i in range(2)]; b_ht = [Buf(), Buf()]
    memT = sb("memT", [128, KC, NMEM], BF); b_memT = Buf()
    KmT = sb("KmT", [128, KC, NMEM], BF); b_KmT = Buf()
    Vm = sb("Vm", [128, 2, D], BF); b_Vm = Buf()
    pc = [sb("pc%d" % i, [128, ST], BF) for i in range(2)]; b_pc = [Buf(), Buf()]
    rinv = sb("rinv", [128, ST], F32); b_rinv = Buf()

    load_w(Wb, b_Wb, w_out, 0, D)
    hctr = 0
    for j in range(NOWN):
        for kc in range(KC):
            k.op("sp", lambda e, kc=kc, j=j: e.dma_start(out=catT[:, kc, :], in_=cat_d[kc * 128:(kc + 1) * 128, j * ST:(j + 1) * ST]), reads=[B_cat[kc][j]], writes=[b_catT], dma=True)
        for tt in range(4):
            hi = hctr % 2
            hctr += 1
            row0 = (S0 + j) * ST + tt * 128
            k.op("sp", lambda e, hi=hi, row0=row0: e.dma_start(out=xt[hi][:, :], in_=xseq[row0:row0 + 128, :]), writes=[b_xt[hi]], dma=True)
            for n4 in range(4):
                bank = n4 % 2
                for kc in range(KC):
                    k.op("pe", lambda e, kc=kc, tt=tt, n4=n4, bank=bank: e.matmul(PS[bank][:, :], lhsT=catT[:, kc, tt * 128:(tt + 1) * 128], rhs=Wb[:, kc, n4 * 512:(n4 + 1) * 512], start=(kc == 0), stop=(kc == KC - 1)),
                         reads=[b_catT, b_Wb], writes=[PSB[bank]])
                k.op("dve", lambda e, hi=hi, n4=n4, bank=bank: e.tensor_tensor(out=ht[hi][:, n4 * 512:(n4 + 1) * 512], in0=PS[bank][:, :], in1=xt[hi][:, n4 * 512:(n4 + 1) * 512], op=ALU.add),
                     reads=[PSB[bank], b_xt[hi]], writes=[b_ht[hi]])
            ti = j * 4 + tt
            k.op("sp", lambda e, hi=hi, ti=ti: e.dma_start(out=h_d[ti * 128:(ti + 1) * 128, :], in_=ht[hi][:, :]), reads=[b_ht[hi]], writes=[B_h[ti]], dma=True)

    k.op("sp", lambda e: e.dma_start(out=gv[:, :], in_=g_mem[:, :]), writes=[b_gv], dma=True)
    tile_ctr[0] = 0
    for mt in range(2):
        i = mt
        k.op("sp", lambda e, mt=mt, i=i: e.dma_start(out=xt[i][:, :], in_=mem_d[mt * 128:(mt + 1) * 128, :]), writes=[b_xt[i]], dma=True)
        rms_tile(xt[i][:, :], b_xt[i], gv[:, :], b_gv, nb[i][:, :], b_nb[i], nb[i][:, :], b_nb[i], ss[i][:, :], b_ss[i], rstd[i][:, :], b_rstd[i], EPS)
        transpose_tile(nb[i], b_nb[i], memT, b_memT, mt * 128)
    load_w(Wb, b_Wb, w_ckv, 0, D)
    for fc in range(KC):
        bank = 2 + fc % 2
        for kc in range(KC):
            k.op("pe", lambda e, kc=kc, fc=fc, bank=bank: e.matmul(PS[bank][:, 0:NMEM], lhsT=Wb[:, kc, fc * 128:(fc + 1) * 128], rhs=memT[:, kc, :], start=(kc == 0), stop=(kc == KC - 1)),
                 reads=[b_Wb, b_memT], writes=[PSB[bank]])
        k.op("act", lambda e, fc=fc, bank=bank: e.activation(out=KmT[:, fc, :], in_=PS[bank][:, 0:NMEM], func=AF.Copy), reads=[PSB[bank]], writes=[b_KmT])
    load_w(Wb, b_Wb, w_ckv, D, 2 * D)
    for mt in range(2):
        for n4 in range(4):
            bank = 2 + n4 % 2
            for kc in range(KC):
                k.op("pe", lambda e, kc=kc, mt=mt, n4=n4, bank=bank: e.matmul(PS[bank][:, :], lhsT=memT[:, kc, mt * 128:(mt + 1) * 128], rhs=Wb[:, kc, n4 * 512:(n4 + 1) * 512], start=(kc == 0), stop=(kc == KC - 1)),
                     reads=[b_Wb, b_memT], writes=[PSB[bank]])
            k.op("act", lambda e, mt=mt, n4=n4, bank=bank: e.activation(out=Vm[:, mt, n4 * 512:(n4 + 1) * 512], in_=PS[bank][:, :], func=AF.Copy), reads=[PSB[bank]], writes=[b_Vm])
    load_w(Wb, b_Wb, w_cq, 0, D)
    k.op("sp", lambda e: e.dma_start(out=gv[:, :], in_=g_cross[:, :]), writes=[b_gv], dma=True)
    qcT = catT; b_qcT = b_catT
    CS = 512.0 ** -0.5
    for j in range(NOWN):
        norm_transpose_supertile(j, gv, b_gv, nT0, b_nT0, lambda tt, j=j: h_d[(j * 4 + tt) * 128:(j * 4 + tt + 1) * 128, :], b_src=B_h[j * 4:(j + 1) * 4])
        for fc in range(KC):
            bank = 2 + fc % 2
            for kc in range(KC):
                k.op("pe", lambda e, kc=kc, fc=fc, bank=bank: e.matmul(PS[bank][:, :], lhsT=Wb[:, kc, fc * 128:(fc + 1) * 128], rhs=nT0[:, kc, :], start=(kc == 0), stop=(kc == KC - 1)),
                     reads=[b_Wb, b_nT0], writes=[PSB[bank]])
            k.op("act", lambda e, fc=fc, bank=bank: e.activation(out=qcT[:, fc, :], in_=PS[bank][:, :], func=AF.Copy), reads=[PSB[bank]], writes=[b_qcT])
        ocT = nT0; b_ocT = b_nT0
        for hc in range(4):
            for mc in range(2):
                bank = 4 + mc
                for dc in range(4):
                    k.op("pe", lambda e, hc=hc, mc=mc, dc=dc, bank=bank: e.matmul(PS[bank][:, :], lhsT=KmT[:, 4 * hc + dc, mc * 128:(mc + 1) * 128], rhs=qcT[:, 4 * hc + dc, :], start=(dc == 0), stop=(dc == 3)),
                         reads=[b_KmT, b_qcT], writes=[PSB[bank]])
                k.op("act", lambda e, mc=mc, bank=bank: e.activation(out=pc[mc][:, :], in_=PS[bank][:, :], func=AF.Exp, scale=CS), reads=[PSB[bank]], writes=[b_pc[mc]])
            for mc in range(2):
                k.op("pe", lambda e, mc=mc: e.matmul(PS[6][:, :], lhsT=ones_bf[:, :], rhs=pc[mc][:, :], start=(mc == 0), stop=(mc == 1)), reads=[b_ones, b_pc[mc]], writes=[PSB[6]])
            k.op("dve", lambda e: e.reciprocal(out=rinv[:, :], in_=PS[6][:, :]), reads=[PSB[6]], writes=[b_rinv])
            for dvc in range(4):
                bank = dvc % 2
                for mc in range(2):
                    k.op("pe", lambda e, hc=hc, mc=mc, dvc=dvc, bank=bank: e.matmul(PS[bank][:, :], lhsT=Vm[:, mc, hc * 512 + dvc * 128:hc * 512 + (dvc + 1) * 128], rhs=pc[mc][:, :], start=(mc == 0), stop=(mc == 1)),
                         reads=[b_Vm, b_pc[mc]], writes=[PSB[bank]])
                k.op("dve", lambda e, hc=hc, dvc=dvc, bank=bank: e.tensor_tensor(out=ocT[:, 4 * hc + dvc, :], in0=PS[bank][:, :], in1=rinv[:, :], op=ALU.mult), reads=[PSB[bank], b_rinv], writes=[b_ocT])
        for kc in range(KC):
            k.op("sp", lambda e, kc=kc, j=j: e.dma_start(out=oc_d[kc * 128:(kc + 1) * 128, j * ST:(j + 1) * ST], in_=ocT[:, kc, :]), reads=[b_ocT], writes=[B_oc[j]], dma=True)

    load_w(Wb, b_Wb, w_co, 0, D)
    for j in range(NOWN):
        for kc in range(KC):
            k.op("sp", lambda e, kc=kc, j=j: e.dma_start(out=catT[:, kc, :], in_=oc_d[kc * 128:(kc + 1) * 128, j * ST:(j + 1) * ST]), reads=[B_oc[j]], writes=[b_catT], dma=True)
        for tt in range(4):
            hi = hctr % 2
            hctr += 1
            ti = j * 4 + tt
            k.op("sp", lambda e, hi=hi, ti=ti: e.dma_start(out=xt[hi][:, :], in_=h_d[ti * 128:(ti + 1) * 128, :]), reads=[B_h[ti]], writes=[b_xt[hi]], dma=True)
            for n4 in range(4):
                bank = n4 % 2
                for kc in range(KC):
                    k.op("pe", lambda e, kc=kc, tt=tt, n4=n4, bank=bank: e.matmul(PS[bank][:, :], lhsT=catT[:, kc, tt * 128:(tt + 1) * 128], rhs=Wb[:, kc, n4 * 512:(n4 + 1) * 512], start=(kc == 0), stop=(kc == KC - 1)),
                         reads=[b_catT, b_Wb], writes=[PSB[bank]])
                k.op("dve", lambda e, hi=hi, n4=n4, bank=bank: e.tensor_tensor(out=ht[hi][:, n4 * 512:(n4 + 1) * 512], in0=PS[bank][:, :], in1=xt[hi][:, n4 * 512:(n4 + 1) * 512], op=ALU.add),
                     reads=[PSB[bank], b_xt[hi]], writes=[b_ht[hi]])
            k.op("sp", lambda e, hi=hi, ti=ti: e.dma_start(out=h_d[ti * 128:(ti + 1) * 128, :], in_=ht[hi][:, :]), reads=[b_ht[hi]], writes=[B_h[ti]], dma=True)
    k.barrier()
    ph3.close()

    ph4 = ExitStack()
    cur[0] = ph4
    gv = sb("gv", [128, D], F32); b_gv = Buf()
    gfin = sb("gfin", [128, D], F32); b_gfin = Buf()
    k.op("sp", lambda e: e.dma_start(out=gv[:, :], in_=g_ffn[:, :]), writes=[b_gv], dma=True)
    k.op("sp", lambda e: e.dma_start(out=gfin[:, :], in_=g_final[:, :]), writes=[b_gfin], dma=True)
    acc = sb("acc", [128, 4, D], F32); b_acc = [Buf() for _ in range(4)]
    nb = [sb("nb%d" % i, [128, D], BF) for i in range(2)]; b_nb = [Buf(), Buf()]
    ss = [sb("ss%d" % i, [128, 1], F32) for i in range(2)]; b_ss = [Buf(), Buf()]
    rstd = [sb("rstd%d" % i, [128, 1], F32) for i in range(2)]; b_rstd = [Buf(), Buf()]
    n2T = sb("n2T", [128, KC, ST], BF); b_n2T = Buf()
    Wr = sb("Wr", [128, KC, NEXP_], BF); b_Wr = Buf()
    k.op("pool", lambda e: e.dma_start(out=Wr[:, :, :], in_=w_router.rearrange("(kc p) n -> p kc n", p=128)), writes=[b_Wr], dma=True)
    brt = sb("brt", [128, NEXP_], F32); b_brt = Buf()
    k.op("sp", lambda e: e.dma_start(out=brt[:, :], in_=brt_d[:, :]), writes=[b_brt], dma=True)
    bgu = sb("bgu", [128, NEXP_, 32], F32); b_bgu = Buf()
    k.op("sp", lambda e: e.dma_start(out=bgu[:, :, :], in_=bgu_d[:, :, :]), writes=[b_bgu], dma=True)
    bdn = sb("bdn", [NEXP_, D], F32); b_bdn = Buf()
    k.op("sp", lambda e: e.dma_start(out=bdn[:, :], in_=bdown_d[:, :]), writes=[b_bdn], dma=True)
    logit = sb("logit", [128, NEXP_], F32); b_logit = Buf()
    top8 = sb("top8", [128, 8], F32); b_top8 = Buf()
    msk = sb("msk", [128, NEXP_], F32); b_msk = Buf()
    den = sb("den", [128, 2], F32); b_den = Buf()
    gates = sb("gates", [128, 4, NEXP_], F32); b_gates = [Buf() for _ in range(4)]
    gT = sb("gT", [NEXP_, 4, 128], F32); b_gT = Buf()
    wgu = [sb("wgu%d" % i, [128, KC, 2, 256], BF) for i in range(2)]; b_wgu = [Buf(), Buf()]
    wd = [sb("wd%d" % i, [128, KC, 512], BF) for i in range(2)]; b_wd = [Buf(), Buf()]
    actT = sb("actT", [128, KC, ST], BF); b_actT = [Buf() for _ in range(KC)]
    g32 = [sb("g32%d" % i, [128, ST], F32) for i in range(2)]; b_g32 = [Buf(), Buf()]
    sg = [sb("sg%d" % i, [128, ST], F32) for i in range(2)]; b_sg = [Buf(), Buf()]
    u32 = [sb("u32%d" % i, [128, ST], F32) for i in range(2)]; b_u32 = [Buf(), Buf()]
    wctr = 0
    dctr = 0
    ectr = 0
    for j in range(NOWN):
        for tt in range(4):
            ti = j * 4 + tt
            i = tt % 2
            k.op("sp", lambda e, tt=tt, ti=ti: e.dma_start(out=acc[:, tt, :], in_=h_d[ti * 128:(ti + 1) * 128, :]), reads=[B_h[ti]], writes=[b_acc[tt]], dma=True)
            rms_tile(acc[:, tt, :], b_acc[tt], gv[:, :], b_gv, nb[i][:, :], b_nb[i], nb[i][:, :], b_nb[i], ss[i][:, :], b_ss[i], rstd[i][:, :], b_rstd[i], EPS)
            transpose_tile(nb[i], b_nb[i], n2T, b_n2T, tt * 128)
        for tt in range(4):
            for kc in range(KC):
                k.op("pe", lambda e, kc=kc, tt=tt: e.matmul(PS[2][:, 0:NEXP_], lhsT=n2T[:, kc, tt * 128:(tt + 1) * 128], rhs=Wr[:, kc, :], start=(kc == 0), stop=(kc == KC - 1)),
                     reads=[b_n2T, b_Wr], writes=[PSB[2]])
            k.op("dve", lambda e: e.tensor_tensor(out=logit[:, :], in0=PS[2][:, 0:NEXP_], in1=brt[:, :], op=ALU.add), reads=[PSB[2], b_brt], writes=[b_logit])
            k.op("dve", lambda e: e.max(out=top8[:, :], in_=logit[:, :]), reads=[b_logit], writes=[b_top8])
            k.op("dve", lambda e: e.tensor_scalar(out=msk[:, :], in0=logit[:, :], scalar1=top8[:, 3:4], scalar2=None, op0=ALU.is_ge), reads=[b_logit, b_top8], writes=[b_msk])
            k.op("dve", lambda e: e.tensor_scalar(out=den[:, 0:1], in0=top8[:, 0:1], scalar1=-1.0, scalar2=None, op0=ALU.mult), reads=[b_top8], writes=[b_den])
            k.op("act", lambda e: e.activation(out=logit[:, :], in_=logit[:, :], func=AF.Exp, bias=den[:, 0:1]), reads=[b_logit, b_den], writes=[b_logit])
            k.op("dve", lambda e: e.tensor_tensor(out=msk[:, :], in0=msk[:, :], in1=logit[:, :], op=ALU.mult), reads=[b_msk, b_logit], writes=[b_msk])
            k.op("dve", lambda e: e.reduce_sum(out=den[:, 1:2], in_=msk[:, :], axis=AX.X), reads=[b_msk], writes=[b_den])
            k.op("dve", lambda e: e.reciprocal(out=den[:, 1:2], in_=den[:, 1:2]), reads=[b_den], writes=[b_den])
            k.op("dve", lambda e, tt=tt: e.tensor_scalar(out=gates[:, tt, :], in0=msk[:, :], scalar1=den[:, 1:2], scalar2=None, op0=ALU.mult), reads=[b_msk, b_den], writes=[b_gates[tt]])
            k.op("pe", lambda e, tt=tt: e.transpose(out=PS[3][0:NEXP_, 0:128], in_=gates[:, tt, :], identity=identf[:, :]), reads=[b_gates[tt], b_identf], writes=[PSB[3]])
            k.op("act", lambda e, tt=tt: e.activation(out=gT[:, tt, :], in_=PS[3][0:NEXP_, 0:128], func=AF.Copy), reads=[PSB[3]], writes=[b_gT])
        for tt in range(4):
            for n4 in range(4):
                bank = 2 + n4 % 2
                k.op("pe", lambda e, tt=tt, n4=n4, bank=bank: e.matmul(PS[bank][:, :], lhsT=gT[:, tt, :], rhs=bdn[:, n4 * 512:(n4 + 1) * 512], start=True, stop=True), reads=[b_gT, b_bdn], writes=[PSB[bank]])
                k.op("dve", lambda e, tt=tt, n4=n4, bank=bank: e.tensor_tensor(out=acc[:, tt, n4 * 512:(n4 + 1) * 512], in0=PS[bank][:, :], in1=acc[:, tt, n4 * 512:(n4 + 1) * 512], op=ALU.add),
                     reads=[PSB[bank], b_acc[tt]], writes=[b_acc[tt]])
        chunks = []
        for ex in range(NEXP_):
            for fcb in range(8):
                chunks.append((ex, "gu", fcb))
            for dc in range(4):
                chunks.append((ex, "d", dc))
        cbuf = {}

        def issue(ch):
            nonlocal wctr, dctr
            ex, kind, idx = ch
            if kind == "gu":
                wi = wctr % 2
                wctr += 1
                cbuf[ch] = wi
                wguv = wgu_b[ex].rearrange("(kc p) n -> p kc n", p=128)
                for gu in range(2):
                    k.op("sp", lambda e, wi=wi, gu=gu, idx=idx, wguv=wguv: e.dma_start(out=wgu[wi][:, :, gu, :], in_=wguv[:, :, gu * D + idx * 256:gu * D + (idx + 1) * 256]),
                         reads=B_wgub[ex], writes=[b_wgu[wi]], dma=True)
            else:
                di = dctr % 2
                dctr += 1
                cbuf[ch] = di
                wdv = wd_b[ex].rearrange("(fc p) n -> p fc n", p=128)
                k.op("sp", lambda e, di=di, idx=idx, wdv=wdv: e.dma_start(out=wd[di][:, :, :], in_=wdv[:, :, idx * 512:(idx + 1) * 512]), reads=B_wdb[ex], writes=[b_wd[di]], dma=True)

        def compute(ch):
            nonlocal ectr
            ex, kind, idx = ch
            if kind == "gu":
                wi = cbuf[ch]
                fcb = idx
                for f2 in range(2):
                    fc = fcb * 2 + f2
                    ei = ectr % 2
                    ectr += 1
                    for gu in range(2):
                        bank = 4 + 2 * gu + ei
                        for kc in range(KC):
                            k.op("pe", lambda e, kc=kc, wi=wi, gu=gu, f2=f2, bank=bank: e.matmul(PS[bank][:, :], lhsT=wgu[wi][:, kc, gu, f2 * 128:(f2 + 1) * 128], rhs=n2T[:, kc, :], start=(kc == 0), stop=(kc == KC - 1)),
                                 reads=[b_wgu[wi], b_n2T], writes=[PSB[bank]])
                    bg, bu = 4 + ei, 6 + ei
                    k.op("dve", lambda e, ei=ei, bg=bg, ex=ex, fc=fc: e.tensor_scalar(out=g32[ei][:, :], in0=PS[bg][:, :], scalar1=bgu[:, ex, fc:fc + 1], scalar2=7.0, op0=ALU.add, op1=ALU.min),
                         reads=[PSB[bg], b_bgu], writes=[b_g32[ei]])
                    k.op("act", lambda e, ei=ei: e.activation(out=sg[ei][:, :], in_=g32[ei][:, :], func=AF.Sigmoid, scale=1.702), reads=[b_g32[ei]], writes=[b_sg[ei]])
                    k.op("dve", lambda e, ei=ei, bu=bu, ex=ex, fc=fc: e.tensor_scalar(out=u32[ei][:, :], in0=PS[bu][:, :], scalar1=bgu[:, ex, 16 + fc:16 + fc + 1], scalar2=7.0, op0=ALU.add, op1=ALU.min),
                         reads=[PSB[bu], b_bgu], writes=[b_u32[ei]])
                    k.op("pool", lambda e, ei=ei: e.tensor_scalar(out=u32[ei][:, :], in0=u32[ei][:, :], scalar1=-7.0, scalar2=1.0, op0=ALU.max, op1=ALU.add), reads=[b_u32[ei]], writes=[b_u32[ei]])
                    k.op("pool", lambda e, ei=ei: e.tensor_tensor(out=g32[ei][:, :], in0=g32[ei][:, :], in1=sg[ei][:, :], op=ALU.mult), reads=[b_g32[ei], b_sg[ei]], writes=[b_g32[ei]])
                    k.op("pool", lambda e, ei=ei, fc=fc: e.tensor_tensor(out=actT[:, fc, :], in0=g32[ei][:, :], in1=u32[ei][:, :], op=ALU.mult), reads=[b_g32[ei], b_u32[ei]], writes=[b_actT[fc]])
            else:
                di = cbuf[ch]
                dc = idx
                for tt in range(4):
                    bank = (tt % 2)
                    for fc in range(KC):
                        k.op("pe", lambda e, fc=fc, tt=tt, di=di, bank=bank: e.matmul(PS[bank][:, :], lhsT=actT[:, fc, tt * 128:(tt + 1) * 128], rhs=wd[di][:, fc, :], start=(fc == 0), stop=(fc == KC - 1)),
                             reads=[b_actT[fc], b_wd[di]], writes=[PSB[bank]])
                    k.op("dve", lambda e, tt=tt, dc=dc, ex=ex, bank=bank: e.scalar_tensor_tensor(out=acc[:, tt, dc * 512:(dc + 1) * 512], in0=PS[bank][:, :], scalar=gates[:, tt, ex:ex + 1], in1=acc[:, tt, dc * 512:(dc + 1) * 512], op0=ALU.mult, op1=ALU.add),
                         reads=[PSB[bank], b_gates[tt], b_acc[tt]], writes=[b_acc[tt]])

        issue(chunks[0])
        for ci in range(len(chunks)):
            if ci + 1 < len(chunks):
                issue(chunks[ci + 1])
            compute(chunks[ci])
        for tt in range(4):
            i = tt % 2
            ti = j * 4 + tt
            k.op("act", lambda e, tt=tt, i=i: e.activation(out=nb[i][:, :], in_=acc[:, tt, :], func=AF.Square, accum_out=ss[i][:, :]), reads=[b_acc[tt]], writes=[b_nb[i], b_ss[i]])
            k.op("act", lambda e, i=i: e.activation(out=rstd[i][:, :], in_=ss[i][:, :], func=AF.Sqrt, scale=1.0 / D, bias=epst[EPS]), reads=[b_ss[i]], writes=[b_rstd[i]])
            k.op("dve", lambda e, i=i: e.reciprocal(out=rstd[i][:, :], in_=rstd[i][:, :]), reads=[b_rstd[i]], writes=[b_rstd[i]])
            k.op("dve", lambda e, tt=tt, i=i: e.scalar_tensor_tensor(out=acc[:, tt, :], in0=acc[:, tt, :], scalar=rstd[i][:, :], in1=gfin[:, :], op0=ALU.mult, op1=ALU.mult),
                 reads=[b_acc[tt], b_rstd[i], b_gfin], writes=[b_acc[tt]])
            k.op("sp", lambda e, tt=tt, ti=ti: e.dma_start(out=out_d[ti * 128:(ti + 1) * 128, :], in_=acc[:, tt, :]), reads=[b_acc[tt]], writes=[B_out], dma=True)
        k.barrier()
    ph4.close()
    stack_outer.close()
    return nc, k, None


def _bf(a):
    return np.ascontiguousarray(a).astype(ml_dtypes.bfloat16)


def prep_inputs(inp, SEQ):
    OWN = SEQ // NCORES
    NKT = SEQ // 128
    f = lambda a: np.ascontiguousarray(np.asarray(a, dtype=np.float32))
    x = f(inp["x"])[0]
    rep = lambda v: np.ascontiguousarray(np.broadcast_to(f(v).reshape(1, -1), (128, f(v).size)))
    shared = {
        "mem": f(inp["mem"])[0],
        "w_in": f(inp["w_in"])[0], "w_out": f(inp["w_out"])[0], "w_cq": f(inp["w_cq"])[0],
        "w_ckv": f(inp["w_ckv"])[0], "w_co": f(inp["w_co"])[0], "w_router": f(inp["w_router"])[0],
        "w_gu": f(inp["w_gate_up"])[0], "w_down": f(inp["w_down"])[0],
        "w_rga": f(inp["w_rg_a"])[0], "w_rgx": f(inp["w_rg_x"])[0],
        "g_mix": rep(inp["norm_mix_g"][0]), "g_cross": rep(inp["norm_cross_g"][0]), "g_mem": rep(inp["norm_mem_g"][0]),
        "g_ffn": rep(inp["norm_ffn_g"][0]), "g_final": rep(inp["norm_final_g"]),
        "convw": np.ascontiguousarray(f(inp["conv_w"])[0].reshape(4, 8, 128).transpose(2, 1, 0)),
        "recv": np.ascontiguousarray(np.stack([f(inp[n])[0].reshape(8, 128) for n in ("conv_b", "b_rg_a", "b_rg_x", "rg_lambda", "rec_norm_g")], 0).transpose(2, 0, 1)),
        "lamv": np.ascontiguousarray(np.broadcast_to(np.stack([f(inp[n])[0] for n in ("lambda_q1", "lambda_k1", "lambda_q2", "lambda_k2")], 0)[None], (128, 4, 128))),
        "subg": np.ascontiguousarray(f(inp["subln_g"])[0].reshape(2, 128).T),
        "brt": rep(inp["b_router"][0]),
        "bgu": np.ascontiguousarray(f(inp["b_gate_up"])[0].reshape(-1, 32, 128).transpose(2, 0, 1)),
        "bdown": f(inp["b_down"])[0],
        "ident": _bf(np.eye(128, dtype=np.float32)),
        "identf": np.eye(128, dtype=np.float32),
    }
    kk = np.arange(128)[:, None, None]
    kr = np.arange(4)[None, :, None]
    qq = np.arange(ST)[None, None, :]
    shared["dmask"] = _bf(((kr * 128 + kk) <= qq).astype(np.float32))
    maps = []
    for c in range(NCORES):
        npad = SEQ - OWN * (c + 1)
        xs = np.zeros((SEQ, D), np.float32)
        xs[npad:] = x[: OWN * (c + 1)]
        valid = (np.arange(SEQ) >= npad).astype(np.float32)
        m = dict(shared)
        m["xseq"] = xs
        m["validrow"] = np.ascontiguousarray(np.broadcast_to(valid[None, :], (128, SEQ)))
        m["kbias"] = np.ascontiguousarray(((valid - 1.0) * 30000.0).reshape(NKT, 128).T)
        maps.append(m)
    return maps


def kernel(**inputs):
    SEQ = 16384
    nc, k, L = build(SEQ)
    maps = prep_inputs(inputs, SEQ)
    res = run_bass_kernel_spmd(nc, maps, core_ids=list(range(NCORES)))
    out = np.concatenate([np.asarray(r["out"]) for r in res.results], 0)[None]
    return np.ascontiguousarray(out.astype(np.float32))
```

```python
import math
from contextlib import ExitStack
import numpy as np
import ml_dtypes
import concourse.bass as bass
import concourse.mybir as mybir
from concourse.bass_utils import run_bass_kernel_spmd

F32 = mybir.dt.float32
BF = mybir.dt.bfloat16
AF = mybir.ActivationFunctionType
ALU = mybir.AluOpType
AX = mybir.AxisListType

D = 2048
KC = 16
ST = 512
NCORES = 8
NEXP = 32
NMEM = 256
EPS = 1e-6
DA_EPS = 1e-5
LAMBDA_INIT = 0.8 - 0.6 * math.exp(0.0)


class Buf:
    __slots__ = ("w", "r")

    def __init__(self):
        self.w = None
        self.r = {}


class EngQ:
    def __init__(self, name, sem, dsems):
        self.name = name
        self.sem = sem
        self.cnt = 0
        self.items = []
        self.dsems = dsems
        self.dn = 0
        self.waited = {}


class K:
    def __init__(self, nc, nds=6):
        self.nc = nc
        self.epoch = 0
        self.q = {}
        self.h = {"pe": nc.tensor, "act": nc.scalar, "dve": nc.vector, "pool": nc.gpsimd, "sp": nc.sync}
        for name in ("pe", "act", "dve", "pool", "sp"):
            sem = nc.alloc_semaphore("prog_" + name)
            dsems = []
            if name in ("sp", "pool", "act"):
                dsems = [nc.alloc_semaphore("dma_%s_%d" % (name, i)) for i in range(nds)]
            self.q[name] = EngQ(name, sem, dsems)

    def _waits(self, e, deps):
        out = []
        for (sem, val, src) in deps:
            if src == "pe" and e.name == "pe":
                continue
            key = id(sem)
            if e.waited.get(key, (None, 0))[1] >= val:
                continue
            e.waited[key] = (sem, val)
            out.append((sem, val))
        return out

    def op(self, eng, fn, reads=(), writes=(), dma=False):
        e = self.q[eng]
        deps = []
        for b in reads:
            if b.w is not None:
                deps.append(b.w)
        for b in writes:
            if b.w is not None:
                deps.append(b.w)
            deps.extend(b.r.values())
        if dma:
            ns = len(e.dsems)
            slot = e.dn % ns
            rnd = e.dn // ns
            e.dn += 1
            sem = e.dsems[slot]
            if rnd > 0:
                deps.append((sem, 16 * rnd, "dma"))
            tok = (sem, 16 * (rnd + 1), "dma")
            inc = (sem, 16)
        else:
            e.cnt += 1
            tok = (e.sem, e.cnt, eng)
            inc = (e.sem, 1)
        waits = self._waits(e, deps)
        h = self.h[eng]
        for (wsem, wval) in waits:
            h.wait_ge(wsem, wval)
        fn(h).then_inc(inc[0], inc[1])
        for b in reads:
            k = id(tok[0])
            if k not in b.r or b.r[k][1] < tok[1]:
                b.r[k] = tok
        for b in writes:
            b.w = tok
            b.r = {}
        return tok

    def all_tokens(self):
        toks = []
        for e in self.q.values():
            if e.cnt > 0:
                toks.append((e.sem, e.cnt, e.name))
            ns = len(e.dsems)
            for slot in range(min(ns, e.dn)):
                n_uses = (e.dn - slot + ns - 1) // ns
                toks.append((e.dsems[slot], 16 * n_uses, "dma"))
        return toks

    def barrier(self, engines=("pe", "act", "dve", "pool", "sp")):
        toks = self.all_tokens()
        for name in engines:
            e = self.q[name]
            waits = []
            for (sem, val, src) in toks:
                key = id(sem)
                if e.waited.get(key, (None, 0))[1] >= val:
                    continue
                e.waited[key] = (sem, val)
                waits.append((sem, val))
            for (wsem, wval) in waits:
                self.h[name].wait_ge(wsem, wval)
        self.epoch += 1
        for name in engines:
            e = self.q[name]
            if e.cnt > 0:
                e.sem = self.nc.alloc_semaphore("prog%d_%s" % (self.epoch, name))
                e.cnt = 0


def build(SEQ, dbg=False, nexp=NEXP):
    NEXP_ = nexp
    OWN = SEQ // NCORES
    NST = SEQ // ST
    NOWN = OWN // ST
    S0 = NST - NOWN
    NKT = SEQ // 128

    nc = bass.Bass("TRN2", target_bir_lowering=False)

    def din(name, shape, dt=F32):
        return nc.dram_tensor(name, list(shape), dt, kind="ExternalInput").ap()

    xseq = din("xseq", [SEQ, D])
    validrow = din("validrow", [128, SEQ])
    kbias_d = din("kbias", [128, NKT])
    mem_d = din("mem", [NMEM, D])
    w_in = din("w_in", [D, 5120])
    w_out = din("w_out", [D, D])
    w_cq = din("w_cq", [D, D])
    w_ckv = din("w_ckv", [D, 2 * D])
    w_co = din("w_co", [D, D])
    w_router = din("w_router", [D, NEXP_])
    w_gu = din("w_gu", [NEXP_, D, 2 * D])
    w_down = din("w_down", [NEXP_, D, D])
    w_rga = din("w_rga", [8, 128, 128])
    w_rgx = din("w_rgx", [8, 128, 128])
    g_mix = din("g_mix", [128, D])
    g_cross = din("g_cross", [128, D])
    g_mem = din("g_mem", [128, D])
    g_ffn = din("g_ffn", [128, D])
    g_final = din("g_final", [128, D])
    convw_d = din("convw", [128, 8, 4])
    recv_d = din("recv", [128, 5, 8])
    lamv_d = din("lamv", [128, 4, 128])
    subg_d = din("subg", [128, 2])
    brt_d = din("brt", [128, NEXP_])
    bgu_d = din("bgu", [128, NEXP_, 32])
    bdown_d = din("bdown", [NEXP_, D])
    ident_d = din("ident", [128, 128], BF)
    identf_d = din("identf", [128, 128])
    dmask_d = din("dmask", [128, 4, ST], BF)

    out_d = nc.dram_tensor("out", [OWN, D], F32, kind="ExternalOutput").ap()

    skind = "ExternalOutput" if dbg else "Internal"

    def dscr(name, shape, dt):
        return nc.dram_tensor(name, list(shape), dt, kind=skind).ap()

    kT_d = dscr("kT_s", [1024, SEQ], BF)
    v_d = dscr("v_s", [SEQ, 1024], BF)
    hown_d = dscr("hown_s", [1024, OWN], F32)
    qT_d = dscr("qT_s", [1024, OWN], BF)
    cat_d = dscr("cat_s", [D, OWN], BF)
    h_d = dscr("h_s", [OWN, D], F32)
    oc_d = dscr("oc_s", [D, OWN], BF)

    wgu_b = [nc.dram_tensor("wgu_b%d" % ex, [D, 2 * D], BF, kind="Internal").ap() for ex in range(NEXP_)]
    wd_b = [nc.dram_tensor("wd_b%d" % ex, [D, D], BF, kind="Internal").ap() for ex in range(NEXP_)]
    k = K(nc)
    B_wgub = [[Buf() for _ in range(4)] for _ in range(NEXP_)]
    B_wdb = [[Buf() for _ in range(2)] for _ in range(NEXP_)]
    cast_list = []
    for ex in range(NEXP_):
        for a in range(4):
            cast_list.append((wgu_b[ex][a * 512:(a + 1) * 512, :].rearrange("(r p) n -> p r n", p=128),
                              w_gu[ex, a * 512:(a + 1) * 512, :].rearrange("(r p) n -> p r n", p=128), B_wgub[ex][a]))
        for a in range(2):
            cast_list.append((wd_b[ex][a * 1024:(a + 1) * 1024, :].rearrange("(r p) n -> p r n", p=128),
                              w_down[ex, a * 1024:(a + 1) * 1024, :].rearrange("(r p) n -> p r n", p=128), B_wdb[ex][a]))
    cast_pos = [0]

    def cast_some(n):
        for _ in range(n):
            if cast_pos[0] >= len(cast_list):
                return
            dst, srcap, bf_ = cast_list[cast_pos[0]]
            cast_pos[0] += 1
            k.op("pool", lambda e, dst=dst, srcap=srcap: e.dma_start(out=dst, in_=srcap), writes=[bf_], dma=True)

    B_kT = [[Buf() for _ in range(NST)] for _ in range(8)]
    B_v = [Buf() for _ in range(NKT)]
    B_hown = [[Buf() for _ in range(NOWN)] for _ in range(8)]
    B_qT = [[Buf() for _ in range(NOWN)] for _ in range(8)]
    B_cat = [[Buf() for _ in range(NOWN)] for _ in range(KC)]
    B_h = [Buf() for _ in range(OWN // 128)]
    B_oc = [Buf() for _ in range(NOWN)]
    B_out = Buf()

    stack_outer = ExitStack()
    cur = [stack_outer]

    sbn = [0]

    def sb(name, shape, dt):
        sbn[0] += 1
        return cur[0].enter_context(nc.sbuf_tensor("sb%d_%s" % (sbn[0], name), list(shape), dt))

    ident = sb("ident", [128, 128], BF); b_ident = Buf()
    identf = sb("identf", [128, 128], F32); b_identf = Buf()
    ones_bf = sb("ones_bf", [128, 128], BF); b_ones = Buf()
    k.op("sp", lambda e: e.dma_start(out=ident[:, :], in_=ident_d[:, :]), writes=[b_ident], dma=True)
    k.op("sp", lambda e: e.dma_start(out=identf[:, :], in_=identf_d[:, :]), writes=[b_identf], dma=True)
    k.op("dve", lambda e: e.memset(ones_bf[:, :], 1.0), writes=[b_ones])
    epst = {}
    for ci, cv in enumerate((EPS, DA_EPS, 1.0)):
        tcst = sb("cst%d" % ci, [128, 1], F32)
        k.op("dve", lambda e, tcst=tcst, cv=cv: e.memset(tcst[:, :], cv), writes=[Buf()])
        epst[cv] = tcst[:, :]
    k.barrier()

    PS = [nc.alloc_psum_tensor("ps%d" % i, [128, 512], F32) for i in range(8)]
    PSB = [Buf() for _ in range(8)]

    def psbf(i):
        return PS[i][:, :].bitcast(BF)

    def load_w(dst, dst_buf, src2d, c0, c1, kcn=KC, dcol0=0):
        srcv = src2d.rearrange("(kc p) n -> p kc n", p=128)
        for kc in range(kcn):
            k.op("pool", lambda e, kc=kc: e.dma_start(out=dst[:, kc, dcol0:dcol0 + (c1 - c0)], in_=srcv[:, kc, c0:c1]),
                 writes=[dst_buf], dma=True)

    def rms_tile(xt_ap, b_x, grep, b_g, nb_ap, b_nb, junk, b_junk, ss, b_ss, rstd, b_rstd, eps=EPS):
        k.op("act", lambda e: e.activation(out=junk, in_=xt_ap, func=AF.Square, accum_out=ss),
             reads=[b_x], writes=[b_junk, b_ss])
        k.op("act", lambda e: e.activation(out=rstd, in_=ss, func=AF.Sqrt, scale=1.0 / D, bias=epst[eps]),
             reads=[b_ss], writes=[b_rstd])
        k.op("dve", lambda e: e.reciprocal(out=rstd, in_=rstd),
             reads=[b_rstd], writes=[b_rstd])
        k.op("dve", lambda e: e.scalar_tensor_tensor(out=nb_ap, in0=xt_ap, scalar=rstd, in1=grep, op0=ALU.mult, op1=ALU.mult),
             reads=[b_x, b_rstd, b_g], writes=[b_nb])

    def transpose_tile(nb_t, b_nb, nT, b_nT, col0, banks=(0, 1)):
        for half in range(2):
            bank = banks[half]
            pv = psbf(bank)
            for i in range(8):
                kc = half * 8 + i
                k.op("pe", lambda e, kc=kc, i=i, pv=pv: e.transpose(out=pv[:, i * 128:(i + 1) * 128], in_=nb_t[:, kc * 128:(kc + 1) * 128], identity=ident[:, :]),
                     reads=[b_nb, b_ident], writes=[PSB[bank]])
            k.op("act", lambda e, half=half, pv=pv: e.activation(out=nT[:, half * 8:(half + 1) * 8, col0:col0 + 128],
                                                                 in_=pv.rearrange("p (a b) -> p a b", a=8), func=AF.Copy),
                 reads=[PSB[bank]], writes=[b_nT])

    ph1 = ExitStack()
    cur[0] = ph1
    W1 = sb("W1", [128, KC, 3072], BF); b_W1 = Buf()
    load_w(W1, b_W1, w_in, 1024, 4096)
    gmix = sb("gmix", [128, D], F32); b_gmix = Buf()
    k.op("sp", lambda e: e.dma_start(out=gmix[:, :], in_=g_mix[:, :]), writes=[b_gmix], dma=True)
    wa = sb("wa", [128, 8, 128], BF); b_wa = Buf()
    wx = sb("wx", [128, 8, 128], BF); b_wx = Buf()
    k.op("pool", lambda e: e.dma_start(out=wa[:, :, :], in_=w_rga.rearrange("n c d -> c n d")), writes=[b_wa], dma=True)
    k.op("pool", lambda e: e.dma_start(out=wx[:, :, :], in_=w_rgx.rearrange("n c d -> c n d")), writes=[b_wx], dma=True)
    convw = sb("convw", [128, 8, 4], F32); b_convw = Buf()
    recv = sb("recv", [128, 5, 8], F32); b_recv = Buf()
    k.op("sp", lambda e: e.dma_start(out=convw[:, :, :], in_=convw_d[:, :, :]), writes=[b_convw], dma=True)
    k.op("sp", lambda e: e.dma_start(out=recv[:, :, :], in_=recv_d[:, :, :]), writes=[b_recv], dma=True)

    uu = sb("uu", [128, 8], F32); b_uu = Buf()
    pp = sb("pp", [128, 8], F32); b_pp = Buf()
    cneg = sb("cneg", [128, 8], F32); b_cneg = Buf()
    cneg2 = sb("cneg2", [128, 8], F32); b_cneg2 = Buf()
    k.op("act", lambda e: e.activation(out=uu[:, :], in_=recv[:, 3, :], func=AF.Exp, scale=-1.0), reads=[b_recv], writes=[b_uu])
    k.op("dve", lambda e: e.tensor_scalar(out=pp[:, :], in0=uu[:, :], scalar1=-1.0 / 8, scalar2=1.0 / 7, op0=ALU.mult, op1=ALU.add), reads=[b_uu], writes=[b_pp])
    for c in (6, 5, 4, 3, 2, 1):
        k.op("dve", lambda e: e.tensor_tensor(out=pp[:, :], in0=pp[:, :], in1=uu[:, :], op=ALU.mult), reads=[b_pp, b_uu], writes=[b_pp])
        k.op("dve", lambda e, c=c: e.tensor_scalar(out=pp[:, :], in0=pp[:, :], scalar1=-1.0, scalar2=1.0 / c, op0=ALU.mult, op1=ALU.add), reads=[b_pp], writes=[b_pp])
    k.op("dve", lambda e: e.tensor_tensor(out=pp[:, :], in0=pp[:, :], in1=uu[:, :], op=ALU.mult), reads=[b_pp, b_uu], writes=[b_pp])
    k.op("dve", lambda e: e.tensor_scalar(out=cneg[:, :], in0=pp[:, :], scalar1=-8.0, scalar2=None, op0=ALU.mult), reads=[b_pp], writes=[b_cneg])
    k.op("dve", lambda e: e.tensor_scalar(out=cneg2[:, :], in0=pp[:, :], scalar1=-16.0, scalar2=None, op0=ALU.mult), reads=[b_pp], writes=[b_cneg2])

    xt = [sb("xt%d" % i, [128, D], F32) for i in range(2)]; b_xt = [Buf(), Buf()]
    nb = [sb("nb%d" % i, [128, D], BF) for i in range(2)]; b_nb = [Buf(), Buf()]
    ss = [sb("ss%d" % i, [128, 1], F32) for i in range(2)]; b_ss = [Buf(), Buf()]
    rstd = [sb("rstd%d" % i, [128, 1], F32) for i in range(2)]; b_rstd = [Buf(), Buf()]
    nT0 = sb("nT0", [128, KC, ST], BF); nT = [nT0, nT0]; b_nT0 = Buf(); b_nT = [b_nT0, b_nT0]

    tile_ctr = [0]

    def norm_transpose_supertile(s, grep, b_g, nTbuf, b_nTbuf, src_rows, b_src=None, eps=EPS):
        for tt in range(4):
            i = tile_ctr[0] % 2
            tile_ctr[0] += 1
            rd = [b_src[tt]] if b_src is not None else []
            k.op("sp", lambda e, tt=tt, i=i: e.dma_start(out=xt[i][:, :], in_=src_rows(tt)), reads=rd, writes=[b_xt[i]], dma=True)
            rms_tile(xt[i][:, :], b_xt[i], grep[:, :], b_g, nb[i][:, :], b_nb[i], nb[i][:, :], b_nb[i], ss[i][:, :], b_ss[i], rstd[i][:, :], b_rstd[i], eps)
            transpose_tile(nb[i], b_nb[i], nTbuf, b_nTbuf, tt * 128)

    xrp = sb("xrp", [128, 8, 3 + ST], F32); b_xrp = [Buf() for _ in range(8)]
    k.op("pool", lambda e: e.memset(xrp[:, :, :], 0.0), writes=b_xrp)
    hlast = sb("hlast", [128, 8], F32); b_hlast = [Buf() for _ in range(8)]
    k.op("pool", lambda e: e.memset(hlast[:, :], 0.0), writes=b_hlast)
    NR = 1
    xc = [sb("xc%d" % i, [128, ST], F32) for i in range(NR)]; b_xc = [Buf() for _ in range(NR)]
    xcb = [sb("xcb%d" % i, [128, ST], BF) for i in range(NR)]; b_xcb = [Buf() for _ in range(NR)]
    rr = [sb("rr%d" % i, [128, ST], F32) for i in range(NR)]; b_rr = [Buf() for _ in range(NR)]
    ii = [sb("ii%d" % i, [128, ST], F32) for i in range(NR)]; b_ii = [Buf() for _ in range(NR)]
    aa = [sb("aa%d" % i, [128, ST], F32) for i in range(NR)]; b_aa = [Buf() for _ in range(NR)]
    a2 = [sb("a2%d" % i, [128, ST], F32) for i in range(NR)]; b_a2 = [Buf() for _ in range(NR)]
    bb = [sb("bb%d" % i, [128, ST], F32) for i in range(NR)]; b_bb = [Buf() for _ in range(NR)]
    hh = [sb("hh%d" % i, [128, ST], F32) for i in range(NR)]; b_hh = [Buf() for _ in range(NR)]
    vrow = [sb("vrow%d" % i, [128, ST], F32) for i in range(2)]; b_vrow = [Buf(), Buf()]
    kst = [sb("kst%d" % i, [128, ST], BF) for i in range(2)]; b_kst = [Buf(), Buf()]
    vst = [sb("vst%d" % i, [128, 1024], BF) for i in range(2)]; b_vst = [Buf(), Buf()]

    rec_ctr = [0]
    for s in range(NST):
        nTb, b_nTb = nT[s % 2], b_nT[s % 2]
        norm_transpose_supertile(s, gmix, b_gmix, nTb, b_nTb, lambda tt, s=s: xseq[s * ST + tt * 128: s * ST + (tt + 1) * 128, :])
        own = s >= S0
        if not own:
            vi = s % 2
            k.op("sp", lambda e, s=s, vi=vi: e.dma_start(out=vrow[vi][:, :], in_=validrow[:, s * ST:(s + 1) * ST]), writes=[b_vrow[vi]], dma=True)
        for m in range(8):
            bank = 2 + (m % 2)
            for kc in range(KC):
                k.op("pe", lambda e, kc=kc, m=m, bank=bank: e.matmul(PS[bank][:, :], lhsT=W1[:, kc, m * 128:(m + 1) * 128], rhs=nTb[:, kc, :], start=(kc == 0), stop=(kc == KC - 1)),
                     reads=[b_W1, b_nTb], writes=[PSB[bank]])
            ki = m % 2
            k.op("act", lambda e, ki=ki, bank=bank: e.activation(out=kst[ki][:, :], in_=PS[bank][:, :], func=AF.Copy), reads=[PSB[bank]], writes=[b_kst[ki]])
            k.op("sp", lambda e, ki=ki, m=m, s=s: e.dma_start(out=kT_d[m * 128:(m + 1) * 128, s * ST:(s + 1) * ST], in_=kst[ki][:, :]),
                 reads=[b_kst[ki]], writes=[B_kT[m][s]], dma=True)
        for tt in range(4):
            vi = tt % 2
            for half in range(2):
                bank = 2 + half
                for kc in range(KC):
                    k.op("pe", lambda e, kc=kc, tt=tt, half=half, bank=bank: e.matmul(PS[bank][:, :], lhsT=nTb[:, kc, tt * 128:(tt + 1) * 128], rhs=W1[:, kc, 1024 + half * 512:1024 + (half + 1) * 512], start=(kc == 0), stop=(kc == KC - 1)),
                         reads=[b_W1, b_nTb], writes=[PSB[bank]])
                k.op("act", lambda e, vi=vi, half=half, bank=bank: e.activation(out=vst[vi][:, half * 512:(half + 1) * 512], in_=PS[bank][:, :], func=AF.Copy), reads=[PSB[bank]], writes=[b_vst[vi]])
            kt = s * 4 + tt
            k.op("sp", lambda e, vi=vi, kt=kt: e.dma_start(out=v_d[kt * 128:(kt + 1) * 128, :], in_=vst[vi][:, :]), reads=[b_vst[vi]], writes=[B_v[kt]], dma=True)
        cast_some((len(cast_list) // 2 + NST - 1) // NST)
        for ct in range(8):
            ri = rec_ctr[0] % NR
            rec_ctr[0] += 1
            bank = 4 + (ct % 2)
            for kc in range(KC):
                k.op("pe", lambda e, kc=kc, ct=ct, bank=bank: e.matmul(PS[bank][:, :], lhsT=W1[:, kc, 2048 + ct * 128:2048 + (ct + 1) * 128], rhs=nTb[:, kc, :], start=(kc == 0), stop=(kc == KC - 1)),
                     reads=[b_W1, b_nTb], writes=[PSB[bank]])
            k.op("act", lambda e, ct=ct, bank=bank: e.activation(out=xrp[:, ct, 3:3 + ST], in_=PS[bank][:, :], func=AF.Copy), reads=[PSB[bank]], writes=[b_xrp[ct]])
            k.op("pool", lambda e, ct=ct, ri=ri: e.tensor_scalar(out=xc[ri][:, :], in0=xrp[:, ct, 0:ST], scalar1=convw[:, ct, 0:1], scalar2=recv[:, 0, ct:ct + 1], op0=ALU.mult, op1=ALU.add),
                 reads=[b_xrp[ct], b_convw, b_recv], writes=[b_xc[ri]])
            for j in (1, 2, 3):
                k.op("dve", lambda e, ct=ct, ri=ri, j=j: e.scalar_tensor_tensor(out=xc[ri][:, :], in0=xrp[:, ct, j:j + ST], scalar=convw[:, ct, j:j + 1], in1=xc[ri][:, :], op0=ALU.mult, op1=ALU.add),
                     reads=[b_xrp[ct], b_convw, b_xc[ri]], writes=[b_xc[ri]])
            k.op("pool", lambda e, ct=ct: e.tensor_copy(out=xrp[:, ct, 0:3], in_=xrp[:, ct, ST:ST + 3]), reads=[b_xrp[ct]], writes=[b_xrp[ct]])
            k.op("pool", lambda e, ri=ri: e.tensor_copy(out=xcb[ri][:, :], in_=xc[ri][:, :]), reads=[b_xc[ri]], writes=[b_xcb[ri]])
            k.op("pe", lambda e, ct=ct, ri=ri: e.matmul(PS[6][:, :], lhsT=wa[:, ct, :], rhs=xcb[ri][:, :], start=True, stop=True), reads=[b_wa, b_xcb[ri]], writes=[PSB[6]])
            k.op("pe", lambda e, ct=ct, ri=ri: e.matmul(PS[7][:, :], lhsT=wx[:, ct, :], rhs=xcb[ri][:, :], start=True, stop=True), reads=[b_wx, b_xcb[ri]], writes=[PSB[7]])
            k.op("act", lambda e, ct=ct, ri=ri: e.activation(out=rr[ri][:, :], in_=PS[6][:, :], func=AF.Sigmoid, bias=recv[:, 1, ct:ct + 1]), reads=[PSB[6], b_recv], writes=[b_rr[ri]])
            k.op("act", lambda e, ct=ct, ri=ri: e.activation(out=ii[ri][:, :], in_=PS[7][:, :], func=AF.Sigmoid, bias=recv[:, 2, ct:ct + 1]), reads=[PSB[7], b_recv], writes=[b_ii[ri]])
            k.op("act", lambda e, ct=ct, ri=ri: e.activation(out=aa[ri][:, :], in_=rr[ri][:, :], func=AF.Exp, scale=cneg[:, ct:ct + 1]), reads=[b_rr[ri], b_cneg], writes=[b_aa[ri]])
            k.op("act", lambda e, ct=ct, ri=ri: e.activation(out=a2[ri][:, :], in_=rr[ri][:, :], func=AF.Exp, scale=cneg2[:, ct:ct + 1]), reads=[b_rr[ri], b_cneg2], writes=[b_a2[ri]])
            k.op("act", lambda e, ri=ri: e.activation(out=a2[ri][:, :], in_=a2[ri][:, :], func=AF.Sqrt, scale=-1.0, bias=epst[1.0]), reads=[b_a2[ri]], writes=[b_a2[ri]])
            k.op("dve", lambda e, ri=ri: e.tensor_tensor(out=bb[ri][:, :], in0=ii[ri][:, :], in1=xc[ri][:, :], op=ALU.mult), reads=[b_ii[ri], b_xc[ri]], writes=[b_bb[ri]])
            k.op("dve", lambda e, ri=ri: e.tensor_tensor(out=bb[ri][:, :], in0=bb[ri][:, :], in1=a2[ri][:, :], op=ALU.mult), reads=[b_bb[ri], b_a2[ri]], writes=[b_bb[ri]])
            if not own:
                k.op("dve", lambda e, ri=ri, vi=s % 2: e.tensor_tensor(out=bb[ri][:, :], in0=bb[ri][:, :], in1=vrow[vi][:, :], op=ALU.mult), reads=[b_bb[ri], b_vrow[s % 2]], writes=[b_bb[ri]])
            k.op("dve", lambda e, ri=ri, ct=ct: e.tensor_tensor_scan(out=hh[ri][:, :], data0=aa[ri][:, :], data1=bb[ri][:, :], initial=hlast[:, ct:ct + 1], op0=ALU.mult, op1=ALU.add),
                 reads=[b_aa[ri], b_bb[ri], b_hlast[ct]], writes=[b_hh[ri]])
            k.op("dve", lambda e, ri=ri, ct=ct: e.tensor_copy(out=hlast[:, ct:ct + 1], in_=hh[ri][:, ST - 1:ST]), reads=[b_hh[ri]], writes=[b_hlast[ct]])
            if own:
                j = s - S0
                k.op("sp", lambda e, ri=ri, ct=ct, j=j: e.dma_start(out=hown_d[ct * 128:(ct + 1) * 128, j * ST:(j + 1) * ST], in_=hh[ri][:, :]), reads=[b_hh[ri]], writes=[B_hown[ct][j]], dma=True)

    k.barrier()

    W2 = W1
    b_W2 = Buf()
    load_w(W2, b_W2, w_in, 0, 1024, dcol0=0)
    load_w(W2, b_W2, w_in, 4096, 5120, dcol0=1024)
    ybuf = W1[:, 0:8, 2048:3072].bitcast(F32); b_ybuf = [b_W2 for _ in range(8)]
    ysq = W1[:, 8:16, 2048:2048 + ST]; b_ysq = [b_W2 for _ in range(8)]
    rs = a2[0]; b_rs = b_a2[0]
    ynb = [xcb[0], xcb[0]]; b_ynb = [b_xcb[0], b_xcb[0]]
    for j in range(NOWN):
        s = S0 + j
        nTb, b_nTb = nT[s % 2], b_nT[s % 2]
        norm_transpose_supertile(s, gmix, b_gmix, nTb, b_nTb, lambda tt, s=s: xseq[s * ST + tt * 128: s * ST + (tt + 1) * 128, :])
        for m in range(8):
            bank = 2 + (m % 2)
            for kc in range(KC):
                k.op("pe", lambda e, kc=kc, m=m, bank=bank: e.matmul(PS[bank][:, :], lhsT=W2[:, kc, m * 128:(m + 1) * 128], rhs=nTb[:, kc, :], start=(kc == 0), stop=(kc == KC - 1)),
                     reads=[b_W2, b_nTb], writes=[PSB[bank]])
            ki = m % 2
            k.op("act", lambda e, ki=ki, bank=bank: e.activation(out=kst[ki][:, :], in_=PS[bank][:, :], func=AF.Copy), reads=[PSB[bank]], writes=[b_kst[ki]])
            k.op("sp", lambda e, ki=ki, m=m, j=j: e.dma_start(out=qT_d[m * 128:(m + 1) * 128, j * ST:(j + 1) * ST], in_=kst[ki][:, :]),
                 reads=[b_kst[ki]], writes=[B_qT[m][j]], dma=True)
        for ct in range(8):
            ri = ct % NR
            bank = 4 + (ct % 2)
            for kc in range(KC):
                k.op("pe", lambda e, kc=kc, ct=ct, bank=bank: e.matmul(PS[bank][:, :], lhsT=W2[:, kc, 1024 + ct * 128:1024 + (ct + 1) * 128], rhs=nTb[:, kc, :], start=(kc == 0), stop=(kc == KC - 1)),
                     reads=[b_W2, b_nTb], writes=[PSB[bank]])
            k.op("act", lambda e, ri=ri, bank=bank: e.activation(out=xc[ri][:, :], in_=PS[bank][:, :], func=AF.Copy), reads=[PSB[bank]], writes=[b_xc[ri]])
            k.op("dve", lambda e, ri=ri: e.tensor_tensor(out=rr[ri][:, :], in0=xc[ri][:, :], in1=xc[ri][:, :], op=ALU.mult), reads=[b_xc[ri]], writes=[b_rr[ri]])
            k.op("dve", lambda e, ri=ri: e.tensor_scalar(out=rr[ri][:, :], in0=rr[ri][:, :], scalar1=0.044715, scalar2=1.0, op0=ALU.mult, op1=ALU.add), reads=[b_rr[ri]], writes=[b_rr[ri]])
            k.op("dve", lambda e, ri=ri: e.tensor_tensor(out=rr[ri][:, :], in0=rr[ri][:, :], in1=xc[ri][:, :], op=ALU.mult), reads=[b_rr[ri], b_xc[ri]], writes=[b_rr[ri]])
            k.op("act", lambda e, ri=ri: e.activation(out=ii[ri][:, :], in_=rr[ri][:, :], func=AF.Sigmoid, scale=1.5957691216057308), reads=[b_rr[ri]], writes=[b_ii[ri]])
            k.op("sp", lambda e, ri=ri, ct=ct, j=j: e.dma_start(out=hh[ri][:, :], in_=hown_d[ct * 128:(ct + 1) * 128, j * ST:(j + 1) * ST]), reads=[B_hown[ct][j]], writes=[b_hh[ri]], dma=True)
            k.op("dve", lambda e, ri=ri: e.tensor_tensor(out=bb[ri][:, :], in0=xc[ri][:, :], in1=ii[ri][:, :], op=ALU.mult), reads=[b_xc[ri], b_ii[ri]], writes=[b_bb[ri]])
            k.op("dve", lambda e, ri=ri, ct=ct: e.tensor_tensor(out=ybuf[:, ct, :], in0=bb[ri][:, :], in1=hh[ri][:, :], op=ALU.mult), reads=[b_bb[ri], b_hh[ri]], writes=[b_ybuf[ct]])
            k.op("act", lambda e, ct=ct: e.activation(out=ysq[:, ct, :], in_=ybuf[:, ct, :], func=AF.Square), reads=[b_ybuf[ct]], writes=[b_ysq[ct]])
        for ct in range(8):
            k.op("pe", lambda e, ct=ct: e.matmul(PS[6][:, :], lhsT=ones_bf[:, :], rhs=ysq[:, ct, :], start=(ct == 0), stop=(ct == 7)), reads=[b_ones, b_ysq[ct]], writes=[PSB[6]])
        k.op("act", lambda e: e.activation(out=rs[:, :], in_=PS[6][:, :], func=AF.Sqrt, scale=1.0 / 1024, bias=epst[EPS]), reads=[PSB[6]], writes=[b_rs])
        k.op("dve", lambda e: e.reciprocal(out=rs[:, :], in_=rs[:, :]), reads=[b_rs], writes=[b_rs])
        for ct in range(8):
            yi = ct % 2
            k.op("dve", lambda e, ct=ct, yi=yi: e.scalar_tensor_tensor(out=ynb[yi][:, :], in0=ybuf[:, ct, :], scalar=recv[:, 4, ct:ct + 1], in1=rs[:, :], op0=ALU.mult, op1=ALU.mult),
                 reads=[b_ybuf[ct], b_recv, b_rs], writes=[b_ynb[yi]])
            k.op("sp", lambda e, ct=ct, yi=yi, j=j: e.dma_start(out=cat_d[1024 + ct * 128:1024 + (ct + 1) * 128, j * ST:(j + 1) * ST], in_=ynb[yi][:, :]), reads=[b_ynb[yi]], writes=[B_cat[8 + ct][j]], dma=True)

    k.barrier()
    ph1.close()

    ph2 = ExitStack()
    cur[0] = ph2
    cast_some(len(cast_list))
    kT2 = sb("kT2", [128, 2, SEQ], BF); b_kT2 = [Buf(), Buf()]
    vv = sb("vv", [128, NKT, 256], BF); b_vv = Buf()
    qh = sb("qh", [128, 2, OWN], BF); b_qh = Buf()
    kbias = sb("kbias", [128, NKT], F32); b_kbias = Buf()
    dmask = sb("dmask", [128, 4, ST], BF); b_dmask = Buf()
    lamv = sb("lamv", [128, 4, 128], F32); b_lamv = Buf()
    subg = sb("subg", [128, 2], F32); b_subg = Buf()
    lam_t = sb("lam_t", [128, 4], F32); b_lam = Buf()
    prod = sb("prod", [128, 128], F32); b_prod = Buf()
    pT = [sb("pT%d" % i, [128, ST], BF) for i in range(3)]; b_pT = [Buf() for _ in range(3)]
    rinv = sb("rinv", [128, ST], F32); b_rinv = Buf()
    racc = sb("racc", [128, ST], F32); b_racc = Buf()
    ones_f = sb("ones_f", [128, 128], F32); b_onesf = Buf()
    k.op("dve", lambda e: e.memset(ones_f[:, :], 1.0), writes=[b_onesf])
    osb = sb("osb", [128, 2, 2, ST], F32); b_osb = [Buf(), Buf()]
    od = sb("od", [128, 2, ST], F32); b_od = Buf()
    osq = sb("osq", [128, 2, ST], BF); b_osq = Buf()
    rs2 = sb("rs2", [128, ST], F32); b_rs2 = Buf()
    onb = [sb("onb%d" % i, [128, ST], BF) for i in range(2)]; b_onb = [Buf(), Buf()]
    k.op("sp", lambda e: e.dma_start(out=kbias[:, :], in_=kbias_d[:, :]), writes=[b_kbias], dma=True)
    k.op("sp", lambda e: e.dma_start(out=dmask[:, :, :], in_=dmask_d[:, :, :]), writes=[b_dmask], dma=True)
    k.op("sp", lambda e: e.dma_start(out=lamv[:, :, :], in_=lamv_d[:, :, :]), writes=[b_lamv], dma=True)
    k.op("sp", lambda e: e.dma_start(out=subg[:, :], in_=subg_d[:, :]), writes=[b_subg], dma=True)
    for t in range(2):
        k.op("dve", lambda e, t=t: e.tensor_tensor(out=prod[:, :], in0=lamv[:, 2 * t, :], in1=lamv[:, 2 * t + 1, :], op=ALU.mult), reads=[b_lamv], writes=[b_prod])
        k.op("dve", lambda e, t=t: e.reduce_sum(out=lam_t[:, t:t + 1], in_=prod[:, :], axis=AX.X), reads=[b_prod], writes=[b_lam])
    k.op("act", lambda e: e.activation(out=lam_t[:, 0:2], in_=lam_t[:, 0:2], func=AF.Exp), reads=[b_lam], writes=[b_lam])
    k.op("dve", lambda e: e.tensor_tensor(out=lam_t[:, 2:3], in0=lam_t[:, 1:2], in1=lam_t[:, 0:1], op=ALU.subtract), reads=[b_lam], writes=[b_lam])
    k.op("dve", lambda e: e.tensor_scalar(out=lam_t[:, 2:3], in0=lam_t[:, 2:3], scalar1=-LAMBDA_INIT, scalar2=None, op0=ALU.add), reads=[b_lam], writes=[b_lam])
    k.op("dve", lambda e: e.tensor_scalar(out=subg[:, :], in0=subg[:, :], scalar1=1.0 - LAMBDA_INIT, scalar2=None, op0=ALU.mult), reads=[b_subg], writes=[b_subg])
    SCALE = 128.0 ** -0.5
    pctr = 0
    for hd in range(4):
        for m in range(2):
            gm = hd * 2 + m
            for s in range(NST):
                k.op("sp", lambda e, m=m, gm=gm, s=s: e.dma_start(out=kT2[:, m, s * ST:(s + 1) * ST], in_=kT_d[gm * 128:(gm + 1) * 128, s * ST:(s + 1) * ST]),
                     reads=[B_kT[gm][s]], writes=[b_kT2[m]], dma=True)
            k.op("sp", lambda e, m=m, gm=gm: e.dma_start(out=qh[:, m, :], in_=qT_d[gm * 128:(gm + 1) * 128, :]), reads=B_qT[gm], writes=[b_qh], dma=True)
        for s in range(NST):
            k.op("sp", lambda e, s=s, hd=hd: e.dma_start(out=vv[:, s * 4:(s + 1) * 4, :], in_=v_d[s * ST:(s + 1) * ST, hd * 256:(hd + 1) * 256].rearrange("(t p) c -> p t c", p=128)),
                 reads=B_v[s * 4:(s + 1) * 4], writes=[b_vv], dma=True)
        for j in range(NOWN):
            nkb = (S0 + j + 1) * 4
            for m in range(2):
                pis = []
                for kb in range(nkb):
                    pis.append(pctr % 3)
                    pctr += 1

                def emit_S(kb, m=m, j=j):
                    sbk = kb % 2
                    k.op("pe", lambda e, kb=kb, sbk=sbk: e.matmul(PS[sbk][:, :], lhsT=kT2[:, m, kb * 128:(kb + 1) * 128], rhs=qh[:, m, j * ST:(j + 1) * ST], start=True, stop=True),
                         reads=[b_kT2[m], b_qh], writes=[PSB[sbk]])

                emit_S(0)
                for kb in range(nkb):
                    sbk = kb % 2
                    pi = pis[kb]
                    if kb + 1 < nkb:
                        emit_S(kb + 1)
                    k.op("act", lambda e, kb=kb, sbk=sbk, pi=pi: e.activation(out=pT[pi][:, :], in_=PS[sbk][:, :], func=AF.Exp, scale=SCALE, bias=kbias[:, kb:kb + 1]),
                         reads=[PSB[sbk], b_kbias], writes=[b_pT[pi]])
                    kr = kb - (nkb - 4)
                    if kr >= 0:
                        k.op("dve", lambda e, pi=pi, kr=kr: e.tensor_tensor(out=pT[pi][:, :], in0=pT[pi][:, :], in1=dmask[:, kr, :], op=ALU.mult), reads=[b_pT[pi], b_dmask], writes=[b_pT[pi]])
                    for dvc in range(2):
                        k.op("pe", lambda e, kb=kb, dvc=dvc, pi=pi, nkb=nkb: e.matmul(PS[2 + dvc][:, :], lhsT=vv[:, kb, dvc * 128:(dvc + 1) * 128], rhs=pT[pi][:, :], start=(kb == 0), stop=(kb == nkb - 1)),
                             reads=[b_vv, b_pT[pi]], writes=[PSB[2 + dvc]])
                    if kb == 0:
                        k.op("dve", lambda e, pi=pi: e.tensor_copy(out=racc[:, :], in_=pT[pi][:, :]), reads=[b_pT[pi]], writes=[b_racc])
                    else:
                        k.op("dve", lambda e, pi=pi: e.tensor_tensor(out=racc[:, :], in0=racc[:, :], in1=pT[pi][:, :], op=ALU.add), reads=[b_pT[pi], b_racc], writes=[b_racc])
                k.op("pe", lambda e: e.matmul(PS[4][:, :], lhsT=ones_f[:, :], rhs=racc[:, :], start=True, stop=True), reads=[b_onesf, b_racc], writes=[PSB[4]])
                k.op("dve", lambda e: e.reciprocal(out=rinv[:, :], in_=PS[4][:, :]), reads=[PSB[4]], writes=[b_rinv])
                for dvc in range(2):
                    k.op("dve", lambda e, m=m, dvc=dvc: e.tensor_tensor(out=osb[:, m, dvc, :], in0=PS[2 + dvc][:, :], in1=rinv[:, :], op=ALU.mult), reads=[PSB[2 + dvc], b_rinv], writes=[b_osb[m]])
            k.op("dve", lambda e: e.scalar_tensor_tensor(out=od[:, :, :], in0=osb[:, 1, :, :], scalar=lam_t[:, 2:3], in1=osb[:, 0, :, :], op0=ALU.mult, op1=ALU.add),
                 reads=[b_osb[0], b_osb[1], b_lam], writes=[b_od])
            k.op("act", lambda e: e.activation(out=osq[:, :, :], in_=od[:, :, :], func=AF.Square), reads=[b_od], writes=[b_osq])
            for dvc in range(2):
                k.op("pe", lambda e, dvc=dvc: e.matmul(PS[5][:, :], lhsT=ones_bf[:, :], rhs=osq[:, dvc, :], start=(dvc == 0), stop=(dvc == 1)), reads=[b_ones, b_osq], writes=[PSB[5]])
            k.op("act", lambda e: e.activation(out=rs2[:, :], in_=PS[5][:, :], func=AF.Sqrt, scale=1.0 / 256, bias=epst[DA_EPS]), reads=[PSB[5]], writes=[b_rs2])
            k.op("dve", lambda e: e.reciprocal(out=rs2[:, :], in_=rs2[:, :]), reads=[b_rs2], writes=[b_rs2])
            for dvc in range(2):
                k.op("dve", lambda e, dvc=dvc: e.scalar_tensor_tensor(out=onb[dvc][:, :], in0=od[:, dvc, :], scalar=subg[:, dvc:dvc + 1], in1=rs2[:, :], op0=ALU.mult, op1=ALU.mult),
                     reads=[b_od, b_subg, b_rs2], writes=[b_onb[dvc]])
                kc = hd * 2 + dvc
                k.op("sp", lambda e, dvc=dvc, kc=kc, j=j: e.dma_start(out=cat_d[kc * 128:(kc + 1) * 128, j * ST:(j + 1) * ST], in_=onb[dvc][:, :]), reads=[b_onb[dvc]], writes=[B_cat[kc][j]], dma=True)
    k.barrier()
    ph2.close()

    ph3 = ExitStack()
    cur[0] = ph3
    Wb = sb("Wb", [128, KC, D], BF); b_Wb = Buf()
    gv = sb("gv", [128, D], F32); b_gv = Buf()
    xt = [sb("xt%d" % i, [128, D], F32) for i in range(2)]; b_xt = [Buf(), Buf()]
    nb = [sb("nb%d" % i, [128, D], BF) for i in range(2)]; b_nb = [Buf(), Buf()]
    ss = [sb("ss%d" % i, [128, 1], F32) for i in range(2)]; b_ss = [Buf(), Buf()]
    rstd = [sb("rstd%d" % i, [128, 1], F32) for i in range(2)]; b_rstd = [Buf(), Buf()]
    nT0 = sb("nT0", [128, KC, ST], BF); b_nT0 = Buf()
    catT = sb("catT", [128, KC, ST], BF); b_catT = Buf()
    ht = [sb("ht%d" % i, [128, D], F32) for i in range(2)]; b_ht = [Buf(), Buf()]
    memT = sb("memT", [128, KC, NMEM], BF); b_memT = Buf()
    KmT = sb("KmT", [128, KC, NMEM], BF); b_KmT = Buf()
    Vm = sb("Vm", [128, 2, D], BF); b_Vm = Buf()
    pc = [sb("pc%d" % i, [128, ST], BF) for i in range(2)]; b_pc = [Buf(), Buf()]
    rinv = sb("rinv", [128, ST], F32); b_rinv = Buf()

    load_w(Wb, b_Wb, w_out, 0, D)
    hctr = 0
    for j in range(NOWN):
        for kc in range(KC):
            k.op("sp", lambda e, kc=kc, j=j: e.dma_start(out=catT[:, kc, :], in_=cat_d[kc * 128:(kc + 1) * 128, j * ST:(j + 1) * ST]), reads=[B_cat[kc][j]], writes=[b_catT], dma=True)
        for tt in range(4):
            hi = hctr % 2
            hctr += 1
            row0 = (S0 + j) * ST + tt * 128
            k.op("sp", lambda e, hi=hi, row0=row0: e.dma_start(out=xt[hi][:, :], in_=xseq[row0:row0 + 128, :]), writes=[b_xt[hi]], dma=True)
            for n4 in range(4):
                bank = n4 % 2
                for kc in range(KC):
                    k.op("pe", lambda e, kc=kc, tt=tt, n4=n4, bank=bank: e.matmul(PS[bank][:, :], lhsT=catT[:, kc, tt * 128:(tt + 1) * 128], rhs=Wb[:, kc, n4 * 512:(n4 + 1) * 512], start=(kc == 0), stop=(kc == KC - 1)),
                         reads=[b_catT, b_Wb], writes=[PSB[bank]])
                k.op("dve", lambda e, hi=hi, n4=n4, bank=bank: e.tensor_tensor(out=ht[hi][:, n4 * 512:(n4 + 1) * 512], in0=PS[bank][:, :], in1=xt[hi][:, n4 * 512:(n4 + 1) * 512], op=ALU.add),
                     reads=[PSB[bank], b_xt[hi]], writes=[b_ht[hi]])
            ti = j * 4 + tt
            k.op("sp", lambda e, hi=hi, ti=ti: e.dma_start(out=h_d[ti * 128:(ti + 1) * 128, :], in_=ht[hi][:, :]), reads=[b_ht[hi]], writes=[B_h[ti]], dma=True)

    k.op("sp", lambda e: e.dma_start(out=gv[:, :], in_=g_mem[:, :]), writes=[b_gv], dma=True)
    tile_ctr[0] = 0
    for mt in range(2):
        i = mt
        k.op("sp", lambda e, mt=mt, i=i: e.dma_start(out=xt[i][:, :], in_=mem_d[mt * 128:(mt + 1) * 128, :]), writes=[b_xt[i]], dma=True)
        rms_tile(xt[i][:, :], b_xt[i], gv[:, :], b_gv, nb[i][:, :], b_nb[i], nb[i][:, :], b_nb[i], ss[i][:, :], b_ss[i], rstd[i][:, :], b_rstd[i], EPS)
        transpose_tile(nb[i], b_nb[i], memT, b_memT, mt * 128)
    load_w(Wb, b_Wb, w_ckv, 0, D)
    for fc in range(KC):
        bank = 2 + fc % 2
        for kc in range(KC):
            k.op("pe", lambda e, kc=kc, fc=fc, bank=bank: e.matmul(PS[bank][:, 0:NMEM], lhsT=Wb[:, kc, fc * 128:(fc + 1) * 128], rhs=memT[:, kc, :], start=(kc == 0), stop=(kc == KC - 1)),
                 reads=[b_Wb, b_memT], writes=[PSB[bank]])
        k.op("act", lambda e, fc=fc, bank=bank: e.activation(out=KmT[:, fc, :], in_=PS[bank][:, 0:NMEM], func=AF.Copy), reads=[PSB[bank]], writes=[b_KmT])
    load_w(Wb, b_Wb, w_ckv, D, 2 * D)
    for mt in range(2):
        for n4 in range(4):
            bank = 2 + n4 % 2
            for kc in range(KC):
                k.op("pe", lambda e, kc=kc, mt=mt, n4=n4, bank=bank: e.matmul(PS[bank][:, :], lhsT=memT[:, kc, mt * 128:(mt + 1) * 128], rhs=Wb[:, kc, n4 * 512:(n4 + 1) * 512], start=(kc == 0), stop=(kc == KC - 1)),
                     reads=[b_Wb, b_memT], writes=[PSB[bank]])
            k.op("act", lambda e, mt=mt, n4=n4, bank=bank: e.activation(out=Vm[:, mt, n4 * 512:(n4 + 1) * 512], in_=PS[bank][:, :], func=AF.Copy), reads=[PSB[bank]], writes=[b_Vm])
    load_w(Wb, b_Wb, w_cq, 0, D)
    k.op("sp", lambda e: e.dma_start(out=gv[:, :], in_=g_cross[:, :]), writes=[b_gv], dma=True)
    qcT = catT; b_qcT = b_catT
    CS = 512.0 ** -0.5
    for j in range(NOWN):
        norm_transpose_supertile(j, gv, b_gv, nT0, b_nT0, lambda tt, j=j: h_d[(j * 4 + tt) * 128:(j * 4 + tt + 1) * 128, :], b_src=B_h[j * 4:(j + 1) * 4])
        for fc in range(KC):
            bank = 2 + fc % 2
            for kc in range(KC):
                k.op("pe", lambda e, kc=kc, fc=fc, bank=bank: e.matmul(PS[bank][:, :], lhsT=Wb[:, kc, fc * 128:(fc + 1) * 128], rhs=nT0[:, kc, :], start=(kc == 0), stop=(kc == KC - 1)),
                     reads=[b_Wb, b_nT0], writes=[PSB[bank]])
            k.op("act", lambda e, fc=fc, bank=bank: e.activation(out=qcT[:, fc, :], in_=PS[bank][:, :], func=AF.Copy), reads=[PSB[bank]], writes=[b_qcT])
        ocT = nT0; b_ocT = b_nT0
        for hc in range(4):
            for mc in range(2):
                bank = 4 + mc
                for dc in range(4):
                    k.op("pe", lambda e, hc=hc, mc=mc, dc=dc, bank=bank: e.matmul(PS[bank][:, :], lhsT=KmT[:, 4 * hc + dc, mc * 128:(mc + 1) * 128], rhs=qcT[:, 4 * hc + dc, :], start=(dc == 0), stop=(dc == 3)),
                         reads=[b_KmT, b_qcT], writes=[PSB[bank]])
                k.op("act", lambda e, mc=mc, bank=bank: e.activation(out=pc[mc][:, :], in_=PS[bank][:, :], func=AF.Exp, scale=CS), reads=[PSB[bank]], writes=[b_pc[mc]])
            for mc in range(2):
                k.op("pe", lambda e, mc=mc: e.matmul(PS[6][:, :], lhsT=ones_bf[:, :], rhs=pc[mc][:, :], start=(mc == 0), stop=(mc == 1)), reads=[b_ones, b_pc[mc]], writes=[PSB[6]])
            k.op("dve", lambda e: e.reciprocal(out=rinv[:, :], in_=PS[6][:, :]), reads=[PSB[6]], writes=[b_rinv])
            for dvc in range(4):
                bank = dvc % 2
                for mc in range(2):
                    k.op("pe", lambda e, hc=hc, mc=mc, dvc=dvc, bank=bank: e.matmul(PS[bank][:, :], lhsT=Vm[:, mc, hc * 512 + dvc * 128:hc * 512 + (dvc + 1) * 128], rhs=pc[mc][:, :], start=(mc == 0), stop=(mc == 1)),
                         reads=[b_Vm, b_pc[mc]], writes=[PSB[bank]])
                k.op("dve", lambda e, hc=hc, dvc=dvc, bank=bank: e.tensor_tensor(out=ocT[:, 4 * hc + dvc, :], in0=PS[bank][:, :], in1=rinv[:, :], op=ALU.mult), reads=[PSB[bank], b_rinv], writes=[b_ocT])
        for kc in range(KC):
            k.op("sp", lambda e, kc=kc, j=j: e.dma_start(out=oc_d[kc * 128:(kc + 1) * 128, j * ST:(j + 1) * ST], in_=ocT[:, kc, :]), reads=[b_ocT], writes=[B_oc[j]], dma=True)

    load_w(Wb, b_Wb, w_co, 0, D)
    for j in range(NOWN):
        for kc in range(KC):
            k.op("sp", lambda e, kc=kc, j=j: e.dma_start(out=catT[:, kc, :], in_=oc_d[kc * 128:(kc + 1) * 128, j * ST:(j + 1) * ST]), reads=[B_oc[j]], writes=[b_catT], dma=True)
        for tt in range(4):
            hi = hctr % 2
            hctr += 1
            ti = j * 4 + tt
            k.op("sp", lambda e, hi=hi, ti=ti: e.dma_start(out=xt[hi][:, :], in_=h_d[ti * 128:(ti + 1) * 128, :]), reads=[B_h[ti]], writes=[b_xt[hi]], dma=True)
            for n4 in range(4):
                bank = n4 % 2
                for kc in range(KC):
                    k.op("pe", lambda e, kc=kc, tt=tt, n4=n4, bank=bank: e.matmul(PS[bank][:, :], lhsT=catT[:, kc, tt * 128:(tt + 1) * 128], rhs=Wb[:, kc, n4 * 512:(n4 + 1) * 512], start=(kc == 0), stop=(kc == KC - 1)),
                         reads=[b_catT, b_Wb], writes=[PSB[bank]])
                k.op("dve", lambda e, hi=hi, n4=n4, bank=bank: e.tensor_tensor(out=ht[hi][:, n4 * 512:(n4 + 1) * 512], in0=PS[bank][:, :], in1=xt[hi][:, n4 * 512:(n4 + 1) * 512], op=ALU.add),
                     reads=[PSB[bank], b_xt[hi]], writes=[b_ht[hi]])
            k.op("sp", lambda e, hi=hi, ti=ti: e.dma_start(out=h_d[ti * 128:(ti + 1) * 128, :], in_=ht[hi][:, :]), reads=[b_ht[hi]], writes=[B_h[ti]], dma=True)
    k.barrier()
    ph3.close()

    ph4 = ExitStack()
    cur[0] = ph4
    gv = sb("gv", [128, D], F32); b_gv = Buf()
    gfin = sb("gfin", [128, D], F32); b_gfin = Buf()
    k.op("sp", lambda e: e.dma_start(out=gv[:, :], in_=g_ffn[:, :]), writes=[b_gv], dma=True)
    k.op("sp", lambda e: e.dma_start(out=gfin[:, :], in_=g_final[:, :]), writes=[b_gfin], dma=True)
    acc = sb("acc", [128, 4, D], F32); b_acc = [Buf() for _ in range(4)]
    nb = [sb("nb%d" % i, [128, D], BF) for i in range(2)]; b_nb = [Buf(), Buf()]
    ss = [sb("ss%d" % i, [128, 1], F32) for i in range(2)]; b_ss = [Buf(), Buf()]
    rstd = [sb("rstd%d" % i, [128, 1], F32) for i in range(2)]; b_rstd = [Buf(), Buf()]
    n2T = sb("n2T", [128, KC, ST], BF); b_n2T = Buf()
    Wr = sb("Wr", [128, KC, NEXP_], BF); b_Wr = Buf()
    k.op("pool", lambda e: e.dma_start(out=Wr[:, :, :], in_=w_router.rearrange("(kc p) n -> p kc n", p=128)), writes=[b_Wr], dma=True)
    brt = sb("brt", [128, NEXP_], F32); b_brt = Buf()
    k.op("sp", lambda e: e.dma_start(out=brt[:, :], in_=brt_d[:, :]), writes=[b_brt], dma=True)
    bgu = sb("bgu", [128, NEXP_, 32], F32); b_bgu = Buf()
    k.op("sp", lambda e: e.dma_start(out=bgu[:, :, :], in_=bgu_d[:, :, :]), writes=[b_bgu], dma=True)
    bdn = sb("bdn", [NEXP_, D], F32); b_bdn = Buf()
    k.op("sp", lambda e: e.dma_start(out=bdn[:, :], in_=bdown_d[:, :]), writes=[b_bdn], dma=True)
    logit = sb("logit", [128, NEXP_], F32); b_logit = Buf()
    top8 = sb("top8", [128, 8], F32); b_top8 = Buf()
    msk = sb("msk", [128, NEXP_], F32); b_msk = Buf()
    den = sb("den", [128, 2], F32); b_den = Buf()
    gates = sb("gates", [128, 4, NEXP_], F32); b_gates = [Buf() for _ in range(4)]
    gT = sb("gT", [NEXP_, 4, 128], F32); b_gT = Buf()
    wgu = [sb("wgu%d" % i, [128, KC, 2, 256], BF) for i in range(2)]; b_wgu = [Buf(), Buf()]
    wd = [sb("wd%d" % i, [128, KC, 512], BF) for i in range(2)]; b_wd = [Buf(), Buf()]
    actT = sb("actT", [128, KC, ST], BF); b_actT = [Buf() for _ in range(KC)]
    g32 = [sb("g32%d" % i, [128, ST], F32) for i in range(2)]; b_g32 = [Buf(), Buf()]
    sg = [sb("sg%d" % i, [128, ST], F32) for i in range(2)]; b_sg = [Buf(), Buf()]
    u32 = [sb("u32%d" % i, [128, ST], F32) for i in range(2)]; b_u32 = [Buf(), Buf()]
    wctr = 0
    dctr = 0
    ectr = 0
    for j in range(NOWN):
        for tt in range(4):
            ti = j * 4 + tt
            i = tt % 2
            k.op("sp", lambda e, tt=tt, ti=ti: e.dma_start(out=acc[:, tt, :], in_=h_d[ti * 128:(ti + 1) * 128, :]), reads=[B_h[ti]], writes=[b_acc[tt]], dma=True)
            rms_tile(acc[:, tt, :], b_acc[tt], gv[:, :], b_gv, nb[i][:, :], b_nb[i], nb[i][:, :], b_nb[i], ss[i][:, :], b_ss[i], rstd[i][:, :], b_rstd[i], EPS)
            transpose_tile(nb[i], b_nb[i], n2T, b_n2T, tt * 128)
        for tt in range(4):
            for kc in range(KC):
                k.op("pe", lambda e, kc=kc, tt=tt: e.matmul(PS[2][:, 0:NEXP_], lhsT=n2T[:, kc, tt * 128:(tt + 1) * 128], rhs=Wr[:, kc, :], start=(kc == 0), stop=(kc == KC - 1)),
                     reads=[b_n2T, b_Wr], writes=[PSB[2]])
            k.op("dve", lambda e: e.tensor_tensor(out=logit[:, :], in0=PS[2][:, 0:NEXP_], in1=brt[:, :], op=ALU.add), reads=[PSB[2], b_brt], writes=[b_logit])
            k.op("dve", lambda e: e.max(out=top8[:, :], in_=logit[:, :]), reads=[b_logit], writes=[b_top8])
            k.op("dve", lambda e: e.tensor_scalar(out=msk[:, :], in0=logit[:, :], scalar1=top8[:, 3:4], scalar2=None, op0=ALU.is_ge), reads=[b_logit, b_top8], writes=[b_msk])
            k.op("dve", lambda e: e.tensor_scalar(out=den[:, 0:1], in0=top8[:, 0:1], scalar1=-1.0, scalar2=None, op0=ALU.mult), reads=[b_top8], writes=[b_den])
            k.op("act", lambda e: e.activation(out=logit[:, :], in_=logit[:, :], func=AF.Exp, bias=den[:, 0:1]), reads=[b_logit, b_den], writes=[b_logit])
            k.op("dve", lambda e: e.tensor_tensor(out=msk[:, :], in0=msk[:, :], in1=logit[:, :], op=ALU.mult), reads=[b_msk, b_logit], writes=[b_msk])
            k.op("dve", lambda e: e.reduce_sum(out=den[:, 1:2], in_=msk[:, :], axis=AX.X), reads=[b_msk], writes=[b_den])
            k.op("dve", lambda e: e.reciprocal(out=den[:, 1:2], in_=den[:, 1:2]), reads=[b_den], writes=[b_den])
            k.op("dve", lambda e, tt=tt: e.tensor_scalar(out=gates[:, tt, :], in0=msk[:, :], scalar1=den[:, 1:2], scalar2=None, op0=ALU.mult), reads=[b_msk, b_den], writes=[b_gates[tt]])
            k.op("pe", lambda e, tt=tt: e.transpose(out=PS[3][0:NEXP_, 0:128], in_=gates[:, tt, :], identity=identf[:, :]), reads=[b_gates[tt], b_identf], writes=[PSB[3]])
            k.op("act", lambda e, tt=tt: e.activation(out=gT[:, tt, :], in_=PS[3][0:NEXP_, 0:128], func=AF.Copy), reads=[PSB[3]], writes=[b_gT])
        for tt in range(4):
            for n4 in range(4):
                bank = 2 + n4 % 2
                k.op("pe", lambda e, tt=tt, n4=n4, bank=bank: e.matmul(PS[bank][:, :], lhsT=gT[:, tt, :], rhs=bdn[:, n4 * 512:(n4 + 1) * 512], start=True, stop=True), reads=[b_gT, b_bdn], writes=[PSB[bank]])
                k.op("dve", lambda e, tt=tt, n4=n4, bank=bank: e.tensor_tensor(out=acc[:, tt, n4 * 512:(n4 + 1) * 512], in0=PS[bank][:, :], in1=acc[:, tt, n4 * 512:(n4 + 1) * 512], op=ALU.add),
                     reads=[PSB[bank], b_acc[tt]], writes=[b_acc[tt]])
        chunks = []
        for ex in range(NEXP_):
            for fcb in range(8):
                chunks.append((ex, "gu", fcb))
            for dc in range(4):
                chunks.append((ex, "d", dc))
        cbuf = {}

        def issue(ch):
            nonlocal wctr, dctr
            ex, kind, idx = ch
            if kind == "gu":
                wi = wctr % 2
                wctr += 1
                cbuf[ch] = wi
                wguv = wgu_b[ex].rearrange("(kc p) n -> p kc n", p=128)
                for gu in range(2):
                    k.op("sp", lambda e, wi=wi, gu=gu, idx=idx, wguv=wguv: e.dma_start(out=wgu[wi][:, :, gu, :], in_=wguv[:, :, gu * D + idx * 256:gu * D + (idx + 1) * 256]),
                         reads=B_wgub[ex], writes=[b_wgu[wi]], dma=True)
            else:
                di = dctr % 2
                dctr += 1
                cbuf[ch] = di
                wdv = wd_b[ex].rearrange("(fc p) n -> p fc n", p=128)
                k.op("sp", lambda e, di=di, idx=idx, wdv=wdv: e.dma_start(out=wd[di][:, :, :], in_=wdv[:, :, idx * 512:(idx + 1) * 512]), reads=B_wdb[ex], writes=[b_wd[di]], dma=True)

        def compute(ch):
            nonlocal ectr
            ex, kind, idx = ch
            if kind == "gu":
                wi = cbuf[ch]
                fcb = idx
                for f2 in range(2):
                    fc = fcb * 2 + f2
                    ei = ectr % 2
                    ectr += 1
                    for gu in range(2):
                        bank = 4 + 2 * gu + ei
                        for kc in range(KC):
                            k.op("pe", lambda e, kc=kc, wi=wi, gu=gu, f2=f2, bank=bank: e.matmul(PS[bank][:, :], lhsT=wgu[wi][:, kc, gu, f2 * 128:(f2 + 1) * 128], rhs=n2T[:, kc, :], start=(kc == 0), stop=(kc == KC - 1)),
                                 reads=[b_wgu[wi], b_n2T], writes=[PSB[bank]])
                    bg, bu = 4 + ei, 6 + ei
                    k.op("dve", lambda e, ei=ei, bg=bg, ex=ex, fc=fc: e.tensor_scalar(out=g32[ei][:, :], in0=PS[bg][:, :], scalar1=bgu[:, ex, fc:fc + 1], scalar2=7.0, op0=ALU.add, op1=ALU.min),
                         reads=[PSB[bg], b_bgu], writes=[b_g32[ei]])
                    k.op("act", lambda e, ei=ei: e.activation(out=sg[ei][:, :], in_=g32[ei][:, :], func=AF.Sigmoid, scale=1.702), reads=[b_g32[ei]], writes=[b_sg[ei]])
                    k.op("dve", lambda e, ei=ei, bu=bu, ex=ex, fc=fc: e.tensor_scalar(out=u32[ei][:, :], in0=PS[bu][:, :], scalar1=bgu[:, ex, 16 + fc:16 + fc + 1], scalar2=7.0, op0=ALU.add, op1=ALU.min),
                         reads=[PSB[bu], b_bgu], writes=[b_u32[ei]])
                    k.op("pool", lambda e, ei=ei: e.tensor_scalar(out=u32[ei][:, :], in0=u32[ei][:, :], scalar1=-7.0, scalar2=1.0, op0=ALU.max, op1=ALU.add), reads=[b_u32[ei]], writes=[b_u32[ei]])
                    k.op("pool", lambda e, ei=ei: e.tensor_tensor(out=g32[ei][:, :], in0=g32[ei][:, :], in1=sg[ei][:, :], op=ALU.mult), reads=[b_g32[ei], b_sg[ei]], writes=[b_g32[ei]])
                    k.op("pool", lambda e, ei=ei, fc=fc: e.tensor_tensor(out=actT[:, fc, :], in0=g32[ei][:, :], in1=u32[ei][:, :], op=ALU.mult), reads=[b_g32[ei], b_u32[ei]], writes=[b_actT[fc]])
            else:
                di = cbuf[ch]
                dc = idx
                for tt in range(4):
                    bank = (tt % 2)
                    for fc in range(KC):
                        k.op("pe", lambda e, fc=fc, tt=tt, di=di, bank=bank: e.matmul(PS[bank][:, :], lhsT=actT[:, fc, tt * 128:(tt + 1) * 128], rhs=wd[di][:, fc, :], start=(fc == 0), stop=(fc == KC - 1)),
                             reads=[b_actT[fc], b_wd[di]], writes=[PSB[bank]])
                    k.op("dve", lambda e, tt=tt, dc=dc, ex=ex, bank=bank: e.scalar_tensor_tensor(out=acc[:, tt, dc * 512:(dc + 1) * 512], in0=PS[bank][:, :], scalar=gates[:, tt, ex:ex + 1], in1=acc[:, tt, dc * 512:(dc + 1) * 512], op0=ALU.mult, op1=ALU.add),
                         reads=[PSB[bank], b_gates[tt], b_acc[tt]], writes=[b_acc[tt]])

        issue(chunks[0])
        for ci in range(len(chunks)):
            if ci + 1 < len(chunks):
                issue(chunks[ci + 1])
            compute(chunks[ci])
        for tt in range(4):
            i = tt % 2
            ti = j * 4 + tt
            k.op("act", lambda e, tt=tt, i=i: e.activation(out=nb[i][:, :], in_=acc[:, tt, :], func=AF.Square, accum_out=ss[i][:, :]), reads=[b_acc[tt]], writes=[b_nb[i], b_ss[i]])
            k.op("act", lambda e, i=i: e.activation(out=rstd[i][:, :], in_=ss[i][:, :], func=AF.Sqrt, scale=1.0 / D, bias=epst[EPS]), reads=[b_ss[i]], writes=[b_rstd[i]])
            k.op("dve", lambda e, i=i: e.reciprocal(out=rstd[i][:, :], in_=rstd[i][:, :]), reads=[b_rstd[i]], writes=[b_rstd[i]])
            k.op("dve", lambda e, tt=tt, i=i: e.scalar_tensor_tensor(out=acc[:, tt, :], in0=acc[:, tt, :], scalar=rstd[i][:, :], in1=gfin[:, :], op0=ALU.mult, op1=ALU.mult),
                 reads=[b_acc[tt], b_rstd[i], b_gfin], writes=[b_acc[tt]])
            k.op("sp", lambda e, tt=tt, ti=ti: e.dma_start(out=out_d[ti * 128:(ti + 1) * 128, :], in_=acc[:, tt, :]), reads=[b_acc[tt]], writes=[B_out], dma=True)
        k.barrier()
    ph4.close()
    stack_outer.close()
    return nc, k, None


def _bf(a):
    return np.ascontiguousarray(a).astype(ml_dtypes.bfloat16)


def prep_inputs(inp, SEQ):
    OWN = SEQ // NCORES
    NKT = SEQ // 128
    f = lambda a: np.ascontiguousarray(np.asarray(a, dtype=np.float32))
    x = f(inp["x"])[0]
    rep = lambda v: np.ascontiguousarray(np.broadcast_to(f(v).reshape(1, -1), (128, f(v).size)))
    shared = {
        "mem": f(inp["mem"])[0],
        "w_in": f(inp["w_in"])[0], "w_out": f(inp["w_out"])[0], "w_cq": f(inp["w_cq"])[0],
        "w_ckv": f(inp["w_ckv"])[0], "w_co": f(inp["w_co"])[0], "w_router": f(inp["w_router"])[0],
        "w_gu": f(inp["w_gate_up"])[0], "w_down": f(inp["w_down"])[0],
        "w_rga": f(inp["w_rg_a"])[0], "w_rgx": f(inp["w_rg_x"])[0],
        "g_mix": rep(inp["norm_mix_g"][0]), "g_cross": rep(inp["norm_cross_g"][0]), "g_mem": rep(inp["norm_mem_g"][0]),
        "g_ffn": rep(inp["norm_ffn_g"][0]), "g_final": rep(inp["norm_final_g"]),
        "convw": np.ascontiguousarray(f(inp["conv_w"])[0].reshape(4, 8, 128).transpose(2, 1, 0)),
        "recv": np.ascontiguousarray(np.stack([f(inp[n])[0].reshape(8, 128) for n in ("conv_b", "b_rg_a", "b_rg_x", "rg_lambda", "rec_norm_g")], 0).transpose(2, 0, 1)),
        "lamv": np.ascontiguousarray(np.broadcast_to(np.stack([f(inp[n])[0] for n in ("lambda_q1", "lambda_k1", "lambda_q2", "lambda_k2")], 0)[None], (128, 4, 128))),
        "subg": np.ascontiguousarray(f(inp["subln_g"])[0].reshape(2, 128).T),
        "brt": rep(inp["b_router"][0]),
        "bgu": np.ascontiguousarray(f(inp["b_gate_up"])[0].reshape(-1, 32, 128).transpose(2, 0, 1)),
        "bdown": f(inp["b_down"])[0],
        "ident": _bf(np.eye(128, dtype=np.float32)),
        "identf": np.eye(128, dtype=np.float32),
    }
    kk = np.arange(128)[:, None, None]
    kr = np.arange(4)[None, :, None]
    qq = np.arange(ST)[None, None, :]
    shared["dmask"] = _bf(((kr * 128 + kk) <= qq).astype(np.float32))
    maps = []
    for c in range(NCORES):
        npad = SEQ - OWN * (c + 1)
        xs = np.zeros((SEQ, D), np.float32)
        xs[npad:] = x[: OWN * (c + 1)]
        valid = (np.arange(SEQ) >= npad).astype(np.float32)
        m = dict(shared)
        m["xseq"] = xs
        m["validrow"] = np.ascontiguousarray(np.broadcast_to(valid[None, :], (128, SEQ)))
        m["kbias"] = np.ascontiguousarray(((valid - 1.0) * 30000.0).reshape(NKT, 128).T)
        maps.append(m)
    return maps


def kernel(**inputs):
    SEQ = 16384
    nc, k, L = build(SEQ)
    maps = prep_inputs(inputs, SEQ)
    res = run_bass_kernel_spmd(nc, maps, core_ids=list(range(NCORES)))
    out = np.concatenate([np.asarray(r["out"]) for r in res.results], 0)[None]
    return np.ascontiguousarray(out.astype(np.float32))
```

```python
import math
from contextlib import ExitStack
import numpy as np
import ml_dtypes
import concourse.bass as bass
import concourse.mybir as mybir
from concourse.bass_utils import run_bass_kernel_spmd

F32 = mybir.dt.float32
BF = mybir.dt.bfloat16
AF = mybir.ActivationFunctionType
ALU = mybir.AluOpType
AX = mybir.AxisListType

D = 2048
KC = 16
ST = 512
NCORES = 8
NEXP = 32
NMEM = 256
EPS = 1e-6
DA_EPS = 1e-5
LAMBDA_INIT = 0.8 - 0.6 * math.exp(0.0)


class Buf:
    __slots__ = ("w", "r")

    def __init__(self):
        self.w = None
        self.r = {}


class EngQ:
    def __init__(self, name, sem, dsems):
        self.name = name
        self.sem = sem
        self.cnt = 0
        self.items = []
        self.dsems = dsems
        self.dn = 0
        self.waited = {}


class K:
    def __init__(self, nc, nds=6):
        self.nc = nc
        self.epoch = 0
        self.q = {}
        self.h = {"pe": nc.tensor, "act": nc.scalar, "dve": nc.vector, "pool": nc.gpsimd, "sp": nc.sync}
        for name in ("pe", "act", "dve", "pool", "sp"):
            sem = nc.alloc_semaphore("prog_" + name)
            dsems = []
            if name in ("sp", "pool", "act"):
                dsems = [nc.alloc_semaphore("dma_%s_%d" % (name, i)) for i in range(nds)]
            self.q[name] = EngQ(name, sem, dsems)

    def _waits(self, e, deps):
        out = []
        for (sem, val, src) in deps:
            if src == "pe" and e.name == "pe":
                continue
            key = id(sem)
            if e.waited.get(key, (None, 0))[1] >= val:
                continue
            e.waited[key] = (sem, val)
            out.append((sem, val))
        return out

    def op(self, eng, fn, reads=(), writes=(), dma=False):
        e = self.q[eng]
        deps = []
        for b in reads:
            if b.w is not None:
                deps.append(b.w)
        for b in writes:
            if b.w is not None:
                deps.append(b.w)
            deps.extend(b.r.values())
        if dma:
            ns = len(e.dsems)
            slot = e.dn % ns
            rnd = e.dn // ns
            e.dn += 1
            sem = e.dsems[slot]
            if rnd > 0:
                deps.append((sem, 16 * rnd, "dma"))
            tok = (sem, 16 * (rnd + 1), "dma")
            inc = (sem, 16)
        else:
            e.cnt += 1
            tok = (e.sem, e.cnt, eng)
            inc = (e.sem, 1)
        waits = self._waits(e, deps)
        h = self.h[eng]
        for (wsem, wval) in waits:
            h.wait_ge(wsem, wval)
        fn(h).then_inc(inc[0], inc[1])
        for b in reads:
            k = id(tok[0])
            if k not in b.r or b.r[k][1] < tok[1]:
                b.r[k] = tok
        for b in writes:
            b.w = tok
            b.r = {}
        return tok

    def all_tokens(self, skip_pool_dma=False):
        toks = []
        for e in self.q.values():
            if e.cnt > 0:
                toks.append((e.sem, e.cnt, e.name))
            if skip_pool_dma and e.name == "pool":
                continue
            ns = len(e.dsems)
            for slot in range(min(ns, e.dn)):
                n_uses = (e.dn - slot + ns - 1) // ns
                toks.append((e.dsems[slot], 16 * n_uses, "dma"))
        return toks

    def barrier(self, engines=("pe", "act", "dve", "pool", "sp"), skip_pool_dma=False):
        toks = self.all_tokens(skip_pool_dma)
        for name in engines:
            e = self.q[name]
            waits = []
            for (sem, val, src) in toks:
                key = id(sem)
                if e.waited.get(key, (None, 0))[1] >= val:
                    continue
                e.waited[key] = (sem, val)
                waits.append((sem, val))
            for (wsem, wval) in waits:
                self.h[name].wait_ge(wsem, wval)
        self.epoch += 1
        for name in engines:
            e = self.q[name]
            if e.cnt > 0:
                e.sem = self.nc.alloc_semaphore("prog%d_%s" % (self.epoch, name))
                e.cnt = 0


def build(SEQ, dbg=False, nexp=NEXP):
    NEXP_ = nexp
    OWN = SEQ // NCORES
    NST = SEQ // ST
    NOWN = OWN // ST
    S0 = NST - NOWN
    NKT = SEQ // 128

    nc = bass.Bass("TRN2", target_bir_lowering=False)

    def din(name, shape, dt=F32):
        return nc.dram_tensor(name, list(shape), dt, kind="ExternalInput").ap()

    xseq = din("xseq", [SEQ, D])
    validrow = din("validrow", [128, SEQ])
    kbias_d = din("kbias", [128, NKT])
    mem_d = din("mem", [NMEM, D])
    w_in = din("w_in", [D, 5120])
    w_out = din("w_out", [D, D])
    w_cq = din("w_cq", [D, D])
    w_ckv = din("w_ckv", [D, 2 * D])
    w_co = din("w_co", [D, D])
    w_router = din("w_router", [D, NEXP_])
    w_gu = din("w_gu", [NEXP_, D, 2 * D])
    w_down = din("w_down", [NEXP_, D, D])
    w_rga = din("w_rga", [8, 128, 128])
    w_rgx = din("w_rgx", [8, 128, 128])
    g_mix = din("g_mix", [128, D])
    g_cross = din("g_cross", [128, D])
    g_mem = din("g_mem", [128, D])
    g_ffn = din("g_ffn", [128, D])
    g_final = din("g_final", [128, D])
    convw_d = din("convw", [128, 8, 4])
    recv_d = din("recv", [128, 5, 8])
    lamv_d = din("lamv", [128, 4, 128])
    subg_d = din("subg", [128, 2])
    brt_d = din("brt", [128, NEXP_])
    bgu_d = din("bgu", [128, NEXP_, 32])
    bdown_d = din("bdown", [NEXP_, D])
    ident_d = din("ident", [128, 128], BF)
    identf_d = din("identf", [128, 128])
    dmask_d = din("dmask", [128, 4, ST], BF)

    out_d = nc.dram_tensor("out", [OWN, D], F32, kind="ExternalOutput").ap()

    skind = "ExternalOutput" if dbg else "Internal"

    def dscr(name, shape, dt):
        return nc.dram_tensor(name, list(shape), dt, kind=skind).ap()

    kT_d = dscr("kT_s", [1024, SEQ], BF)
    v_d = dscr("v_s", [SEQ, 1024], BF)
    hown_d = dscr("hown_s", [1024, OWN], F32)
    qT_d = dscr("qT_s", [1024, OWN], BF)
    cat_d = dscr("cat_s", [D, OWN], BF)
    h_d = dscr("h_s", [OWN, D], F32)
    oc_d = dscr("oc_s", [D, OWN], BF)

    wgu_b = [nc.dram_tensor("wgu_b%d" % ex, [D, 2 * D], BF, kind="Internal").ap() for ex in range(NEXP_)]
    wd_b = [nc.dram_tensor("wd_b%d" % ex, [D, D], BF, kind="Internal").ap() for ex in range(NEXP_)]
    k = K(nc)
    B_wgub = [[Buf() for _ in range(4)] for _ in range(NEXP_)]
    B_wdb = [[Buf() for _ in range(2)] for _ in range(NEXP_)]
    cast_list = []
    for ex in range(NEXP_):
        for a in range(4):
            cast_list.append((wgu_b[ex][a * 512:(a + 1) * 512, :].rearrange("(r p) n -> p r n", p=128),
                              w_gu[ex, a * 512:(a + 1) * 512, :].rearrange("(r p) n -> p r n", p=128), B_wgub[ex][a]))
        for a in range(2):
            cast_list.append((wd_b[ex][a * 1024:(a + 1) * 1024, :].rearrange("(r p) n -> p r n", p=128),
                              w_down[ex, a * 1024:(a + 1) * 1024, :].rearrange("(r p) n -> p r n", p=128), B_wdb[ex][a]))
    cast_pos = [0]

    def cast_some(n):
        for _ in range(n):
            if cast_pos[0] >= len(cast_list):
                return
            dst, srcap, bf_ = cast_list[cast_pos[0]]
            cast_pos[0] += 1
            k.op("pool", lambda e, dst=dst, srcap=srcap: e.dma_start(out=dst, in_=srcap), writes=[bf_], dma=True)

    B_kT = [[Buf() for _ in range(NST)] for _ in range(8)]
    B_v = [Buf() for _ in range(NKT)]
    B_hown = [[Buf() for _ in range(NOWN)] for _ in range(8)]
    B_qT = [[Buf() for _ in range(NOWN)] for _ in range(8)]
    B_cat = [[Buf() for _ in range(NOWN)] for _ in range(KC)]
    B_h = [Buf() for _ in range(OWN // 128)]
    B_oc = [Buf() for _ in range(NOWN)]
    B_out = Buf()

    stack_outer = ExitStack()
    cur = [stack_outer]

    sbn = [0]

    def sb(name, shape, dt):
        sbn[0] += 1
        return cur[0].enter_context(nc.sbuf_tensor("sb%d_%s" % (sbn[0], name), list(shape), dt))

    ident = sb("ident", [128, 128], BF); b_ident = Buf()
    identf = sb("identf", [128, 128], F32); b_identf = Buf()
    ones_bf = sb("ones_bf", [128, 128], BF); b_ones = Buf()
    k.op("sp", lambda e: e.dma_start(out=ident[:, :], in_=ident_d[:, :]), writes=[b_ident], dma=True)
    k.op("sp", lambda e: e.dma_start(out=identf[:, :], in_=identf_d[:, :]), writes=[b_identf], dma=True)
    k.op("dve", lambda e: e.memset(ones_bf[:, :], 1.0), writes=[b_ones])
    epst = {}
    for ci, cv in enumerate((EPS, DA_EPS, 1.0)):
        tcst = sb("cst%d" % ci, [128, 1], F32)
        k.op("dve", lambda e, tcst=tcst, cv=cv: e.memset(tcst[:, :], cv), writes=[Buf()])
        epst[cv] = tcst[:, :]
    k.barrier()

    PS = [nc.alloc_psum_tensor("ps%d" % i, [128, 512], F32) for i in range(8)]
    PSB = [Buf() for _ in range(8)]

    def psbf(i):
        return PS[i][:, :].bitcast(BF)

    def load_w(dst, dst_buf, src2d, c0, c1, kcn=KC, dcol0=0):
        srcv = src2d.rearrange("(kc p) n -> p kc n", p=128)
        for kc in range(kcn):
            k.op("pool", lambda e, kc=kc: e.dma_start(out=dst[:, kc, dcol0:dcol0 + (c1 - c0)], in_=srcv[:, kc, c0:c1]),
                 writes=[dst_buf], dma=True)

    def rms_tile(xt_ap, b_x, grep, b_g, nb_ap, b_nb, junk, b_junk, ss, b_ss, rstd, b_rstd, eps=EPS):
        k.op("act", lambda e: e.activation(out=junk, in_=xt_ap, func=AF.Square, accum_out=ss),
             reads=[b_x], writes=[b_junk, b_ss])
        k.op("act", lambda e: e.activation(out=rstd, in_=ss, func=AF.Sqrt, scale=1.0 / D, bias=epst[eps]),
             reads=[b_ss], writes=[b_rstd])
        k.op("dve", lambda e: e.reciprocal(out=rstd, in_=rstd),
             reads=[b_rstd], writes=[b_rstd])
        k.op("dve", lambda e: e.scalar_tensor_tensor(out=nb_ap, in0=xt_ap, scalar=rstd, in1=grep, op0=ALU.mult, op1=ALU.mult),
             reads=[b_x, b_rstd, b_g], writes=[b_nb])

    def transpose_tile(nb_t, b_nb, nT, b_nT, col0, banks=(0, 1)):
        for half in range(2):
            bank = banks[half]
            pv = psbf(bank)
            for i in range(8):
                kc = half * 8 + i
                k.op("pe", lambda e, kc=kc, i=i, pv=pv: e.transpose(out=pv[:, i * 128:(i + 1) * 128], in_=nb_t[:, kc * 128:(kc + 1) * 128], identity=ident[:, :]),
                     reads=[b_nb, b_ident], writes=[PSB[bank]])
            k.op("act", lambda e, half=half, pv=pv: e.activation(out=nT[:, half * 8:(half + 1) * 8, col0:col0 + 128],
                                                                 in_=pv.rearrange("p (a b) -> p a b", a=8), func=AF.Copy),
                 reads=[PSB[bank]], writes=[b_nT])

    ph1 = ExitStack()
    cur[0] = ph1
    W1 = sb("W1", [128, KC, 3072], BF); b_W1 = Buf()
    load_w(W1, b_W1, w_in, 1024, 4096)
    gmix = sb("gmix", [128, D], F32); b_gmix = Buf()
    k.op("sp", lambda e: e.dma_start(out=gmix[:, :], in_=g_mix[:, :]), writes=[b_gmix], dma=True)
    wa = sb("wa", [128, 8, 128], BF); b_wa = Buf()
    wx = sb("wx", [128, 8, 128], BF); b_wx = Buf()
    k.op("pool", lambda e: e.dma_start(out=wa[:, :, :], in_=w_rga.rearrange("n c d -> c n d")), writes=[b_wa], dma=True)
    k.op("pool", lambda e: e.dma_start(out=wx[:, :, :], in_=w_rgx.rearrange("n c d -> c n d")), writes=[b_wx], dma=True)
    convw = sb("convw", [128, 8, 4], F32); b_convw = Buf()
    recv = sb("recv", [128, 5, 8], F32); b_recv = Buf()
    k.op("sp", lambda e: e.dma_start(out=convw[:, :, :], in_=convw_d[:, :, :]), writes=[b_convw], dma=True)
    k.op("sp", lambda e: e.dma_start(out=recv[:, :, :], in_=recv_d[:, :, :]), writes=[b_recv], dma=True)

    uu = sb("uu", [128, 8], F32); b_uu = Buf()
    pp = sb("pp", [128, 8], F32); b_pp = Buf()
    cneg = sb("cneg", [128, 8], F32); b_cneg = Buf()
    cneg2 = sb("cneg2", [128, 8], F32); b_cneg2 = Buf()
    k.op("act", lambda e: e.activation(out=uu[:, :], in_=recv[:, 3, :], func=AF.Exp, scale=-1.0), reads=[b_recv], writes=[b_uu])
    k.op("dve", lambda e: e.tensor_scalar(out=pp[:, :], in0=uu[:, :], scalar1=-1.0 / 8, scalar2=1.0 / 7, op0=ALU.mult, op1=ALU.add), reads=[b_uu], writes=[b_pp])
    for c in (6, 5, 4, 3, 2, 1):
        k.op("dve", lambda e: e.tensor_tensor(out=pp[:, :], in0=pp[:, :], in1=uu[:, :], op=ALU.mult), reads=[b_pp, b_uu], writes=[b_pp])
        k.op("dve", lambda e, c=c: e.tensor_scalar(out=pp[:, :], in0=pp[:, :], scalar1=-1.0, scalar2=1.0 / c, op0=ALU.mult, op1=ALU.add), reads=[b_pp], writes=[b_pp])
    k.op("dve", lambda e: e.tensor_tensor(out=pp[:, :], in0=pp[:, :], in1=uu[:, :], op=ALU.mult), reads=[b_pp, b_uu], writes=[b_pp])
    k.op("dve", lambda e: e.tensor_scalar(out=cneg[:, :], in0=pp[:, :], scalar1=-8.0, scalar2=None, op0=ALU.mult), reads=[b_pp], writes=[b_cneg])
    k.op("dve", lambda e: e.tensor_scalar(out=cneg2[:, :], in0=pp[:, :], scalar1=-16.0, scalar2=None, op0=ALU.mult), reads=[b_pp], writes=[b_cneg2])

    xt = [sb("xt%d" % i, [128, D], F32) for i in range(2)]; b_xt = [Buf(), Buf()]
    nb = [sb("nb%d" % i, [128, D], BF) for i in range(2)]; b_nb = [Buf(), Buf()]
    ss = [sb("ss%d" % i, [128, 1], F32) for i in range(2)]; b_ss = [Buf(), Buf()]
    rstd = [sb("rstd%d" % i, [128, 1], F32) for i in range(2)]; b_rstd = [Buf(), Buf()]
    nT0 = sb("nT0", [128, KC, ST], BF); nT = [nT0, nT0]; b_nT0 = Buf(); b_nT = [b_nT0, b_nT0]

    tile_ctr = [0]

    def norm_transpose_supertile(s, grep, b_g, nTbuf, b_nTbuf, src_rows, b_src=None, eps=EPS):
        for tt in range(4):
            i = tile_ctr[0] % 2
            tile_ctr[0] += 1
            rd = [b_src[tt]] if b_src is not None else []
            k.op("sp", lambda e, tt=tt, i=i: e.dma_start(out=xt[i][:, :], in_=src_rows(tt)), reads=rd, writes=[b_xt[i]], dma=True)
            rms_tile(xt[i][:, :], b_xt[i], grep[:, :], b_g, nb[i][:, :], b_nb[i], nb[i][:, :], b_nb[i], ss[i][:, :], b_ss[i], rstd[i][:, :], b_rstd[i], eps)
            transpose_tile(nb[i], b_nb[i], nTbuf, b_nTbuf, tt * 128)

    xrp = sb("xrp", [128, 8, 3 + ST], F32); b_xrp = [Buf() for _ in range(8)]
    k.op("pool", lambda e: e.memset(xrp[:, :, :], 0.0), writes=b_xrp)
    hlast = sb("hlast", [128, 8], F32); b_hlast = [Buf() for _ in range(8)]
    k.op("pool", lambda e: e.memset(hlast[:, :], 0.0), writes=b_hlast)
    NR = 1
    xc = [sb("xc%d" % i, [128, ST], F32) for i in range(NR)]; b_xc = [Buf() for _ in range(NR)]
    xcb = [sb("xcb%d" % i, [128, ST], BF) for i in range(NR)]; b_xcb = [Buf() for _ in range(NR)]
    rr = [sb("rr%d" % i, [128, ST], F32) for i in range(NR)]; b_rr = [Buf() for _ in range(NR)]
    ii = [sb("ii%d" % i, [128, ST], F32) for i in range(NR)]; b_ii = [Buf() for _ in range(NR)]
    aa = [sb("aa%d" % i, [128, ST], F32) for i in range(NR)]; b_aa = [Buf() for _ in range(NR)]
    a2 = [sb("a2%d" % i, [128, ST], F32) for i in range(NR)]; b_a2 = [Buf() for _ in range(NR)]
    bb = [sb("bb%d" % i, [128, ST], F32) for i in range(NR)]; b_bb = [Buf() for _ in range(NR)]
    hh = [sb("hh%d" % i, [128, ST], F32) for i in range(NR)]; b_hh = [Buf() for _ in range(NR)]
    vrow = [sb("vrow%d" % i, [128, ST], F32) for i in range(2)]; b_vrow = [Buf(), Buf()]
    kst = [sb("kst%d" % i, [128, ST], BF) for i in range(2)]; b_kst = [Buf(), Buf()]
    vst = [sb("vst%d" % i, [128, 1024], BF) for i in range(2)]; b_vst = [Buf(), Buf()]

    rec_ctr = [0]
    for s in range(NST):
        nTb, b_nTb = nT[s % 2], b_nT[s % 2]
        norm_transpose_supertile(s, gmix, b_gmix, nTb, b_nTb, lambda tt, s=s: xseq[s * ST + tt * 128: s * ST + (tt + 1) * 128, :])
        own = s >= S0
        if not own:
            vi = s % 2
            k.op("sp", lambda e, s=s, vi=vi: e.dma_start(out=vrow[vi][:, :], in_=validrow[:, s * ST:(s + 1) * ST]), writes=[b_vrow[vi]], dma=True)
        for m in range(8):
            bank = 2 + (m % 2)
            for kc in range(KC):
                k.op("pe", lambda e, kc=kc, m=m, bank=bank: e.matmul(PS[bank][:, :], lhsT=W1[:, kc, m * 128:(m + 1) * 128], rhs=nTb[:, kc, :], start=(kc == 0), stop=(kc == KC - 1)),
                     reads=[b_W1, b_nTb], writes=[PSB[bank]])
            ki = m % 2
            k.op("act", lambda e, ki=ki, bank=bank: e.activation(out=kst[ki][:, :], in_=PS[bank][:, :], func=AF.Copy), reads=[PSB[bank]], writes=[b_kst[ki]])
            k.op("sp", lambda e, ki=ki, m=m, s=s: e.dma_start(out=kT_d[m * 128:(m + 1) * 128, s * ST:(s + 1) * ST], in_=kst[ki][:, :]),
                 reads=[b_kst[ki]], writes=[B_kT[m][s]], dma=True)
        for tt in range(4):
            vi = tt % 2
            for half in range(2):
                bank = 2 + half
                for kc in range(KC):
                    k.op("pe", lambda e, kc=kc, tt=tt, half=half, bank=bank: e.matmul(PS[bank][:, :], lhsT=nTb[:, kc, tt * 128:(tt + 1) * 128], rhs=W1[:, kc, 1024 + half * 512:1024 + (half + 1) * 512], start=(kc == 0), stop=(kc == KC - 1)),
                         reads=[b_W1, b_nTb], writes=[PSB[bank]])
                k.op("act", lambda e, vi=vi, half=half, bank=bank: e.activation(out=vst[vi][:, half * 512:(half + 1) * 512], in_=PS[bank][:, :], func=AF.Copy), reads=[PSB[bank]], writes=[b_vst[vi]])
            kt = s * 4 + tt
            k.op("sp", lambda e, vi=vi, kt=kt: e.dma_start(out=v_d[kt * 128:(kt + 1) * 128, :], in_=vst[vi][:, :]), reads=[b_vst[vi]], writes=[B_v[kt]], dma=True)
        cast_some((2 * len(cast_list) // 3 + NST - 1) // NST)
        for ct in range(8):
            ri = rec_ctr[0] % NR
            rec_ctr[0] += 1
            bank = 4 + (ct % 2)
            for kc in range(KC):
                k.op("pe", lambda e, kc=kc, ct=ct, bank=bank: e.matmul(PS[bank][:, :], lhsT=W1[:, kc, 2048 + ct * 128:2048 + (ct + 1) * 128], rhs=nTb[:, kc, :], start=(kc == 0), stop=(kc == KC - 1)),
                     reads=[b_W1, b_nTb], writes=[PSB[bank]])
            k.op("act", lambda e, ct=ct, bank=bank: e.activation(out=xrp[:, ct, 3:3 + ST], in_=PS[bank][:, :], func=AF.Copy), reads=[PSB[bank]], writes=[b_xrp[ct]])
            k.op("pool", lambda e, ct=ct, ri=ri: e.tensor_scalar(out=xc[ri][:, :], in0=xrp[:, ct, 0:ST], scalar1=convw[:, ct, 0:1], scalar2=recv[:, 0, ct:ct + 1], op0=ALU.mult, op1=ALU.add),
                 reads=[b_xrp[ct], b_convw, b_recv], writes=[b_xc[ri]])
            for j in (1, 2, 3):
                k.op("dve", lambda e, ct=ct, ri=ri, j=j: e.scalar_tensor_tensor(out=xc[ri][:, :], in0=xrp[:, ct, j:j + ST], scalar=convw[:, ct, j:j + 1], in1=xc[ri][:, :], op0=ALU.mult, op1=ALU.add),
                     reads=[b_xrp[ct], b_convw, b_xc[ri]], writes=[b_xc[ri]])
            k.op("pool", lambda e, ct=ct: e.tensor_copy(out=xrp[:, ct, 0:3], in_=xrp[:, ct, ST:ST + 3]), reads=[b_xrp[ct]], writes=[b_xrp[ct]])
            k.op("pool", lambda e, ri=ri: e.tensor_copy(out=xcb[ri][:, :], in_=xc[ri][:, :]), reads=[b_xc[ri]], writes=[b_xcb[ri]])
            k.op("pe", lambda e, ct=ct, ri=ri: e.matmul(PS[6][:, :], lhsT=wa[:, ct, :], rhs=xcb[ri][:, :], start=True, stop=True), reads=[b_wa, b_xcb[ri]], writes=[PSB[6]])
            k.op("pe", lambda e, ct=ct, ri=ri: e.matmul(PS[7][:, :], lhsT=wx[:, ct, :], rhs=xcb[ri][:, :], start=True, stop=True), reads=[b_wx, b_xcb[ri]], writes=[PSB[7]])
            k.op("act", lambda e, ct=ct, ri=ri: e.activation(out=rr[ri][:, :], in_=PS[6][:, :], func=AF.Sigmoid, bias=recv[:, 1, ct:ct + 1]), reads=[PSB[6], b_recv], writes=[b_rr[ri]])
            k.op("act", lambda e, ct=ct, ri=ri: e.activation(out=ii[ri][:, :], in_=PS[7][:, :], func=AF.Sigmoid, bias=recv[:, 2, ct:ct + 1]), reads=[PSB[7], b_recv], writes=[b_ii[ri]])
            k.op("act", lambda e, ct=ct, ri=ri: e.activation(out=aa[ri][:, :], in_=rr[ri][:, :], func=AF.Exp, scale=cneg[:, ct:ct + 1]), reads=[b_rr[ri], b_cneg], writes=[b_aa[ri]])
            k.op("act", lambda e, ct=ct, ri=ri: e.activation(out=a2[ri][:, :], in_=rr[ri][:, :], func=AF.Exp, scale=cneg2[:, ct:ct + 1]), reads=[b_rr[ri], b_cneg2], writes=[b_a2[ri]])
            k.op("act", lambda e, ri=ri: e.activation(out=a2[ri][:, :], in_=a2[ri][:, :], func=AF.Sqrt, scale=-1.0, bias=epst[1.0]), reads=[b_a2[ri]], writes=[b_a2[ri]])
            k.op("dve", lambda e, ri=ri: e.tensor_tensor(out=bb[ri][:, :], in0=ii[ri][:, :], in1=xc[ri][:, :], op=ALU.mult), reads=[b_ii[ri], b_xc[ri]], writes=[b_bb[ri]])
            k.op("dve", lambda e, ri=ri: e.tensor_tensor(out=bb[ri][:, :], in0=bb[ri][:, :], in1=a2[ri][:, :], op=ALU.mult), reads=[b_bb[ri], b_a2[ri]], writes=[b_bb[ri]])
            if not own:
                k.op("dve", lambda e, ri=ri, vi=s % 2: e.tensor_tensor(out=bb[ri][:, :], in0=bb[ri][:, :], in1=vrow[vi][:, :], op=ALU.mult), reads=[b_bb[ri], b_vrow[s % 2]], writes=[b_bb[ri]])
            k.op("dve", lambda e, ri=ri, ct=ct: e.tensor_tensor_scan(out=hh[ri][:, :], data0=aa[ri][:, :], data1=bb[ri][:, :], initial=hlast[:, ct:ct + 1], op0=ALU.mult, op1=ALU.add),
                 reads=[b_aa[ri], b_bb[ri], b_hlast[ct]], writes=[b_hh[ri]])
            k.op("dve", lambda e, ri=ri, ct=ct: e.tensor_copy(out=hlast[:, ct:ct + 1], in_=hh[ri][:, ST - 1:ST]), reads=[b_hh[ri]], writes=[b_hlast[ct]])
            if own:
                j = s - S0
                k.op("sp", lambda e, ri=ri, ct=ct, j=j: e.dma_start(out=hown_d[ct * 128:(ct + 1) * 128, j * ST:(j + 1) * ST], in_=hh[ri][:, :]), reads=[b_hh[ri]], writes=[B_hown[ct][j]], dma=True)

    k.barrier()

    W2 = W1
    b_W2 = Buf()
    load_w(W2, b_W2, w_in, 0, 1024, dcol0=0)
    load_w(W2, b_W2, w_in, 4096, 5120, dcol0=1024)
    ybuf = W1[:, 0:8, 2048:3072].bitcast(F32); b_ybuf = [b_W2 for _ in range(8)]
    ysq = W1[:, 8:16, 2048:2048 + ST]; b_ysq = [b_W2 for _ in range(8)]
    rs = a2[0]; b_rs = b_a2[0]
    ynb = [xcb[0], xcb[0]]; b_ynb = [b_xcb[0], b_xcb[0]]
    for j in range(NOWN):
        s = S0 + j
        nTb, b_nTb = nT[s % 2], b_nT[s % 2]
        norm_transpose_supertile(s, gmix, b_gmix, nTb, b_nTb, lambda tt, s=s: xseq[s * ST + tt * 128: s * ST + (tt + 1) * 128, :])
        for m in range(8):
            bank = 2 + (m % 2)
            for kc in range(KC):
                k.op("pe", lambda e, kc=kc, m=m, bank=bank: e.matmul(PS[bank][:, :], lhsT=W2[:, kc, m * 128:(m + 1) * 128], rhs=nTb[:, kc, :], start=(kc == 0), stop=(kc == KC - 1)),
                     reads=[b_W2, b_nTb], writes=[PSB[bank]])
            ki = m % 2
            k.op("act", lambda e, ki=ki, bank=bank: e.activation(out=kst[ki][:, :], in_=PS[bank][:, :], func=AF.Copy), reads=[PSB[bank]], writes=[b_kst[ki]])
            k.op("sp", lambda e, ki=ki, m=m, j=j: e.dma_start(out=qT_d[m * 128:(m + 1) * 128, j * ST:(j + 1) * ST], in_=kst[ki][:, :]),
                 reads=[b_kst[ki]], writes=[B_qT[m][j]], dma=True)
        for ct in range(8):
            ri = ct % NR
            bank = 4 + (ct % 2)
            for kc in range(KC):
                k.op("pe", lambda e, kc=kc, ct=ct, bank=bank: e.matmul(PS[bank][:, :], lhsT=W2[:, kc, 1024 + ct * 128:1024 + (ct + 1) * 128], rhs=nTb[:, kc, :], start=(kc == 0), stop=(kc == KC - 1)),
                     reads=[b_W2, b_nTb], writes=[PSB[bank]])
            k.op("act", lambda e, ri=ri, bank=bank: e.activation(out=xc[ri][:, :], in_=PS[bank][:, :], func=AF.Copy), reads=[PSB[bank]], writes=[b_xc[ri]])
            k.op("dve", lambda e, ri=ri: e.tensor_tensor(out=rr[ri][:, :], in0=xc[ri][:, :], in1=xc[ri][:, :], op=ALU.mult), reads=[b_xc[ri]], writes=[b_rr[ri]])
            k.op("dve", lambda e, ri=ri: e.tensor_scalar(out=rr[ri][:, :], in0=rr[ri][:, :], scalar1=0.044715, scalar2=1.0, op0=ALU.mult, op1=ALU.add), reads=[b_rr[ri]], writes=[b_rr[ri]])
            k.op("dve", lambda e, ri=ri: e.tensor_tensor(out=rr[ri][:, :], in0=rr[ri][:, :], in1=xc[ri][:, :], op=ALU.mult), reads=[b_rr[ri], b_xc[ri]], writes=[b_rr[ri]])
            k.op("act", lambda e, ri=ri: e.activation(out=ii[ri][:, :], in_=rr[ri][:, :], func=AF.Sigmoid, scale=1.5957691216057308), reads=[b_rr[ri]], writes=[b_ii[ri]])
            k.op("sp", lambda e, ri=ri, ct=ct, j=j: e.dma_start(out=hh[ri][:, :], in_=hown_d[ct * 128:(ct + 1) * 128, j * ST:(j + 1) * ST]), reads=[B_hown[ct][j]], writes=[b_hh[ri]], dma=True)
            k.op("dve", lambda e, ri=ri: e.tensor_tensor(out=bb[ri][:, :], in0=xc[ri][:, :], in1=ii[ri][:, :], op=ALU.mult), reads=[b_xc[ri], b_ii[ri]], writes=[b_bb[ri]])
            k.op("dve", lambda e, ri=ri, ct=ct: e.tensor_tensor(out=ybuf[:, ct, :], in0=bb[ri][:, :], in1=hh[ri][:, :], op=ALU.mult), reads=[b_bb[ri], b_hh[ri]], writes=[b_ybuf[ct]])
            k.op("act", lambda e, ct=ct: e.activation(out=ysq[:, ct, :], in_=ybuf[:, ct, :], func=AF.Square), reads=[b_ybuf[ct]], writes=[b_ysq[ct]])
        for ct in range(8):
            k.op("pe", lambda e, ct=ct: e.matmul(PS[6][:, :], lhsT=ones_bf[:, :], rhs=ysq[:, ct, :], start=(ct == 0), stop=(ct == 7)), reads=[b_ones, b_ysq[ct]], writes=[PSB[6]])
        k.op("act", lambda e: e.activation(out=rs[:, :], in_=PS[6][:, :], func=AF.Sqrt, scale=1.0 / 1024, bias=epst[EPS]), reads=[PSB[6]], writes=[b_rs])
        k.op("dve", lambda e: e.reciprocal(out=rs[:, :], in_=rs[:, :]), reads=[b_rs], writes=[b_rs])
        for ct in range(8):
            yi = ct % 2
            k.op("dve", lambda e, ct=ct, yi=yi: e.scalar_tensor_tensor(out=ynb[yi][:, :], in0=ybuf[:, ct, :], scalar=recv[:, 4, ct:ct + 1], in1=rs[:, :], op0=ALU.mult, op1=ALU.mult),
                 reads=[b_ybuf[ct], b_recv, b_rs], writes=[b_ynb[yi]])
            k.op("sp", lambda e, ct=ct, yi=yi, j=j: e.dma_start(out=cat_d[1024 + ct * 128:1024 + (ct + 1) * 128, j * ST:(j + 1) * ST], in_=ynb[yi][:, :]), reads=[b_ynb[yi]], writes=[B_cat[8 + ct][j]], dma=True)

    k.barrier(skip_pool_dma=True)
    ph1.close()

    ph2 = ExitStack()
    cur[0] = ph2
    kT2 = sb("kT2", [128, 2, SEQ], BF); b_kT2 = [Buf(), Buf()]
    vv = sb("vv", [128, NKT, 256], BF); b_vv = Buf()
    qh = sb("qh", [128, 2, OWN], BF); b_qh = Buf()
    kbias = sb("kbias", [128, NKT], F32); b_kbias = Buf()
    dmask = sb("dmask", [128, 4, ST], BF); b_dmask = Buf()
    lamv = sb("lamv", [128, 4, 128], F32); b_lamv = Buf()
    subg = sb("subg", [128, 2], F32); b_subg = Buf()
    lam_t = sb("lam_t", [128, 4], F32); b_lam = Buf()
    prod = sb("prod", [128, 128], F32); b_prod = Buf()
    pT = [sb("pT%d" % i, [128, ST], BF) for i in range(3)]; b_pT = [Buf() for _ in range(3)]
    rinv = sb("rinv", [128, ST], F32); b_rinv = Buf()
    racc = sb("racc", [128, ST], F32); b_racc = Buf()
    ones_f = sb("ones_f", [128, 128], F32); b_onesf = Buf()
    k.op("dve", lambda e: e.memset(ones_f[:, :], 1.0), writes=[b_onesf])
    osb = sb("osb", [128, 2, 2, ST], F32); b_osb = [Buf(), Buf()]
    od = sb("od", [128, 2, ST], F32); b_od = Buf()
    osq = sb("osq", [128, 2, ST], BF); b_osq = Buf()
    rs2 = sb("rs2", [128, ST], F32); b_rs2 = Buf()
    onb = [sb("onb%d" % i, [128, ST], BF) for i in range(2)]; b_onb = [Buf(), Buf()]
    k.op("sp", lambda e: e.dma_start(out=kbias[:, :], in_=kbias_d[:, :]), writes=[b_kbias], dma=True)
    k.op("sp", lambda e: e.dma_start(out=dmask[:, :, :], in_=dmask_d[:, :, :]), writes=[b_dmask], dma=True)
    k.op("sp", lambda e: e.dma_start(out=lamv[:, :, :], in_=lamv_d[:, :, :]), writes=[b_lamv], dma=True)
    k.op("sp", lambda e: e.dma_start(out=subg[:, :], in_=subg_d[:, :]), writes=[b_subg], dma=True)
    for t in range(2):
        k.op("dve", lambda e, t=t: e.tensor_tensor(out=prod[:, :], in0=lamv[:, 2 * t, :], in1=lamv[:, 2 * t + 1, :], op=ALU.mult), reads=[b_lamv], writes=[b_prod])
        k.op("dve", lambda e, t=t: e.reduce_sum(out=lam_t[:, t:t + 1], in_=prod[:, :], axis=AX.X), reads=[b_prod], writes=[b_lam])
    k.op("act", lambda e: e.activation(out=lam_t[:, 0:2], in_=lam_t[:, 0:2], func=AF.Exp), reads=[b_lam], writes=[b_lam])
    k.op("dve", lambda e: e.tensor_tensor(out=lam_t[:, 2:3], in0=lam_t[:, 1:2], in1=lam_t[:, 0:1], op=ALU.subtract), reads=[b_lam], writes=[b_lam])
    k.op("dve", lambda e: e.tensor_scalar(out=lam_t[:, 2:3], in0=lam_t[:, 2:3], scalar1=-LAMBDA_INIT, scalar2=None, op0=ALU.add), reads=[b_lam], writes=[b_lam])
    k.op("dve", lambda e: e.tensor_scalar(out=subg[:, :], in0=subg[:, :], scalar1=1.0 - LAMBDA_INIT, scalar2=None, op0=ALU.mult), reads=[b_subg], writes=[b_subg])
    SCALE = 128.0 ** -0.5
    pctr = 0
    for hd in range(4):
        for m in range(2):
            gm = hd * 2 + m
            NQ = 4 if NST % 4 == 0 else 1
            for qd in range(NQ):
                c0, c1 = qd * (SEQ // NQ), (qd + 1) * (SEQ // NQ)
                k.op("sp", lambda e, m=m, gm=gm, c0=c0, c1=c1: e.dma_start(out=kT2[:, m, c0:c1], in_=kT_d[gm * 128:(gm + 1) * 128, c0:c1]),
                     reads=B_kT[gm][qd * (NST // NQ):(qd + 1) * (NST // NQ)], writes=[b_kT2[m]], dma=True)
            k.op("sp", lambda e, m=m, gm=gm: e.dma_start(out=qh[:, m, :], in_=qT_d[gm * 128:(gm + 1) * 128, :]), reads=B_qT[gm], writes=[b_qh], dma=True)
        VG = 4 if NST % 4 == 0 else 1
        for s in range(0, NST, VG):
            k.op("sp", lambda e, s=s, hd=hd: e.dma_start(out=vv[:, s * 4:(s + VG) * 4, :], in_=v_d[s * ST:(s + VG) * ST, hd * 256:(hd + 1) * 256].rearrange("(t p) c -> p t c", p=128)),
                 reads=B_v[s * 4:(s + VG) * 4], writes=[b_vv], dma=True)
        for j in range(NOWN):
            nkb = (S0 + j + 1) * 4
            cast_some((len(cast_list) // 3 + 4 * NOWN - 1) // (4 * NOWN) + 1)
            for m in range(2):
                pis = []
                for kb in range(nkb):
                    pis.append(pctr % 3)
                    pctr += 1

                def emit_S(kb, m=m, j=j):
                    sbk = kb % 2
                    k.op("pe", lambda e, kb=kb, sbk=sbk: e.matmul(PS[sbk][:, :], lhsT=kT2[:, m, kb * 128:(kb + 1) * 128], rhs=qh[:, m, j * ST:(j + 1) * ST], start=True, stop=True),
                         reads=[b_kT2[m], b_qh], writes=[PSB[sbk]])

                emit_S(0)
                for kb in range(nkb):
                    sbk = kb % 2
                    pi = pis[kb]
                    if kb + 1 < nkb:
                        emit_S(kb + 1)
                    k.op("act", lambda e, kb=kb, sbk=sbk, pi=pi: e.activation(out=pT[pi][:, :], in_=PS[sbk][:, :], func=AF.Exp, scale=SCALE, bias=kbias[:, kb:kb + 1]),
                         reads=[PSB[sbk], b_kbias], writes=[b_pT[pi]])
                    kr = kb - (nkb - 4)
                    if kr >= 0:
                        k.op("dve", lambda e, pi=pi, kr=kr: e.tensor_tensor(out=pT[pi][:, :], in0=pT[pi][:, :], in1=dmask[:, kr, :], op=ALU.mult), reads=[b_pT[pi], b_dmask], writes=[b_pT[pi]])
                    for dvc in range(2):
                        k.op("pe", lambda e, kb=kb, dvc=dvc, pi=pi, nkb=nkb: e.matmul(PS[2 + dvc][:, :], lhsT=vv[:, kb, dvc * 128:(dvc + 1) * 128], rhs=pT[pi][:, :], start=(kb == 0), stop=(kb == nkb - 1)),
                             reads=[b_vv, b_pT[pi]], writes=[PSB[2 + dvc]])
                    if kb == 0:
                        k.op("dve", lambda e, pi=pi: e.tensor_copy(out=racc[:, :], in_=pT[pi][:, :]), reads=[b_pT[pi]], writes=[b_racc])
                    else:
                        k.op("dve", lambda e, pi=pi: e.tensor_tensor(out=racc[:, :], in0=racc[:, :], in1=pT[pi][:, :], op=ALU.add), reads=[b_pT[pi], b_racc], writes=[b_racc])
                k.op("pe", lambda e: e.matmul(PS[4][:, :], lhsT=ones_f[:, :], rhs=racc[:, :], start=True, stop=True), reads=[b_onesf, b_racc], writes=[PSB[4]])
                k.op("dve", lambda e: e.reciprocal(out=rinv[:, :], in_=PS[4][:, :]), reads=[PSB[4]], writes=[b_rinv])
                for dvc in range(2):
                    k.op("dve", lambda e, m=m, dvc=dvc: e.tensor_tensor(out=osb[:, m, dvc, :], in0=PS[2 + dvc][:, :], in1=rinv[:, :], op=ALU.mult), reads=[PSB[2 + dvc], b_rinv], writes=[b_osb[m]])
            k.op("dve", lambda e: e.scalar_tensor_tensor(out=od[:, :, :], in0=osb[:, 1, :, :], scalar=lam_t[:, 2:3], in1=osb[:, 0, :, :], op0=ALU.mult, op1=ALU.add),
                 reads=[b_osb[0], b_osb[1], b_lam], writes=[b_od])
            k.op("act", lambda e: e.activation(out=osq[:, :, :], in_=od[:, :, :], func=AF.Square), reads=[b_od], writes=[b_osq])
            for dvc in range(2):
                k.op("pe", lambda e, dvc=dvc: e.matmul(PS[5][:, :], lhsT=ones_bf[:, :], rhs=osq[:, dvc, :], start=(dvc == 0), stop=(dvc == 1)), reads=[b_ones, b_osq], writes=[PSB[5]])
            k.op("act", lambda e: e.activation(out=rs2[:, :], in_=PS[5][:, :], func=AF.Sqrt, scale=1.0 / 256, bias=epst[DA_EPS]), reads=[PSB[5]], writes=[b_rs2])
            k.op("dve", lambda e: e.reciprocal(out=rs2[:, :], in_=rs2[:, :]), reads=[b_rs2], writes=[b_rs2])
            for dvc in range(2):
                k.op("dve", lambda e, dvc=dvc: e.scalar_tensor_tensor(out=onb[dvc][:, :], in0=od[:, dvc, :], scalar=subg[:, dvc:dvc + 1], in1=rs2[:, :], op0=ALU.mult, op1=ALU.mult),
                     reads=[b_od, b_subg, b_rs2], writes=[b_onb[dvc]])
                kc = hd * 2 + dvc
                k.op("sp", lambda e, dvc=dvc, kc=kc, j=j: e.dma_start(out=cat_d[kc * 128:(kc + 1) * 128, j * ST:(j + 1) * ST], in_=onb[dvc][:, :]), reads=[b_onb[dvc]], writes=[B_cat[kc][j]], dma=True)
    cast_some(len(cast_list))
    k.barrier()
    ph2.close()

    ph3 = ExitStack()
    cur[0] = ph3
    Wb = sb("Wb", [128, KC, D], BF); b_Wb = Buf()
    gv = sb("gv", [128, D], F32); b_gv = Buf()
    xt = [sb("xt%d" % i, [128, D], F32) for i in range(2)]; b_xt = [Buf(), Buf()]
    nb = [sb("nb%d" % i, [128, D], BF) for i in range(2)]; b_nb = [Buf(), Buf()]
    ss = [sb("ss%d" % i, [128, 1], F32) for i in range(2)]; b_ss = [Buf(), Buf()]
    rstd = [sb("rstd%d" % i, [128, 1], F32) for i in range(2)]; b_rstd = [Buf(), Buf()]
    nT0 = sb("nT0", [128, KC, ST], BF); b_nT0 = Buf()
    catT = sb("catT", [128, KC, ST], BF); b_catT = Buf()
    ht = [sb("ht%d" % i, [128, D], F32) for i in range(2)]; b_ht = [Buf(), Buf()]
    memT = sb("memT", [128, KC, NMEM], BF); b_memT = Buf()
    KmT = sb("KmT", [128, KC, NMEM], BF); b_KmT = Buf()
    Vm = sb("Vm", [128, 2, D], BF); b_Vm = Buf()
    pc = [sb("pc%d" % i, [128, ST], BF) for i in range(2)]; b_pc = [Buf(), Buf()]
    rinv = sb("rinv", [128, ST], F32); b_rinv = Buf()

    load_w(Wb, b_Wb, w_out, 0, D)
    hctr = 0
    for j in range(NOWN):
        for kc in range(KC):
            k.op("sp", lambda e, kc=kc, j=j: e.dma_start(out=catT[:, kc, :], in_=cat_d[kc * 128:(kc + 1) * 128, j * ST:(j + 1) * ST]), reads=[B_cat[kc][j]], writes=[b_catT], dma=True)
        for tt in range(4):
            hi = hctr % 2
            hctr += 1
            row0 = (S0 + j) * ST + tt * 128
            k.op("sp", lambda e, hi=hi, row0=row0: e.dma_start(out=xt[hi][:, :], in_=xseq[row0:row0 + 128, :]), writes=[b_xt[hi]], dma=True)
            for n4 in range(4):
                bank = n4 % 2
                for kc in range(KC):
                    k.op("pe", lambda e, kc=kc, tt=tt, n4=n4, bank=bank: e.matmul(PS[bank][:, :], lhsT=catT[:, kc, tt * 128:(tt + 1) * 128], rhs=Wb[:, kc, n4 * 512:(n4 + 1) * 512], start=(kc == 0), stop=(kc == KC - 1)),
                         reads=[b_catT, b_Wb], writes=[PSB[bank]])
                k.op("dve", lambda e, hi=hi, n4=n4, bank=bank: e.tensor_tensor(out=ht[hi][:, n4 * 512:(n4 + 1) * 512], in0=PS[bank][:, :], in1=xt[hi][:, n4 * 512:(n4 + 1) * 512], op=ALU.add),
                     reads=[PSB[bank], b_xt[hi]], writes=[b_ht[hi]])
            ti = j * 4 + tt
            k.op("sp", lambda e, hi=hi, ti=ti: e.dma_start(out=h_d[ti * 128:(ti + 1) * 128, :], in_=ht[hi][:, :]), reads=[b_ht[hi]], writes=[B_h[ti]], dma=True)

    k.op("sp", lambda e: e.dma_start(out=gv[:, :], in_=g_mem[:, :]), writes=[b_gv], dma=True)
    tile_ctr[0] = 0
    for mt in range(2):
        i = mt
        k.op("sp", lambda e, mt=mt, i=i: e.dma_start(out=xt[i][:, :], in_=mem_d[mt * 128:(mt + 1) * 128, :]), writes=[b_xt[i]], dma=True)
        rms_tile(xt[i][:, :], b_xt[i], gv[:, :], b_gv, nb[i][:, :], b_nb[i], nb[i][:, :], b_nb[i], ss[i][:, :], b_ss[i], rstd[i][:, :], b_rstd[i], EPS)
        transpose_tile(nb[i], b_nb[i], memT, b_memT, mt * 128)
    load_w(Wb, b_Wb, w_ckv, 0, D)
    for fc in range(KC):
        bank = 2 + fc % 2
        for kc in range(KC):
            k.op("pe", lambda e, kc=kc, fc=fc, bank=bank: e.matmul(PS[bank][:, 0:NMEM], lhsT=Wb[:, kc, fc * 128:(fc + 1) * 128], rhs=memT[:, kc, :], start=(kc == 0), stop=(kc == KC - 1)),
                 reads=[b_Wb, b_memT], writes=[PSB[bank]])
        k.op("act", lambda e, fc=fc, bank=bank: e.activation(out=KmT[:, fc, :], in_=PS[bank][:, 0:NMEM], func=AF.Copy), reads=[PSB[bank]], writes=[b_KmT])
    load_w(Wb, b_Wb, w_ckv, D, 2 * D)
    for mt in range(2):
        for n4 in range(4):
            bank = 2 + n4 % 2
            for kc in range(KC):
                k.op("pe", lambda e, kc=kc, mt=mt, n4=n4, bank=bank: e.matmul(PS[bank][:, :], lhsT=memT[:, kc, mt * 128:(mt + 1) * 128], rhs=Wb[:, kc, n4 * 512:(n4 + 1) * 512], start=(kc == 0), stop=(kc == KC - 1)),
                     reads=[b_Wb, b_memT], writes=[PSB[bank]])
            k.op("act", lambda e, mt=mt, n4=n4, bank=bank: e.activation(out=Vm[:, mt, n4 * 512:(n4 + 1) * 512], in_=PS[bank][:, :], func=AF.Copy), reads=[PSB[bank]], writes=[b_Vm])
    load_w(Wb, b_Wb, w_cq, 0, D)
    k.op("sp", lambda e: e.dma_start(out=gv[:, :], in_=g_cross[:, :]), writes=[b_gv], dma=True)
    qcT = catT; b_qcT = b_catT
    CS = 512.0 ** -0.5
    for j in range(NOWN):
        norm_transpose_supertile(j, gv, b_gv, nT0, b_nT0, lambda tt, j=j: h_d[(j * 4 + tt) * 128:(j * 4 + tt + 1) * 128, :], b_src=B_h[j * 4:(j + 1) * 4])
        for fc in range(KC):
            bank = 2 + fc % 2
            for kc in range(KC):
                k.op("pe", lambda e, kc=kc, fc=fc, bank=bank: e.matmul(PS[bank][:, :], lhsT=Wb[:, kc, fc * 128:(fc + 1) * 128], rhs=nT0[:, kc, :], start=(kc == 0), stop=(kc == KC - 1)),
                     reads=[b_Wb, b_nT0], writes=[PSB[bank]])
            k.op("act", lambda e, fc=fc, bank=bank: e.activation(out=qcT[:, fc, :], in_=PS[bank][:, :], func=AF.Copy), reads=[PSB[bank]], writes=[b_qcT])
        ocT = nT0; b_ocT = b_nT0
        for hc in range(4):
            for mc in range(2):
                bank = 4 + mc
                for dc in range(4):
                    k.op("pe", lambda e, hc=hc, mc=mc, dc=dc, bank=bank: e.matmul(PS[bank][:, :], lhsT=KmT[:, 4 * hc + dc, mc * 128:(mc + 1) * 128], rhs=qcT[:, 4 * hc + dc, :], start=(dc == 0), stop=(dc == 3)),
                         reads=[b_KmT, b_qcT], writes=[PSB[bank]])
                k.op("act", lambda e, mc=mc, bank=bank: e.activation(out=pc[mc][:, :], in_=PS[bank][:, :], func=AF.Exp, scale=CS), reads=[PSB[bank]], writes=[b_pc[mc]])
            for mc in range(2):
                k.op("pe", lambda e, mc=mc: e.matmul(PS[6][:, :], lhsT=ones_bf[:, :], rhs=pc[mc][:, :], start=(mc == 0), stop=(mc == 1)), reads=[b_ones, b_pc[mc]], writes=[PSB[6]])
            k.op("dve", lambda e: e.reciprocal(out=rinv[:, :], in_=PS[6][:, :]), reads=[PSB[6]], writes=[b_rinv])
            for dvc in range(4):
                bank = dvc % 2
                for mc in range(2):
                    k.op("pe", lambda e, hc=hc, mc=mc, dvc=dvc, bank=bank: e.matmul(PS[bank][:, :], lhsT=Vm[:, mc, hc * 512 + dvc * 128:hc * 512 + (dvc + 1) * 128], rhs=pc[mc][:, :], start=(mc == 0), stop=(mc == 1)),
                         reads=[b_Vm, b_pc[mc]], writes=[PSB[bank]])
                k.op("dve", lambda e, hc=hc, dvc=dvc, bank=bank: e.tensor_tensor(out=ocT[:, 4 * hc + dvc, :], in0=PS[bank][:, :], in1=rinv[:, :], op=ALU.mult), reads=[PSB[bank], b_rinv], writes=[b_ocT])
        for kc in range(KC):
            k.op("sp", lambda e, kc=kc, j=j: e.dma_start(out=oc_d[kc * 128:(kc + 1) * 128, j * ST:(j + 1) * ST], in_=ocT[:, kc, :]), reads=[b_ocT], writes=[B_oc[j]], dma=True)

    load_w(Wb, b_Wb, w_co, 0, D)
    for j in range(NOWN):
        for kc in range(KC):
            k.op("sp", lambda e, kc=kc, j=j: e.dma_start(out=catT[:, kc, :], in_=oc_d[kc * 128:(kc + 1) * 128, j * ST:(j + 1) * ST]), reads=[B_oc[j]], writes=[b_catT], dma=True)
        for tt in range(4):
            hi = hctr % 2
            hctr += 1
            ti = j * 4 + tt
            k.op("sp", lambda e, hi=hi, ti=ti: e.dma_start(out=xt[hi][:, :], in_=h_d[ti * 128:(ti + 1) * 128, :]), reads=[B_h[ti]], writes=[b_xt[hi]], dma=True)
            for n4 in range(4):
                bank = n4 % 2
                for kc in range(KC):
                    k.op("pe", lambda e, kc=kc, tt=tt, n4=n4, bank=bank: e.matmul(PS[bank][:, :], lhsT=catT[:, kc, tt * 128:(tt + 1) * 128], rhs=Wb[:, kc, n4 * 512:(n4 + 1) * 512], start=(kc == 0), stop=(kc == KC - 1)),
                         reads=[b_catT, b_Wb], writes=[PSB[bank]])
                k.op("dve", lambda e, hi=hi, n4=n4, bank=bank: e.tensor_tensor(out=ht[hi][:, n4 * 512:(n4 + 1) * 512], in0=PS[bank][:, :], in1=xt[hi][:, n4 * 512:(n4 + 1) * 512], op=ALU.add),
                     reads=[PSB[bank], b_xt[hi]], writes=[b_ht[hi]])
            k.op("sp", lambda e, hi=hi, ti=ti: e.dma_start(out=h_d[ti * 128:(ti + 1) * 128, :], in_=ht[hi][:, :]), reads=[b_ht[hi]], writes=[B_h[ti]], dma=True)
    k.barrier()
    ph3.close()

    ph4 = ExitStack()
    cur[0] = ph4
    gv = sb("gv", [128, D], F32); b_gv = Buf()
    gfin = sb("gfin", [128, D], F32); b_gfin = Buf()
    k.op("sp", lambda e: e.dma_start(out=gv[:, :], in_=g_ffn[:, :]), writes=[b_gv], dma=True)
    k.op("sp", lambda e: e.dma_start(out=gfin[:, :], in_=g_final[:, :]), writes=[b_gfin], dma=True)
    acc = sb("acc", [128, 4, D], F32); b_acc = [Buf() for _ in range(4)]
    nb = [sb("nb%d" % i, [128, D], BF) for i in range(2)]; b_nb = [Buf(), Buf()]
    ss = [sb("ss%d" % i, [128, 1], F32) for i in range(2)]; b_ss = [Buf(), Buf()]
    rstd = [sb("rstd%d" % i, [128, 1], F32) for i in range(2)]; b_rstd = [Buf(), Buf()]
    n2T = sb("n2T", [128, KC, ST], BF); b_n2T = Buf()
    Wr = sb("Wr", [128, KC, NEXP_], BF); b_Wr = Buf()
    k.op("pool", lambda e: e.dma_start(out=Wr[:, :, :], in_=w_router.rearrange("(kc p) n -> p kc n", p=128)), writes=[b_Wr], dma=True)
    brt = sb("brt", [128, NEXP_], F32); b_brt = Buf()
    k.op("sp", lambda e: e.dma_start(out=brt[:, :], in_=brt_d[:, :]), writes=[b_brt], dma=True)
    bgu = sb("bgu", [128, NEXP_, 32], F32); b_bgu = Buf()
    k.op("sp", lambda e: e.dma_start(out=bgu[:, :, :], in_=bgu_d[:, :, :]), writes=[b_bgu], dma=True)
    bdn = sb("bdn", [NEXP_, D], F32); b_bdn = Buf()
    k.op("sp", lambda e: e.dma_start(out=bdn[:, :], in_=bdown_d[:, :]), writes=[b_bdn], dma=True)
    logit = sb("logit", [128, NEXP_], F32); b_logit = Buf()
    top8 = sb("top8", [128, 8], F32); b_top8 = Buf()
    msk = sb("msk", [128, NEXP_], F32); b_msk = Buf()
    den = sb("den", [128, 2], F32); b_den = Buf()
    gates = sb("gates", [128, 4, NEXP_], F32); b_gates = [Buf() for _ in range(4)]
    gT = sb("gT", [NEXP_, 4, 128], F32); b_gT = Buf()
    wgu = [sb("wgu%d" % i, [128, KC, 2, 256], BF) for i in range(2)]; b_wgu = [Buf(), Buf()]
    wd = [sb("wd%d" % i, [128, KC, 512], BF) for i in range(2)]; b_wd = [Buf(), Buf()]
    actT = sb("actT", [128, KC, ST], BF); b_actT = [Buf() for _ in range(KC)]
    g32 = [sb("g32%d" % i, [128, ST], F32) for i in range(2)]; b_g32 = [Buf(), Buf()]
    sg = [sb("sg%d" % i, [128, ST], F32) for i in range(2)]; b_sg = [Buf(), Buf()]
    u32 = [sb("u32%d" % i, [128, ST], F32) for i in range(2)]; b_u32 = [Buf(), Buf()]
    wctr = 0
    dctr = 0
    ectr = 0
    for j in range(NOWN):
        for tt in range(4):
            ti = j * 4 + tt
            i = tt % 2
            k.op("sp", lambda e, tt=tt, ti=ti: e.dma_start(out=acc[:, tt, :], in_=h_d[ti * 128:(ti + 1) * 128, :]), reads=[B_h[ti]], writes=[b_acc[tt]], dma=True)
            rms_tile(acc[:, tt, :], b_acc[tt], gv[:, :], b_gv, nb[i][:, :], b_nb[i], nb[i][:, :], b_nb[i], ss[i][:, :], b_ss[i], rstd[i][:, :], b_rstd[i], EPS)
            transpose_tile(nb[i], b_nb[i], n2T, b_n2T, tt * 128)
        for tt in range(4):
            for kc in range(KC):
                k.op("pe", lambda e, kc=kc, tt=tt: e.matmul(PS[2][:, 0:NEXP_], lhsT=n2T[:, kc, tt * 128:(tt + 1) * 128], rhs=Wr[:, kc, :], start=(kc == 0), stop=(kc == KC - 1)),
                     reads=[b_n2T, b_Wr], writes=[PSB[2]])
            k.op("dve", lambda e: e.tensor_tensor(out=logit[:, :], in0=PS[2][:, 0:NEXP_], in1=brt[:, :], op=ALU.add), reads=[PSB[2], b_brt], writes=[b_logit])
            k.op("dve", lambda e: e.max(out=top8[:, :], in_=logit[:, :]), reads=[b_logit], writes=[b_top8])
            k.op("dve", lambda e: e.tensor_scalar(out=msk[:, :], in0=logit[:, :], scalar1=top8[:, 3:4], scalar2=None, op0=ALU.is_ge), reads=[b_logit, b_top8], writes=[b_msk])
            k.op("dve", lambda e: e.tensor_scalar(out=den[:, 0:1], in0=top8[:, 0:1], scalar1=-1.0, scalar2=None, op0=ALU.mult), reads=[b_top8], writes=[b_den])
            k.op("act", lambda e: e.activation(out=logit[:, :], in_=logit[:, :], func=AF.Exp, bias=den[:, 0:1]), reads=[b_logit, b_den], writes=[b_logit])
            k.op("dve", lambda e: e.tensor_tensor(out=msk[:, :], in0=msk[:, :], in1=logit[:, :], op=ALU.mult), reads=[b_msk, b_logit], writes=[b_msk])
            k.op("dve", lambda e: e.reduce_sum(out=den[:, 1:2], in_=msk[:, :], axis=AX.X), reads=[b_msk], writes=[b_den])
            k.op("dve", lambda e: e.reciprocal(out=den[:, 1:2], in_=den[:, 1:2]), reads=[b_den], writes=[b_den])
            k.op("dve", lambda e, tt=tt: e.tensor_scalar(out=gates[:, tt, :], in0=msk[:, :], scalar1=den[:, 1:2], scalar2=None, op0=ALU.mult), reads=[b_msk, b_den], writes=[b_gates[tt]])
            k.op("pe", lambda e, tt=tt: e.transpose(out=PS[3][0:NEXP_, 0:128], in_=gates[:, tt, :], identity=identf[:, :]), reads=[b_gates[tt], b_identf], writes=[PSB[3]])
            k.op("act", lambda e, tt=tt: e.activation(out=gT[:, tt, :], in_=PS[3][0:NEXP_, 0:128], func=AF.Copy), reads=[PSB[3]], writes=[b_gT])
        for tt in range(4):
            for n4 in range(4):
                bank = 2 + n4 % 2
                k.op("pe", lambda e, tt=tt, n4=n4, bank=bank: e.matmul(PS[bank][:, :], lhsT=gT[:, tt, :], rhs=bdn[:, n4 * 512:(n4 + 1) * 512], start=True, stop=True), reads=[b_gT, b_bdn], writes=[PSB[bank]])
                k.op("dve", lambda e, tt=tt, n4=n4, bank=bank: e.tensor_tensor(out=acc[:, tt, n4 * 512:(n4 + 1) * 512], in0=PS[bank][:, :], in1=acc[:, tt, n4 * 512:(n4 + 1) * 512], op=ALU.add),
                     reads=[PSB[bank], b_acc[tt]], writes=[b_acc[tt]])
        chunks = []
        for ex in range(NEXP_):
            for fcb in range(8):
                chunks.append((ex, "gu", fcb))
            for dc in range(4):
                chunks.append((ex, "d", dc))
        cbuf = {}

        def issue(ch):
            nonlocal wctr, dctr
            ex, kind, idx = ch
            if kind == "gu":
                wi = wctr % 2
                wctr += 1
                cbuf[ch] = wi
                wguv = wgu_b[ex].rearrange("(kc p) n -> p kc n", p=128)
                for gu in range(2):
                    k.op("sp", lambda e, wi=wi, gu=gu, idx=idx, wguv=wguv: e.dma_start(out=wgu[wi][:, :, gu, :], in_=wguv[:, :, gu * D + idx * 256:gu * D + (idx + 1) * 256]),
                         reads=B_wgub[ex], writes=[b_wgu[wi]], dma=True)
            else:
                di = dctr % 2
                dctr += 1
                cbuf[ch] = di
                wdv = wd_b[ex].rearrange("(fc p) n -> p fc n", p=128)
                k.op("sp", lambda e, di=di, idx=idx, wdv=wdv: e.dma_start(out=wd[di][:, :, :], in_=wdv[:, :, idx * 512:(idx + 1) * 512]), reads=B_wdb[ex], writes=[b_wd[di]], dma=True)

        def compute(ch):
            nonlocal ectr
            ex, kind, idx = ch
            if kind == "gu":
                wi = cbuf[ch]
                fcb = idx
                for f2 in range(2):
                    fc = fcb * 2 + f2
                    ei = ectr % 2
                    ectr += 1
                    for gu in range(2):
                        bank = 4 + 2 * gu + ei
                        for kc in range(KC):
                            k.op("pe", lambda e, kc=kc, wi=wi, gu=gu, f2=f2, bank=bank: e.matmul(PS[bank][:, :], lhsT=wgu[wi][:, kc, gu, f2 * 128:(f2 + 1) * 128], rhs=n2T[:, kc, :], start=(kc == 0), stop=(kc == KC - 1)),
                                 reads=[b_wgu[wi], b_n2T], writes=[PSB[bank]])
                    bg, bu = 4 + ei, 6 + ei
                    k.op("dve", lambda e, ei=ei, bg=bg, ex=ex, fc=fc: e.tensor_scalar(out=g32[ei][:, :], in0=PS[bg][:, :], scalar1=bgu[:, ex, fc:fc + 1], scalar2=7.0, op0=ALU.add, op1=ALU.min),
                         reads=[PSB[bg], b_bgu], writes=[b_g32[ei]])
                    k.op("act", lambda e, ei=ei: e.activation(out=sg[ei][:, :], in_=g32[ei][:, :], func=AF.Sigmoid, scale=1.702), reads=[b_g32[ei]], writes=[b_sg[ei]])
                    k.op("dve", lambda e, ei=ei, bu=bu, ex=ex, fc=fc: e.tensor_scalar(out=u32[ei][:, :], in0=PS[bu][:, :], scalar1=bgu[:, ex, 16 + fc:16 + fc + 1], scalar2=7.0, op0=ALU.add, op1=ALU.min),
                         reads=[PSB[bu], b_bgu], writes=[b_u32[ei]])
                    k.op("pool", lambda e, ei=ei: e.tensor_scalar(out=u32[ei][:, :], in0=u32[ei][:, :], scalar1=-7.0, scalar2=1.0, op0=ALU.max, op1=ALU.add), reads=[b_u32[ei]], writes=[b_u32[ei]])
                    k.op("pool", lambda e, ei=ei: e.tensor_tensor(out=g32[ei][:, :], in0=g32[ei][:, :], in1=sg[ei][:, :], op=ALU.mult), reads=[b_g32[ei], b_sg[ei]], writes=[b_g32[ei]])
                    k.op("pool", lambda e, ei=ei, fc=fc: e.tensor_tensor(out=actT[:, fc, :], in0=g32[ei][:, :], in1=u32[ei][:, :], op=ALU.mult), reads=[b_g32[ei], b_u32[ei]], writes=[b_actT[fc]])
            else:
                di = cbuf[ch]
                dc = idx
                for tt in range(4):
                    bank = (tt % 2)
                    for fc in range(KC):
                        k.op("pe", lambda e, fc=fc, tt=tt, di=di, bank=bank: e.matmul(PS[bank][:, :], lhsT=actT[:, fc, tt * 128:(tt + 1) * 128], rhs=wd[di][:, fc, :], start=(fc == 0), stop=(fc == KC - 1)),
                             reads=[b_actT[fc], b_wd[di]], writes=[PSB[bank]])
                    k.op("dve", lambda e, tt=tt, dc=dc, ex=ex, bank=bank: e.scalar_tensor_tensor(out=acc[:, tt, dc * 512:(dc + 1) * 512], in0=PS[bank][:, :], scalar=gates[:, tt, ex:ex + 1], in1=acc[:, tt, dc * 512:(dc + 1) * 512], op0=ALU.mult, op1=ALU.add),
                         reads=[PSB[bank], b_gates[tt], b_acc[tt]], writes=[b_acc[tt]])

        issue(chunks[0])
        for ci in range(len(chunks)):
            if ci + 1 < len(chunks):
                issue(chunks[ci + 1])
            compute(chunks[ci])
        for tt in range(4):
            i = tt % 2
            ti = j * 4 + tt
            k.op("act", lambda e, tt=tt, i=i: e.activation(out=nb[i][:, :], in_=acc[:, tt, :], func=AF.Square, accum_out=ss[i][:, :]), reads=[b_acc[tt]], writes=[b_nb[i], b_ss[i]])
            k.op("act", lambda e, i=i: e.activation(out=rstd[i][:, :], in_=ss[i][:, :], func=AF.Sqrt, scale=1.0 / D, bias=epst[EPS]), reads=[b_ss[i]], writes=[b_rstd[i]])
            k.op("dve", lambda e, i=i: e.reciprocal(out=rstd[i][:, :], in_=rstd[i][:, :]), reads=[b_rstd[i]], writes=[b_rstd[i]])
            k.op("dve", lambda e, tt=tt, i=i: e.scalar_tensor_tensor(out=acc[:, tt, :], in0=acc[:, tt, :], scalar=rstd[i][:, :], in1=gfin[:, :], op0=ALU.mult, op1=ALU.mult),
                 reads=[b_acc[tt], b_rstd[i], b_gfin], writes=[b_acc[tt]])
            k.op("sp", lambda e, tt=tt, ti=ti: e.dma_start(out=out_d[ti * 128:(ti + 1) * 128, :], in_=acc[:, tt, :]), reads=[b_acc[tt]], writes=[B_out], dma=True)
        k.barrier()
    ph4.close()
    stack_outer.close()
    return nc, k, None


def _bf(a):
    return np.ascontiguousarray(a).astype(ml_dtypes.bfloat16)


def prep_inputs(inp, SEQ):
    OWN = SEQ // NCORES
    NKT = SEQ // 128
    f = lambda a: np.ascontiguousarray(np.asarray(a, dtype=np.float32))
    x = f(inp["x"])[0]
    rep = lambda v: np.ascontiguousarray(np.broadcast_to(f(v).reshape(1, -1), (128, f(v).size)))
    shared = {
        "mem": f(inp["mem"])[0],
        "w_in": f(inp["w_in"])[0], "w_out": f(inp["w_out"])[0], "w_cq": f(inp["w_cq"])[0],
        "w_ckv": f(inp["w_ckv"])[0], "w_co": f(inp["w_co"])[0], "w_router": f(inp["w_router"])[0],
        "w_gu": f(inp["w_gate_up"])[0], "w_down": f(inp["w_down"])[0],
        "w_rga": f(inp["w_rg_a"])[0], "w_rgx": f(inp["w_rg_x"])[0],
        "g_mix": rep(inp["norm_mix_g"][0]), "g_cross": rep(inp["norm_cross_g"][0]), "g_mem": rep(inp["norm_mem_g"][0]),
        "g_ffn": rep(inp["norm_ffn_g"][0]), "g_final": rep(inp["norm_final_g"]),
        "convw": np.ascontiguousarray(f(inp["conv_w"])[0].reshape(4, 8, 128).transpose(2, 1, 0)),
        "recv": np.ascontiguousarray(np.stack([f(inp[n])[0].reshape(8, 128) for n in ("conv_b", "b_rg_a", "b_rg_x", "rg_lambda", "rec_norm_g")], 0).transpose(2, 0, 1)),
        "lamv": np.ascontiguousarray(np.broadcast_to(np.stack([f(inp[n])[0] for n in ("lambda_q1", "lambda_k1", "lambda_q2", "lambda_k2")], 0)[None], (128, 4, 128))),
        "subg": np.ascontiguousarray(f(inp["subln_g"])[0].reshape(2, 128).T),
        "brt": rep(inp["b_router"][0]),
        "bgu": np.ascontiguousarray(f(inp["b_gate_up"])[0].reshape(-1, 32, 128).transpose(2, 0, 1)),
        "bdown": f(inp["b_down"])[0],
        "ident": _bf(np.eye(128, dtype=np.float32)),
        "identf": np.eye(128, dtype=np.float32),
    }
    kk = np.arange(128)[:, None, None]
    kr = np.arange(4)[None, :, None]
    qq = np.arange(ST)[None, None, :]
    shared["dmask"] = _bf(((kr * 128 + kk) <= qq).astype(np.float32))
    maps = []
    for c in range(NCORES):
        npad = SEQ - OWN * (c + 1)
        xs = np.zeros((SEQ, D), np.float32)
        xs[npad:] = x[: OWN * (c + 1)]
        valid = (np.arange(SEQ) >= npad).astype(np.float32)
        m = dict(shared)
        m["xseq"] = xs
        m["validrow"] = np.ascontiguousarray(np.broadcast_to(valid[None, :], (128, SEQ)))
        m["kbias"] = np.ascontiguousarray(((valid - 1.0) * 30000.0).reshape(NKT, 128).T)
        maps.append(m)
    return maps


def kernel(**inputs):
    SEQ = 16384
    nc, k, L = build(SEQ)
    maps = prep_inputs(inputs, SEQ)
    res = run_bass_kernel_spmd(nc, maps, core_ids=list(range(NCORES)))
    out = np.concatenate([np.asarray(r["out"]) for r in res.results], 0)[None]
    return np.ascontiguousarray(out.astype(np.float32))
```
